# Optimizing a Trainium2 kernel written in Bass

```python
import math
import jax
import jax.numpy as jnp
from jax import lax
import numpy as np

D_MODEL = 1024
BATCH = 4
SEQ = 8192
DEPTH = 1

HEAD_DIM = 64
Q_BLOCK = 128
DIL_GROUPS = ((128, 1), (512, 4), (2048, 16))
A_HEADS_PER_GROUP = 4
A_HEADS = A_HEADS_PER_GROUP * len(DIL_GROUPS)
A_WIDTH = A_HEADS * HEAD_DIM
A_OUT = A_HEADS_PER_GROUP * HEAD_DIM
B_HEADS = 8
B_KV_HEADS = 2
B_GQA = B_HEADS // B_KV_HEADS
B_WIDTH = B_HEADS * HEAD_DIM
B_KV_WIDTH = B_KV_HEADS * HEAD_DIM
CMP_LEN = 32
CMP_STRIDE = 16
CMP_HIDDEN = 256
SLC_LEN = 64
SLC_TOPK = 16
WIN = 512
N_GROUPS = 4
EXP_PER_GROUP = 8
N_EXPERTS = N_GROUPS * EXP_PER_GROUP
D_EXPERT = 512
MOE_TOP_K = 2
MOE_BLOCK = 128
IN_SIZES = (3 * A_WIDTH, B_WIDTH, 6 * B_KV_WIDTH, 3 * B_HEADS, 2 * D_MODEL)
IN_COLS = sum(IN_SIZES)
ALPHA = (2.0 * DEPTH) ** 0.25
BETA = (8.0 * DEPTH) ** -0.25
LN_EPS = 1e-5
FORCE_BONUS = 1e4
TINY = 1e-30
ATTN_SCALE = HEAD_DIM ** -0.5

kernel_name = 'hybrid_dilated_nsa_hmoe_deepnorm'


def layer_norm(x, gain, bias):
    xf = x.astype(jnp.float32)
    mu = jnp.mean(xf, axis=-1, keepdims=True)
    var = jnp.mean(jnp.square(xf - mu), axis=-1, keepdims=True)
    y = (xf - mu) * lax.rsqrt(var + LN_EPS) * gain + bias
    return y.astype(x.dtype)


def alibi_slopes(n):
    return jnp.exp2(-8.0 * jnp.arange(1, n + 1, dtype=jnp.float32) / n)


def masked_softmax(s, mask):
    s = jnp.where(mask, s.astype(jnp.float32), -jnp.inf)
    m = jnp.max(s, axis=-1, keepdims=True)
    m = jnp.where(jnp.isfinite(m), m, 0.0)
    p = jnp.exp(s - m)
    l = jnp.sum(p, axis=-1, keepdims=True)
    return p / jnp.maximum(l, TINY), m + jnp.log(l)


def dilated_group(q, k, v, window, dil, slopes):
    B_, S_, H, E = q.shape
    L = S_ // dil
    nb = -(-L // Q_BLOCK)
    Lp = nb * Q_BLOCK
    n_back = window // dil

    def to_sub(t):
        t = t.reshape(B_, L, dil, H, E).transpose(0, 2, 3, 1, 4)
        t = jnp.pad(t, ((0, 0), (0, 0), (0, 0), (0, Lp - L), (0, 0)))
        return t.reshape(B_, dil, H, nb, Q_BLOCK, E)

    def with_prev(t):
        prev = jnp.pad(t, ((0, 0), (0, 0), (0, 0), (1, 0), (0, 0), (0, 0)))[:, :, :, :-1]
        return jnp.concatenate([prev, t], axis=4)

    qs = to_sub(q)
    kk = with_prev(to_sub(k))
    vv = with_prev(to_sub(v))
    qi = Q_BLOCK + jnp.arange(Q_BLOCK)
    kj = jnp.arange(2 * Q_BLOCK)
    delta = qi[:, None] - kj[None, :]
    key_idx = jnp.arange(nb)[:, None] * Q_BLOCK - Q_BLOCK + kj[None, :]
    mask = ((delta >= 0) & (delta <= n_back))[None] & (key_idx >= 0)[:, None, :]
    s = jnp.einsum('bdhnqe,bdhnke->bdhnqk', qs, kk).astype(jnp.float32) * ATTN_SCALE
    s = s - slopes[:, None, None, None] * (delta * dil).astype(jnp.float32)
    probs, lse = masked_softmax(s, mask)
    o = jnp.einsum('bdhnqk,bdhnke->bdhnqe', probs, vv)
    o = o.reshape(B_, dil, H, Lp, E)[:, :, :, :L].transpose(0, 3, 1, 2, 4).reshape(B_, S_, H, E)
    lse = lse[..., 0].reshape(B_, dil, H, Lp)[:, :, :, :L].transpose(0, 3, 1, 2).reshape(B_, S_, H)
    return o, lse


def dilated_mixer(a_qkv):
    B_, S_, _ = a_qkv.shape
    n_g = len(DIL_GROUPS)
    qkv = a_qkv.reshape(B_, S_, 3, n_g, A_HEADS_PER_GROUP, HEAD_DIM)
    slopes = alibi_slopes(A_HEADS).reshape(n_g, A_HEADS_PER_GROUP)
    outs, lses = [], []
    for g, (window, dil) in enumerate(DIL_GROUPS):
        o, lse = dilated_group(qkv[:, :, 0, g], qkv[:, :, 1, g], qkv[:, :, 2, g], window, dil, slopes[g])
        outs.append(o)
        lses.append(lse)
    w = jax.nn.softmax(jnp.stack(lses), axis=0)
    o = jnp.sum(w[..., None] * jnp.stack(outs), axis=0)
    return o.reshape(B_, S_, A_OUT)


def compress_tokens(kv, pos_emb, w1, w2):
    B_, S_, G, E = kv.shape
    n_chunk = S_ // CMP_STRIDE
    ratio = CMP_LEN // CMP_STRIDE
    n_cmp = n_chunk - ratio + 1
    chunks = kv.reshape(B_, n_chunk, CMP_STRIDE, G, E)
    blocks = jnp.concatenate([chunks[:, r:r + n_cmp] for r in range(ratio)], axis=2)
    blocks = blocks + pos_emb[:, None, :]
    flat = blocks.transpose(0, 1, 3, 2, 4).reshape(B_, n_cmp, G, CMP_LEN * E)
    return jax.nn.gelu(flat @ w1) @ w2


def nsa_mixer(b_q, b_kv, b_gate, cmp_pos_k, cmp_w1_k, cmp_w2_k, cmp_pos_v, cmp_w1_v, cmp_w2_v):
    B_, S_, _ = b_q.shape
    G, R, E = B_KV_HEADS, B_GQA, HEAD_DIM
    q = b_q.reshape(B_, S_, G, R, E)
    kv = b_kv.reshape(B_, S_, 6, G, E)
    k_cmp = compress_tokens(kv[:, :, 0], cmp_pos_k, cmp_w1_k, cmp_w2_k)
    v_cmp = compress_tokens(kv[:, :, 1], cmp_pos_v, cmp_w1_v, cmp_w2_v)
    n_cmp = k_cmp.shape[1]
    n_slc = S_ // SLC_LEN
    n_sel = min(SLC_TOPK, n_slc)
    k_blk = kv[:, :, 2].reshape(B_, n_slc, SLC_LEN, G, E).transpose(0, 3, 1, 2, 4)
    v_blk = kv[:, :, 3].reshape(B_, n_slc, SLC_LEN, G, E).transpose(0, 3, 1, 2, 4)
    pad = ((0, 0), (WIN, 0), (0, 0), (0, 0))
    k_win = jnp.pad(kv[:, :, 4], pad)
    v_win = jnp.pad(kv[:, :, 5], pad)
    gates = jax.nn.sigmoid(b_gate).reshape(B_, S_, G, R, 3)
    slopes = alibi_slopes(B_HEADS).reshape(G, R)
    c_start = jnp.arange(n_cmp) * CMP_STRIDE
    cmp_end = c_start + (CMP_LEN - 1)
    s_start = jnp.arange(n_slc) * SLC_LEN
    overlap = ((c_start[:, None] < s_start[None, :] + SLC_LEN) &
               (c_start[:, None] + CMP_LEN > s_start[None, :])).astype(jnp.float32)
    blk_ids = jnp.arange(n_slc)
    bi = jnp.arange(B_)[:, None, None, None]
    gi = jnp.arange(G)[None, :, None, None]

    def one_block(n):
        t0 = n * Q_BLOCK
        t = t0 + jnp.arange(Q_BLOCK)
        qb = lax.dynamic_slice_in_dim(q, t0, Q_BLOCK, axis=1).transpose(0, 2, 3, 1, 4)
        gb = lax.dynamic_slice_in_dim(gates, t0, Q_BLOCK, axis=1).transpose(0, 2, 3, 1, 4)
        s_c = jnp.einsum('bgrqe,bcge->bgrqc', qb, k_cmp).astype(jnp.float32) * ATTN_SCALE
        p_c, _ = masked_softmax(s_c, cmp_end[None, :] <= t[:, None])
        o_c = jnp.einsum('bgrqc,bcge->bgrqe', p_c, v_cmp)
        imp = jnp.einsum('bgrqc,cj->bgqj', p_c, overlap)
        cur = t // SLC_LEN
        valid = blk_ids[None, :] * SLC_LEN <= t[:, None]
        forced = (blk_ids[None, :] == 0) | (blk_ids[None, :] == cur[:, None]) | (blk_ids[None, :] == cur[:, None] - 1)
        score = jnp.where(valid, imp + FORCE_BONUS * forced.astype(jnp.float32), -jnp.inf)
        top_s, idx = lax.top_k(score, n_sel)
        k_sel = k_blk[bi, gi, idx]
        v_sel = v_blk[bi, gi, idx]
        pos = idx[..., None] * SLC_LEN + jnp.arange(SLC_LEN)
        dist = t[:, None, None] - pos
        mask_s = (jnp.isfinite(top_s)[..., None] & (dist >= 0))[:, :, None]
        s_s = jnp.einsum('bgrqe,bgqkle->bgrqkl', qb, k_sel).astype(jnp.float32) * ATTN_SCALE
        s_s = s_s - slopes[:, :, None, None, None] * dist[:, :, None].astype(jnp.float32)
        nk = n_sel * SLC_LEN
        p_s, _ = masked_softmax(s_s.reshape(B_, G, R, Q_BLOCK, nk), mask_s.reshape(B_, G, 1, Q_BLOCK, nk))
        o_s = jnp.einsum('bgrqn,bgqne->bgrqe', p_s, v_sel.reshape(B_, G, Q_BLOCK, nk, E))
        kw = lax.dynamic_slice_in_dim(k_win, t0, Q_BLOCK + WIN, axis=1)
        vw = lax.dynamic_slice_in_dim(v_win, t0, Q_BLOCK + WIN, axis=1)
        kpos = t0 - WIN + jnp.arange(Q_BLOCK + WIN)
        dw = t[:, None] - kpos[None, :]
        mask_w = (dw >= 0) & (dw < WIN) & (kpos[None, :] >= 0)
        s_w = jnp.einsum('bgrqe,bkge->bgrqk', qb, kw).astype(jnp.float32) * ATTN_SCALE
        s_w = s_w - slopes[:, :, None, None] * dw.astype(jnp.float32)
        p_w, _ = masked_softmax(s_w, mask_w)
        o_w = jnp.einsum('bgrqk,bkge->bgrqe', p_w, vw)
        o = gb[..., 0:1] * o_c + gb[..., 1:2] * o_s + gb[..., 2:3] * o_w
        return o.transpose(0, 3, 1, 2, 4).reshape(B_, Q_BLOCK, B_WIDTH)

    out = lax.map(one_block, jnp.arange(S_ // Q_BLOCK))
    return out.transpose(1, 0, 2, 3).reshape(B_, S_, B_WIDTH)


def hybrid_mixer(x, w_in, cmp_pos_k, cmp_w1_k, cmp_w2_k, cmp_pos_v, cmp_w1_v, cmp_w2_v,
                 w_branch_a, w_branch_b, w_out):
    u = x @ w_in
    split_idx = np.cumsum(IN_SIZES)[:-1].tolist()
    a_qkv, b_q, b_kv, b_gate, m_gate = jnp.split(u, split_idx, axis=-1)
    y_a = dilated_mixer(a_qkv)
    y_b = nsa_mixer(b_q, b_kv, b_gate, cmp_pos_k, cmp_w1_k, cmp_w2_k, cmp_pos_v, cmp_w1_v, cmp_w2_v)
    g_a, g_b = jnp.split(jax.nn.sigmoid(m_gate), 2, axis=-1)
    merged = g_a * (y_a @ w_branch_a) + g_b * (y_b @ w_branch_b)
    return merged @ w_out


def hier_moe(h, w_coarse, b_coarse, w_fine, b_fine, w_gate_up, w_down):
    B_, S_, D = h.shape
    T = B_ * S_
    xt = h.reshape(T, D)
    lg = (xt @ w_coarse).astype(jnp.float32) + b_coarse
    grp = jnp.argmax(lg, axis=-1)
    p_grp = jnp.take_along_axis(jax.nn.softmax(lg, axis=-1), grp[:, None], axis=-1)
    lf = jnp.einsum('td,gde->tge', xt, w_fine).astype(jnp.float32) + b_fine
    lf = jnp.take_along_axis(lf, grp[:, None, None], axis=1)[:, 0]
    top_v, top_i = lax.top_k(lf, MOE_TOP_K)
    weights = p_grp * jax.nn.softmax(top_v, axis=-1)
    expert = grp[:, None] * EXP_PER_GROUP + top_i
    n_assign = T * MOE_TOP_K
    e_flat = expert.reshape(n_assign)
    w_flat = weights.reshape(n_assign)
    tok = jnp.arange(n_assign) // MOE_TOP_K
    order = jnp.argsort(e_flat)
    e_s, tok_s, w_s = e_flat[order], tok[order], w_flat[order]
    counts = jnp.zeros((N_EXPERTS,), jnp.int32).at[e_flat].add(1)
    starts = jnp.cumsum(counts) - counts
    pcounts = (counts + MOE_BLOCK - 1) // MOE_BLOCK * MOE_BLOCK
    pends = jnp.cumsum(pcounts)
    pstarts = pends - pcounts
    dest = pstarts[e_s] + (jnp.arange(n_assign) - starts[e_s])
    P = -(-(n_assign + N_EXPERTS * (MOE_BLOCK - 1)) // MOE_BLOCK) * MOE_BLOCK
    nb = P // MOE_BLOCK
    tok_buf = jnp.full((P,), T, jnp.int32).at[dest].set(tok_s.astype(jnp.int32))
    w_buf = jnp.zeros((P,), jnp.float32).at[dest].set(w_s)
    blk_e = jnp.minimum(jnp.searchsorted(pends, jnp.arange(nb) * MOE_BLOCK, side='right'), N_EXPERTS - 1)
    x_pad = jnp.concatenate([xt, jnp.zeros((1, D), xt.dtype)], axis=0)
    xb = x_pad[tok_buf].reshape(nb, MOE_BLOCK, D)

    def expert_block(args):
        xblk, e = args
        g, u = jnp.split(xblk @ w_gate_up[e], 2, axis=-1)
        return (jax.nn.silu(g) * u) @ w_down[e]

    yb = lax.map(expert_block, (xb, blk_e)).reshape(P, D)
    y = jax.ops.segment_sum(yb * w_buf[:, None], tok_buf, num_segments=T + 1)[:T]
    return y.reshape(B_, S_, D).astype(h.dtype)


def setup_inputs(seed: int = 0) -> dict:
    key = jax.random.key(seed)
    ks = jax.random.split(key, 22)
    L, D, E = DEPTH, D_MODEL, HEAD_DIM

    def nrm(k, shape, scale):
        return jax.random.normal(k, shape, jnp.float32) * scale

    col_scale = np.ones((IN_COLS,), np.float32)
    col_scale[2 * A_WIDTH:3 * A_WIDTH] = BETA
    kv_off = 3 * A_WIDTH + B_WIDTH
    for i in (1, 3, 5):
        col_scale[kv_off + i * B_KV_WIDTH:kv_off + (i + 1) * B_KV_WIDTH] = BETA
    return {
        'x': nrm(ks[0], (BATCH, SEQ, D), 1.0),
        'w_in': nrm(ks[1], (L, D, IN_COLS), D ** -0.5) * jnp.asarray(col_scale),
        'cmp_pos_k': nrm(ks[2], (L, CMP_LEN, E), 0.5),
        'cmp_w1_k': nrm(ks[3], (L, CMP_LEN * E, CMP_HIDDEN), (CMP_LEN * E) ** -0.5),
        'cmp_w2_k': nrm(ks[4], (L, CMP_HIDDEN, E), CMP_HIDDEN ** -0.5),
        'cmp_pos_v': nrm(ks[5], (L, CMP_LEN, E), 0.5),
        'cmp_w1_v': nrm(ks[6], (L, CMP_LEN * E, CMP_HIDDEN), (CMP_LEN * E) ** -0.5),
        'cmp_w2_v': nrm(ks[7], (L, CMP_HIDDEN, E), CMP_HIDDEN ** -0.5),
        'w_branch_a': nrm(ks[8], (L, A_OUT, D), A_OUT ** -0.5),
        'w_branch_b': nrm(ks[9], (L, B_WIDTH, D), B_WIDTH ** -0.5),
        'w_out': nrm(ks[10], (L, D, D), BETA * D ** -0.5),
        'ln1_g': 1.0 + nrm(ks[11], (L, D), 0.02),
        'ln1_b': nrm(ks[12], (L, D), 0.02),
        'w_coarse': nrm(ks[13], (L, D, N_GROUPS), D ** -0.5),
        'b_coarse': nrm(ks[14], (L, N_GROUPS), 0.01),
        'w_fine': nrm(ks[15], (L, N_GROUPS, D, EXP_PER_GROUP), D ** -0.5),
        'b_fine': nrm(ks[16], (L, N_GROUPS, EXP_PER_GROUP), 0.01),
        'w_gate_up': nrm(ks[17], (L, N_EXPERTS, D, 2 * D_EXPERT), D ** -0.5),
        'w_down': nrm(ks[18], (L, N_EXPERTS, D_EXPERT, D), BETA * D_EXPERT ** -0.5),
        'ln2_g': 1.0 + nrm(ks[19], (L, D), 0.02),
        'ln2_b': nrm(ks[20], (L, D), 0.02),
    }


def reference(x, w_in, cmp_pos_k, cmp_w1_k, cmp_w2_k, cmp_pos_v, cmp_w1_v, cmp_w2_v,
              w_branch_a, w_branch_b, w_out, ln1_g, ln1_b,
              w_coarse, b_coarse, w_fine, b_fine, w_gate_up, w_down, ln2_g, ln2_b):
    h = x
    for l in range(DEPTH):
        mix = hybrid_mixer(h, w_in[l], cmp_pos_k[l], cmp_w1_k[l], cmp_w2_k[l],
                           cmp_pos_v[l], cmp_w1_v[l], cmp_w2_v[l],
                           w_branch_a[l], w_branch_b[l], w_out[l])
        h = layer_norm(ALPHA * h + mix, ln1_g[l], ln1_b[l])
        ffn = hier_moe(h, w_coarse[l], b_coarse[l], w_fine[l], b_fine[l], w_gate_up[l], w_down[l])
        h = layer_norm(ALPHA * h + ffn, ln2_g[l], ln2_b[l])
    return h
```

```python
import contextlib
import numpy as np
import ml_dtypes
import concourse.bass as bass
import concourse.mybir as mybir
from concourse.bass_utils import run_bass_kernel_spmd
from concourse.alu_op_type import AluOpType as ALU

F32 = mybir.dt.float32
BF16 = mybir.dt.bfloat16
AF = mybir.ActivationFunctionType
AX = mybir.AxisListType

D = 1024
SEQ = 8192
NB = 4
OWN = 4096
NT = OWN // 128
HD = 64
DIL = ((128, 1), (512, 4), (2048, 16))
IN_COLS = 5656
C_AQ, C_AK, C_AV = 0, 768, 1536
C_BQ = 2304
C_BKV = 2816
C_BG = 3584
C_MG = 3608
NEG = -30000.0
ALPHA = 2.0 ** 0.25
LN_EPS = 1e-5
N_EXP = 32
D_EXP = 512

DEBUG = {}


class Trk:
    __slots__ = ("w", "r", "x")

    def __init__(self, x=False):
        self.w = None
        self.r = {}
        self.x = x


class TK:
    def __init__(self):
        self.d = {}

    def __getitem__(self, k):
        t = self.d.get(k)
        if t is None:
            t = self.d[k] = Trk()
        return t

    def all(self):
        return list(self.d.values())


class Sy:
    def __init__(self, nc):
        self.nc = nc
        self.eng = {}
        for name in ("tensor", "vector", "scalar", "gpsimd", "sync"):
            self.eng[name] = dict(e=getattr(nc, name), sem=nc.alloc_semaphore(f"s_{name}"), cnt=0, known={})
        self.dsem = {}
        self.ninst = 0
        self.epoch = 0

    def new_epoch(self):
        self.barrier()
        self.epoch += 1
        for name, E in self.eng.items():
            E["sem"] = self.nc.alloc_semaphore(f"s_{name}_{self.epoch}")
            E["cnt"] = 0
            E["known"] = {}
        self.dsem = {}

    def _wait(self, E, deps):
        best = {}
        for sem, val in deps:
            k = id(sem)
            if k not in best or best[k][1] < val:
                best[k] = (sem, val)
        for k, (sem, val) in best.items():
            if E["known"].get(k, 0) >= val:
                continue
            E["e"].wait_ge(sem, val)
            E["known"][k] = val
            self.ninst += 1

    def _deps(self, E, reads, writes, skip_own):
        deps = []
        for t in reads:
            if t.w is not None:
                deps.append(t.w)
        for t in writes:
            if t.w is not None:
                deps.append(t.w)
            deps.extend(t.r.values())
        if skip_own:
            deps = [d for d in deps if d[0] is not E["sem"]]
        return deps

    def op(self, name, fn, reads=(), writes=()):
        E = self.eng[name]
        if any(t.x for t in reads):
            writes = list(writes) + [t for t in reads if t.x]
            reads = [t for t in reads if not t.x]
        self._wait(E, self._deps(E, reads, writes, name == "tensor"))
        ins = fn(E["e"])
        E["cnt"] += 1
        ins.then_inc(E["sem"], 1)
        self.ninst += 1
        tok = (E["sem"], E["cnt"])
        for t in writes:
            t.w = tok
            t.r = {}
        for t in reads:
            t.r[id(E["sem"])] = tok
        return tok

    def dma(self, qname, out, in_, reads=(), writes=(), stream="d"):
        E = self.eng[qname]
        S = self.dsem.get(stream)
        if S is None:
            S = self.dsem[stream] = dict(sem=self.nc.alloc_semaphore(f"d_{stream}_{self.epoch}"), cnt=0)
        self._wait(E, self._deps(E, reads, writes, False))
        ins = E["e"].dma_start(out=out, in_=in_)
        S["cnt"] += 1
        ins.then_inc(S["sem"], 16)
        self.ninst += 1
        tok = (S["sem"], 16 * S["cnt"])
        for t in writes:
            t.w = tok
            t.r = {}
        for t in reads:
            t.r[id(S["sem"])] = tok
        return tok

    def barrier(self):
        toks = [(E["sem"], E["cnt"]) for E in self.eng.values() if E["cnt"] > 0]
        toks += [(S["sem"], 16 * S["cnt"]) for S in self.dsem.values()]
        for E in self.eng.values():
            self._wait(E, [t for t in toks if t[0] is not E["sem"]])


def SSL(base, d):
    return slice(base, base + 127 * d + 1, d)


def _bf(a):
    return np.asarray(a, dtype=np.float32).astype(ml_dtypes.bfloat16)


def alibi(n):
    return np.exp2(-8.0 * np.arange(1, n + 1, dtype=np.float32) / n).astype(np.float32)


def make_consts(hf):
    c = {}
    c["ident"] = np.eye(128, dtype=np.float32)
    c["identb"] = _bf(np.eye(128))
    k = np.arange(128)[:, None]
    q = np.arange(128)[None, :]
    sl = alibi(12).reshape(3, 4)
    bm = np.zeros((128, 3, 4, 2, 128), np.float32)
    for g, (win, d) in enumerate(DIL):
        for h in range(4):
            dprev = (q - k + 128).astype(np.float32)
            dcur = (q - k).astype(np.float32)
            bm[:, g, h, 0, :] = np.where(dprev <= 128, -8.0 * sl[g, h] * d * dprev, 8.0 * NEG)
            bm[:, g, h, 1, :] = np.where(dcur >= 0, -8.0 * sl[g, h] * d * dcur, 8.0 * NEG)
    c["bmA"] = bm.reshape(128, -1)
    c["vflag"] = np.tile(np.array([[float(hf), 1.0]], np.float32), (128, 1))
    slb = alibi(8)
    u = np.arange(2 * OWN)
    c["kaug"] = _bf(np.stack([(u % 128) - 64.0, u // 128, np.ones_like(u)]).astype(np.float32))
    qa = np.zeros((3, 8, OWN), np.float32)
    qt = 32 + np.arange(OWN) // 128
    for h in range(8):
        qa[0, h] = 8.0 * slb[h]
        qa[1, h] = 1024.0 * slb[h]
        qa[2, h] = -1024.0 * slb[h] * qt
    c["qaug"] = _bf(qa.reshape(3, 8 * OWN))
    cup = np.arange(128)[:, None]
    mcb = np.zeros((128, 17, 128), np.float32)
    for idx in range(17):
        off = idx * 8 - 2
        mcb[:, idx, :] = np.where(cup <= off + (q + 1) // 16, 0.0, NEG)
    c["mcb"] = _bf(mcb.reshape(128, -1))
    cu = np.arange(512)
    cval = ((cu <= 510) & ((cu >= 256) | (hf == 1))).astype(np.float32)
    c["cvalid"] = np.ascontiguousarray(cval.reshape(4, 128).T)
    c["validrep"] = _bf(np.repeat(cval.reshape(4, 128).T[:, :, None], 128, axis=2).reshape(128, -1))
    jb = np.arange(128)
    ovl = ((16 * cu[:, None] < 64 * jb[None, :] + 64) & (16 * cu[:, None] + 32 > 64 * jb[None, :])).astype(np.float32)
    ovl = ovl * cval[:, None]
    c["ovl"] = _bf(ovl.reshape(4, 128, 128).transpose(1, 0, 2).reshape(128, -1))
    wd = np.zeros((128, 190), np.float32)
    qq = np.arange(128)[:, None]
    jj = np.arange(190)[None, :] - 62
    cur = 64 + (qq >= 64)
    wd = np.where(jj > cur, -1.0e9, np.where((jj == cur) | (jj == cur - 1), 1.0e4, 0.0)).astype(np.float32)
    c["wd"] = wd
    fb = np.zeros((128,), np.float32)
    if hf == 1:
        fb[0] = 1.0e4
    else:
        fb[:64] = -1.0e9
        fb[64] = 1.0e4
    c["fbvec"] = np.tile(fb[None, :], (128, 1)).astype(np.float32)
    ind = np.zeros((128, 64, 128), np.float32)
    for kt in range(64):
        ind[2 * kt, kt, 0:64] = 1.0
        ind[2 * kt + 1, kt, 64:128] = 1.0
    c["indbig"] = _bf(ind.reshape(128, -1))
    c["tri_le"] = _bf(np.where(k <= q, 0.0, NEG))
    c["tri_gt"] = _bf(np.where(k > q, 0.0, NEG))
    return c


def weight_layouts(inp):
    w = {}
    w["w_in"] = np.ascontiguousarray(inp["w_in"][0])
    for kv in ("k", "v"):
        w1 = np.asarray(inp[f"cmp_w1_{kv}"][0])
        w[f"w1r_{kv}"] = np.ascontiguousarray(w1.reshape(32, 64, 256).transpose(1, 0, 2).reshape(64, 32 * 256))
        w[f"posT_{kv}"] = np.ascontiguousarray(np.asarray(inp[f"cmp_pos_{kv}"][0]).T)
        w[f"w2_{kv}"] = np.ascontiguousarray(inp[f"cmp_w2_{kv}"][0])
    w["w_ba"] = np.ascontiguousarray(inp["w_branch_a"][0])
    w["w_bb"] = np.ascontiguousarray(inp["w_branch_b"][0])
    w["w_out"] = np.ascontiguousarray(inp["w_out"][0])
    ln = np.concatenate([np.asarray(inp[k][0]).reshape(1, D) for k in ("ln1_g", "ln1_b", "ln2_g", "ln2_b")], axis=1)
    w["lnrep"] = np.ascontiguousarray(np.broadcast_to(ln, (128, 4 * D))).astype(np.float32)
    wf = np.asarray(inp["w_fine"][0]).transpose(1, 0, 2).reshape(D, 32)
    w["w_router"] = np.ascontiguousarray(np.concatenate([np.asarray(inp["w_coarse"][0]), wf], axis=1)).astype(np.float32)
    br = np.concatenate([np.asarray(inp["b_coarse"][0]).reshape(1, 4), np.asarray(inp["b_fine"][0]).reshape(1, 32)], axis=1)
    w["b_router"] = np.ascontiguousarray(np.broadcast_to(br, (128, 36))).astype(np.float32)
    if "w_gate_up" in inp:
        w["w_gu"] = np.ascontiguousarray(inp["w_gate_up"][0])
        w["w_dn"] = np.ascontiguousarray(inp["w_down"][0])
    return w


class Prog:
    def __init__(self, dbg=None):
        self.dbg = dbg or {}
        self.nc = bass.Bass("TRN2", target_bir_lowering=False)
        self.sy = Sy(self.nc)
        self.ins = {}
        self.outs = {}

    def din(self, name, shape, dt=F32):
        ap = self.nc.dram_tensor(name, list(shape), dt, kind="ExternalInput").ap()
        self.ins[name] = ap
        return ap

    def dout(self, name, shape, dt=F32):
        ap = self.nc.dram_tensor(name, list(shape), dt, kind="ExternalOutput").ap()
        self.outs[name] = ap
        return ap

    def dscratch(self, name, shape, dt=F32):
        return self.nc.dram_tensor(name, list(shape), dt, kind="Internal").ap()


def build_program(dbg=None):
    P = Prog(dbg)
    nc, sy = P.nc, P.sy
    dbg = P.dbg
    xin = P.din("xin", [2 * OWN, D])
    w_in = P.din("w_in", [D, IN_COLS])
    ident_d = P.din("ident", [128, 128])
    identb_d = P.din("identb", [128, 128], BF16)
    bmA_d = P.din("bmA", [128, 3 * 4 * 2 * 128])
    vflag_d = P.din("vflag", [128, 2])
    out = P.dout("out", [OWN, D])
    cd = {}
    for nm, shp, dt in (("kaug", [3, 2 * OWN], BF16), ("qaug", [3, 8 * OWN], BF16), ("mcb", [128, 17 * 128], BF16),
                        ("cvalid", [128, 4], F32), ("validrep", [128, 512], BF16), ("ovl", [128, 512], BF16),
                        ("wd", [128, 190], F32), ("fbvec", [128, 128], F32), ("indbig", [128, 64 * 128], BF16),
                        ("tri_le", [128, 128], BF16), ("tri_gt", [128, 128], BF16),
                        ("w1r_k", [64, 32 * 256], F32), ("posT_k", [64, 32], F32), ("w2_k", [256, 64], F32),
                        ("w1r_v", [64, 32 * 256], F32), ("posT_v", [64, 32], F32), ("w2_v", [256, 64], F32),
                        ("w_ba", [256, D], F32), ("w_bb", [512, D], F32), ("w_out", [D, D], F32),
                        ("lnrep", [128, 4 * D], F32), ("w_router", [D, 36], F32), ("b_router", [128, 36], F32),
                        ("w_gu", [N_EXP, D, 2 * D_EXP], F32), ("w_dn", [N_EXP, D_EXP, D], F32)):
        if nm in ("w_gu", "w_dn") and dbg.get("skipD"):
            continue
        cd[nm] = P.din(nm, shp, dt)
    h1_d = P.dscratch("h1_scratch", [OWN, D])

    es_glob = contextlib.ExitStack()
    SB = lambda es, name, shape, dt: es.enter_context(nc.sbuf_tensor(name, list(shape), dt))
    psb = [es_glob.enter_context(nc.psum_tensor(f"ps{i}", [128, 512], F32)) for i in range(8)]
    pst = [Trk(True) for _ in range(8)]

    ident = SB(es_glob, "ident_s", [128, 128], F32)
    identb = SB(es_glob, "identb_s", [128, 128], BF16)
    vflag = SB(es_glob, "vflag_s", [128, 2], F32)
    tC = TK()
    sy.dma("sync", ident[:], ident_d[:, :], writes=[tC["ident"]], stream="c")
    sy.dma("sync", identb[:], identb_d[:, :], writes=[tC["identb"]], stream="c")
    sy.dma("sync", vflag[:], vflag_d[:, :], writes=[tC["vflag"]], stream="c")

    Wt = SB(es_glob, "Wt", [128, NT, 32], F32)
    tH = TK()
    es_y = contextlib.ExitStack()
    yaT = SB(es_y, "yaT", [128, 2, OWN], BF16)
    tYa = TK()

    def stage_A():
        es = contextlib.ExitStack()
        xs = [SB(es, f"A_xs{i}", [128, D], F32) for i in range(2)]
        xT = SB(es, "A_xT", [128, 8, 2048], BF16)
        wst = [SB(es, "A_wst0", [128, 2, 768], F32)] * 2
        wA = SB(es, "A_w", [128, 8, 768], BF16)
        bmA = SB(es, "A_bm", [128, 4, 2, 128], F32)
        Kp = [SB(es, f"A_Kp{g}", [64, 4, 128 * d], BF16) for g, (_, d) in enumerate(DIL)]
        Vp = [SB(es, f"A_Vp{g}", [128, d, 4, 128], BF16) for g, (_, d) in enumerate(DIL)]
        Kc = SB(es, "A_Kc", [64, 4, 2048], BF16)
        Qc = SB(es, "A_Qc", [64, 4, 2048], BF16)
        Vc = SB(es, "A_Vc", [128, 16, 4, 128], BF16)
        acc = SB(es, "A_acc", [128, 4, 2048], F32)
        PT = [SB(es, f"A_PT{i}", [128, 512], BF16) for i in range(3)]
        t = TK()
        ps_rot = [0]

        def next_ps():
            i = ps_rot[0]
            ps_rot[0] = (i + 1) % 8
            return i

        xin_t = xin.rearrange("(n p) d -> n p d", p=128)
        nload = [0]

        def load_xT(tile0, ntiles):
            for j in range(ntiles):
                s = nload[0] % 2
                nload[0] += 1
                sy.dma("sync", xs[s][:], xin_t[tile0 + j, :, :], writes=[t[("xs", s)]], stream=f"x{s}")
                for half in range(2):
                    b = next_ps()
                    for kk in range(4):
                        kc = half * 4 + kk
                        sy.op("tensor", lambda e, b=b, kk=kk, kc=kc, s=s: e.transpose(
                            out=psb[b][:, kk * 128:(kk + 1) * 128], in_=xs[s][:, kc * 128:(kc + 1) * 128], identity=ident[:]),
                            reads=[t[("xs", s)], tC["ident"]], writes=[pst[b]])
                    eng = "vector" if half == 0 else "scalar"
                    if eng == "vector":
                        sy.op("vector", lambda e, b=b, half=half, j=j: e.tensor_copy(
                            out=xT[:, half * 4:half * 4 + 4, j * 128:(j + 1) * 128],
                            in_=psb[b][:, :].rearrange("p (k c) -> p k c", k=4)),
                            reads=[pst[b]], writes=[t[("xT", j, half)]])
                    else:
                        sy.op("scalar", lambda e, b=b, half=half, j=j: e.copy(
                            out=xT[:, half * 4:half * 4 + 4, j * 128:(j + 1) * 128],
                            in_=psb[b][:, :].rearrange("p (k c) -> p k c", k=4)),
                            reads=[pst[b]], writes=[t[("xT", j, half)]])

        def load_wA(g):
            sy.dma("gpsimd", bmA[:].rearrange("p b c d -> p (b c d)"), bmA_d[:, g * 1024:(g + 1) * 1024], writes=[t["bm"]], stream="c")
            for kc2 in range(4):
                s = 0
                for part, c0 in enumerate((C_AQ, C_AK, C_AV)):
                    col = c0 + g * 256
                    sy.dma("gpsimd", wst[s][:, :, part * 256:(part + 1) * 256],
                           w_in[kc2 * 256:(kc2 + 1) * 256, col:col + 256].rearrange("(k p) c -> p k c", p=128),
                           writes=[t[("wst", s)]], stream=f"w{s}")
                sy.op("gpsimd", lambda e, s=s, kc2=kc2: e.tensor_copy(out=wA[:, kc2 * 2:kc2 * 2 + 2, :], in_=wst[s][:]),
                      reads=[t[("wst", s)]], writes=[t["wA"]])

        xT_all = [t[("xT", j, hh)] for j in range(16) for hh in range(2)]
        aslopes = alibi(12).reshape(3, 4)

        for sc in (-1, 0, 1):
            own = sc >= 0
            tile0 = 32 + sc * 16
            load_xT(tile0, 16)
            for g, (win, d) in enumerate(DIL):
                nblk = 16 // d
                load_wA(g)
                for which, dst, cbase in (("q", Qc, 0), ("k", Kc, 256)):
                    if which == "q" and not own:
                        continue
                    for h in range(4):
                        for nck in range(4):
                            b = next_ps()
                            for kc in range(8):
                                sy.op("tensor", lambda e, b=b, kc=kc, h=h, nck=nck, cbase=cbase: e.matmul(
                                    psb[b][0:64, :], lhsT=wA[:, kc, cbase + h * 64:cbase + (h + 1) * 64],
                                    rhs=xT[:, kc, nck * 512:(nck + 1) * 512], start=(kc == 0), stop=(kc == 7)),
                                    reads=[t["wA"]] + xT_all[nck * 8:nck * 8 + 8], writes=[pst[b]])
                            eng = "vector" if (h + nck) % 2 == 0 else "scalar"
                            if eng == "vector":
                                sy.op("vector", lambda e, b=b, dst=dst, h=h, nck=nck: e.tensor_copy(
                                    out=dst[:, h, nck * 512:(nck + 1) * 512], in_=psb[b][0:64, :]),
                                    reads=[pst[b]], writes=[t[(which, h)]])
                            else:
                                sy.op("scalar", lambda e, b=b, dst=dst, h=h, nck=nck: e.copy(
                                    out=dst[:, h, nck * 512:(nck + 1) * 512], in_=psb[b][0:64, :]),
                                    reads=[pst[b]], writes=[t[(which, h)]])
                for n in range(nblk):
                    for r in range(d):
                        ti = n * d + r
                        b = next_ps()
                        base = n * 128 * d + r
                        for kc in range(8):
                            sy.op("tensor", lambda e, b=b, kc=kc, base=base, d=d: e.matmul(
                                psb[b][:, 0:256], lhsT=xT[:, kc, SSL(base, d)] if d > 1 else xT[:, kc, base:base + 128],
                                rhs=wA[:, kc, 512:768], start=(kc == 0), stop=(kc == 7)),
                                reads=[t["wA"]] + xT_all, writes=[pst[b]])
                        sy.op("vector", lambda e, b=b, ti=ti: e.tensor_copy(
                            out=Vc[:, ti, :, 0:64], in_=psb[b][:, 0:256].rearrange("p (h e) -> p h e", h=4)),
                            reads=[pst[b]], writes=[t[("V", ti)]])
                        fcol = 1 if own else 0
                        sy.op("gpsimd", lambda e, ti=ti, fcol=fcol: e.tensor_copy(
                            out=Vc[:, ti, :, 64:128], in_=vflag[:, None, fcol:fcol + 1].to_broadcast([128, 4, 64])),
                            reads=[tC["vflag"]], writes=[t[("Vf", ti)]])
                if own:
                    for h in range(4):
                        pairs = [(r, n) for n in range(nblk) for r in range(d)]
                        for p0 in range(0, 16, 4):
                            grp = pairs[p0:p0 + 4]
                            pvb = next_ps()
                            for half in range(2):
                                sb = next_ps()
                                pt = PT[(p0 // 4 * 2 + half) % 3]
                                ptk = t[("PT", (p0 // 4 * 2 + half) % 3)]
                                sub = grp[half * 2:half * 2 + 2]
                                first = True
                                for si, (r, n) in enumerate(sub):
                                    qsl = Qc[:, h, SSL(n * 128 * d + r, d)] if d > 1 else Qc[:, h, n * 128:(n + 1) * 128]
                                    for pc in range(2):
                                        col = (si * 2 + pc) * 128
                                        if pc == 1:
                                            ksl = Kc[:, h, SSL(n * 128 * d + r, d)] if d > 1 else Kc[:, h, n * 128:(n + 1) * 128]
                                            kr = [t[("k", h)]]
                                        elif n > 0:
                                            ksl = Kc[:, h, SSL((n - 1) * 128 * d + r, d)] if d > 1 else Kc[:, h, (n - 1) * 128:n * 128]
                                            kr = [t[("k", h)]]
                                        else:
                                            ksl = Kp[g][:, h, SSL(r, d)] if d > 1 else Kp[g][:, h, 0:128]
                                            kr = [t[("Kp", g)]]
                                        sy.op("tensor", lambda e, sb=sb, col=col, ksl=ksl, qsl=qsl, first=first: e.matmul(
                                            psb[sb][:, col:col + 128], lhsT=ksl, rhs=qsl, start=first, stop=False, skip_group_check=True),
                                            reads=kr + [t[("q", h)]], writes=[pst[sb]])
                                        first = False
                                        sy.op("tensor", lambda e, sb=sb, col=col, g=g, h=h, pc=pc: e.matmul(
                                            psb[sb][:, col:col + 128], lhsT=ident[:], rhs=bmA[:, h, pc, :], start=False, stop=True, skip_group_check=True),
                                            reads=[tC["ident"], t["bm"]], writes=[pst[sb]])
                                sy.op("scalar", lambda e, sb=sb, pt=pt: e.activation(out=pt[:], in_=psb[sb][:, :], func=AF.Exp, scale=0.125),
                                      reads=[pst[sb]], writes=[ptk])
                                for si, (r, n) in enumerate(sub):
                                    reg = (half * 2 + si) * 128
                                    for pc in range(2):
                                        col = (si * 2 + pc) * 128
                                        if pc == 1:
                                            vsl = Vc[:, n * d + r, h, :]
                                            vr = [t[("V", n * d + r)], t[("Vf", n * d + r)]]
                                        elif n > 0:
                                            vsl = Vc[:, (n - 1) * d + r, h, :]
                                            vr = [t[("V", (n - 1) * d + r)], t[("Vf", (n - 1) * d + r)]]
                                        else:
                                            vsl = Vp[g][:, r, h, :]
                                            vr = [t[("Vp", g)]]
                                        sy.op("tensor", lambda e, pvb=pvb, reg=reg, vsl=vsl, pt=pt, col=col, st=(half == 0 and si == 0 and pc == 0): e.matmul(
                                            psb[pvb][:, reg:reg + 128], lhsT=vsl, rhs=pt[:, col:col + 128], start=st, stop=True, skip_group_check=True),
                                            reads=vr + [ptk], writes=[pst[pvb]])
                            r0, n0 = grp[0]
                            if d == 1:
                                dst = acc[:, h, n0 * 128:(n0 + 4) * 128]
                                src = psb[pvb][:, :]
                            else:
                                dst = acc[:, h, n0 * 128 * d:(n0 + 1) * 128 * d].rearrange("p (l r) -> p l r", r=d)[:, :, r0:r0 + 4]
                                src = psb[pvb][:, :].rearrange("p (r l) -> p l r", r=4)
                            if g == 0:
                                sy.op("vector", lambda e, dst=dst, src=src: e.tensor_copy(out=dst, in_=src),
                                      reads=[pst[pvb]], writes=[t[("acc", h)]])
                            else:
                                sy.op("vector", lambda e, dst=dst, src=src: e.tensor_tensor(out=dst, in0=dst, in1=src, op=ALU.add),
                                      reads=[pst[pvb]], writes=[t[("acc", h)]])
                sy.op("gpsimd", lambda e, g=g, d=d: e.tensor_copy(out=Kp[g][:], in_=Kc[:, :, 2048 - 128 * d:2048]),
                      reads=[t[("k", h)] for h in range(4)], writes=[t[("Kp", g)]])
                sy.op("gpsimd", lambda e, g=g, d=d: e.tensor_copy(out=Vp[g][:], in_=Vc[:, 16 - d:16, :, :]),
                      reads=[t[("V", i)] for i in range(16)] + [t[("Vf", i)] for i in range(16)], writes=[t[("Vp", g)]])
            if own:
                for h in range(4):
                    hp, lo = h // 2, (h % 2) * 64
                    for s2 in range(2):
                        sy.op("vector", lambda e, h=h, s2=s2: e.reciprocal(out=xs[s2][0:64, :], in_=acc[64:128, h, s2 * 1024:(s2 + 1) * 1024]),
                              reads=[t[("acc", h)]], writes=[t[("xs", s2)]])
                        sy.op("vector", lambda e, h=h, hp=hp, lo=lo, s2=s2: e.tensor_tensor(
                            out=yaT[lo:lo + 64, hp, sc * 2048 + s2 * 1024:sc * 2048 + (s2 + 1) * 1024],
                            in0=acc[0:64, h, s2 * 1024:(s2 + 1) * 1024], in1=xs[s2][0:64, :], op=ALU.mult),
                            reads=[t[("acc", h)], t[("xs", s2)]], writes=[tYa[(hp, sc, lo, s2)]])
        sy.barrier()
        es.close()

    if not dbg.get("skipA"):
        stage_A()
    else:
        sy.op("gpsimd", lambda e: e.memset(yaT[:], 0.0), writes=[tYa["z"]])

    if "yaT" in dbg:
        o = P.dout("dbg_yaT", [128, 2 * OWN], BF16)
        sy.dma("sync", o[:, :], yaT[:].rearrange("p a b -> p (a b)"), reads=tYa.all(), stream="o")


    ybT = SB(es_y, "ybT", [128, 4, OWN], BF16)
    tYb = TK()

    def stage_B():
        es = contextlib.ExitStack()
        t = TK()
        xin_t = xin.rearrange("(n p) d -> n p d", p=128)
        ps_rot = [0]

        def next_ps(lo=0, hi=8):
            i = ps_rot[0]
            ps_rot[0] = i + 1
            return lo + i % (hi - lo)

        xs = [SB(es, "B_xs0", [128, D], F32)] * 2
        nload = [0]

        ps_hi = [8]

        def emit_xT(tile_u, dst, j, key):
            sI = 0
            nload[0] += 1
            sy.dma("sync", xs[sI][:], xin_t[tile_u, :, :], writes=[t[("xs", sI)]], stream=f"x{sI}")
            for half in range(2):
                b = next_ps(0, ps_hi[0])
                for kk in range(4):
                    kc = half * 4 + kk
                    sy.op("tensor", lambda e, b=b, kk=kk, kc=kc, sI=sI: e.transpose(
                        out=psb[b][:, kk * 128:(kk + 1) * 128], in_=xs[sI][:, kc * 128:(kc + 1) * 128], identity=ident[:]),
                        reads=[t[("xs", sI)], tC["ident"]], writes=[pst[b]])
                src = psb[b][:, :].rearrange("p (k c) -> p k c", k=4)
                o = dst[:, half * 4:half * 4 + 4, j * 128:(j + 1) * 128]
                if half == 0:
                    sy.op("vector", lambda e, o=o, src=src: e.tensor_copy(out=o, in_=src), reads=[pst[b]], writes=[t[(key, j, half)]])
                else:
                    sy.op("scalar", lambda e, o=o, src=src: e.copy(out=o, in_=src), reads=[pst[b]], writes=[t[(key, j, half)]])

        wst = SB(es, "B_wst", [128, 2, 768], F32)

        def load_w(dst, c0, ncol, key):
            for kc2 in range(4):
                sy.dma("gpsimd", wst[:, :, 0:ncol], w_in[kc2 * 256:(kc2 + 1) * 256, c0:c0 + ncol].rearrange("(k p) c -> p k c", p=128),
                       writes=[t["wst"]], stream="w0")
                sy.op("gpsimd", lambda e, kc2=kc2: e.tensor_copy(out=dst[:, kc2 * 2:kc2 * 2 + 2, :], in_=wst[:, :, 0:ncol]),
                      reads=[t["wst"]], writes=[t[key]])

        slcK = SB(es, "B_slcK", [67, 2, 2 * OWN], BF16)
        winK = SB(es, "B_winK", [67, 2, 36 * 128], BF16)
        slcV = SB(es, "B_slcV", [128, 64, 2, 66], BF16)
        winV = SB(es, "B_winV", [128, 36, 2, 66], BF16)
        KcT = SB(es, "B_KcT", [64, 2, 512], BF16)
        Vcm = SB(es, "B_Vcm", [128, 4, 2, 64], BF16)
        for g in range(2):
            sy.dma("gpsimd", slcK[64:67, g, :], cd["kaug"][:, :], writes=[t[("slcKaug", g)]], stream="c")
            sy.dma("gpsimd", winK[64:67, g, :], cd["kaug"][:, 28 * 128:], writes=[t[("winKaug", g)]], stream="c")
        sy.op("gpsimd", lambda e: e.tensor_copy(out=slcV[:, 0:32, :, 64:66], in_=vflag[:, None, None, 0:1].to_broadcast([128, 32, 2, 2])),
              reads=[tC["vflag"]], writes=[t["slcVf"]])
        sy.op("gpsimd", lambda e: e.tensor_copy(out=slcV[:, 32:64, :, 64:66], in_=vflag[:, None, None, 1:2].to_broadcast([128, 32, 2, 2])),
              reads=[tC["vflag"]], writes=[t["slcVf"]])
        sy.op("gpsimd", lambda e: e.tensor_copy(out=winV[:, 0:4, :, 64:66], in_=vflag[:, None, None, 0:1].to_broadcast([128, 4, 2, 2])),
              reads=[tC["vflag"]], writes=[t["winVf"]])
        sy.op("gpsimd", lambda e: e.tensor_copy(out=winV[:, 4:36, :, 64:66], in_=vflag[:, None, None, 1:2].to_broadcast([128, 32, 2, 2])),
              reads=[tC["vflag"]], writes=[t["winVf"]])

        es1 = contextlib.ExitStack()
        raw = SB(es1, "B_raw", [128, 2, 2 * OWN], BF16)
        es1b = contextlib.ExitStack()
        wB = SB(es1b, "B_wB", [128, 8, 768], BF16)
        xTc = [SB(es1b, f"B_xTc{i}", [128, 8, 512], BF16) for i in range(2)]
        load_w(wB, C_BKV, 768, "wB")
        for ch in range(16):
            xb_ = xTc[ch % 2]
            xk = ("xTc", ch % 2)
            for j in range(4):
                emit_xT(ch * 4 + j, xb_, j, xk)
            xr = [t[(xk, j, hh)] for j in range(4) for hh in range(2)]
            for (ii, dst, off, key) in () if dbg.get("noFM") else ((0, raw, 0, "rawK"), (1, raw, 0, "rawV"), (2, slcK, 0, "slcK"), (4, winK, -28 * 128, "winK")):
                if ii == 4 and ch < 7:
                    continue
                for g in range(2):
                    b = next_ps()
                    c0 = ii * 128 + g * 64
                    for kc in range(8):
                        sy.op("tensor", lambda e, b=b, kc=kc, c0=c0, xb_=xb_: e.matmul(
                            psb[b][0:64, :], lhsT=wB[:, kc, c0:c0 + 64], rhs=xb_[:, kc, :], start=(kc == 0), stop=(kc == 7)),
                            reads=[t["wB"]] + xr, writes=[pst[b]])
                    pb = 64 if ii == 1 else 0
                    o = dst[pb:pb + 64, g, ch * 512 + off:ch * 512 + off + 512]
                    if g == 0:
                        sy.op("vector", lambda e, o=o, b=b: e.tensor_copy(out=o, in_=psb[b][0:64, :]), reads=[pst[b]], writes=[t[(key, g, ch)]])
                    else:
                        sy.op("scalar", lambda e, o=o, b=b: e.copy(out=o, in_=psb[b][0:64, :]), reads=[pst[b]], writes=[t[(key, g, ch)]])
            for j in range(0 if dbg.get("noTM") else 4):
                tu = ch * 4 + j
                b = next_ps()
                for kc in range(8):
                    sy.op("tensor", lambda e, b=b, kc=kc, j=j, xb_=xb_: e.matmul(
                        psb[b][:, 0:384], lhsT=xb_[:, kc, j * 128:(j + 1) * 128], rhs=wB[:, kc, 384:768], start=(kc == 0), stop=(kc == 7)),
                        reads=[t["wB"]] + xr, writes=[pst[b]])
                sy.op("vector", lambda e, b=b, tu=tu: e.tensor_copy(
                    out=slcV[:, tu, :, 0:64], in_=psb[b][:, 0:128].rearrange("p (g e) -> p g e", g=2)),
                    reads=[pst[b]], writes=[t[("slcV", tu)]])
                if tu >= 28:
                    sy.op("scalar", lambda e, b=b, tu=tu: e.copy(
                        out=winV[:, tu - 28, :, 0:64], in_=psb[b][:, 256:384].rearrange("p (g e) -> p g e", g=2)),
                        reads=[pst[b]], writes=[t[("winV", tu - 28)]])
        sy.barrier()
        es1b.close()
        if dbg.get("stopB1"):
            if "B1" in dbg and not dbg.get("noOut"):
                o3 = P.dout("dbg_slcK", [67, 2 * 2 * OWN], BF16)
                sy.dma("sync", o3[:, :], slcK[:].rearrange("p a b -> p (a b)"), stream="o")
                o4 = P.dout("dbg_winV", [128, 36 * 2 * 66], BF16)
                sy.dma("sync", o4[:, :], winV[:].rearrange("p a b c -> p (a b c)"), stream="o")
                o5 = P.dout("dbg_raw", [128, 2 * 2 * OWN], BF16)
                sy.dma("sync", o5[:, :], raw[:].rearrange("p a b -> p (a b)"), stream="o")
            sy.barrier()
            es1.close()
            es.close()
            return
        es2 = contextlib.ExitStack()
        w1r = SB(es2, "B_w1r", [128, 32, 256], BF16)
        w1st = SB(es2, "B_w1st", [128, 4, 256], F32)
        posT = SB(es2, "B_posT", [128, 32], F32)
        posTb = SB(es2, "B_posTb", [128, 32], BF16)
        w2s = SB(es2, "B_w2s", [128, 2, 64], F32)
        w2b = SB(es2, "B_w2b", [128, 2, 64], BF16)
        hb = SB(es2, "B_hb", [128, 2], F32)
        h1 = SB(es2, "B_h1", [128, 512], F32)
        h1x = SB(es2, "B_h1x", [128, 512], F32)
        h1T = SB(es2, "B_h1T", [128, 2, 512], BF16)
        cvalid = SB(es2, "B_cvalid", [128, 4], F32)
        sy.dma("sync", cvalid[:], cd["cvalid"][:, :], writes=[t["cvalid"]], stream="c")
        sy.op("gpsimd", lambda e: e.memset(h1T[:], 0.0), writes=[t["h1T"]])
        for kv, PB in (("k", 0), ("v", 64)):
            for pp in range(8):
                sy.dma("sync", w1st[PB:PB + 64].rearrange("e p h -> e (p h)"), cd[f"w1r_{kv}"][:, pp * 1024:(pp + 1) * 1024], writes=[t["w1st"]], stream="c2")
                sy.op("gpsimd", lambda e, pp=pp, PB=PB: e.tensor_copy(out=w1r[PB:PB + 64, pp * 4:pp * 4 + 4, :], in_=w1st[PB:PB + 64]), reads=[t["w1st"]], writes=[t["w1r"]])
            sy.dma("sync", posT[PB:PB + 64, :], cd[f"posT_{kv}"][:, :], writes=[t["posT"]], stream="c2")
            sy.op("gpsimd", lambda e, PB=PB: e.tensor_copy(out=posTb[PB:PB + 64, :], in_=posT[PB:PB + 64, :]), reads=[t["posT"]], writes=[t["posTb"]])
            sy.dma("sync", w2s[:], cd[f"w2_{kv}"].rearrange("(c p) e -> p c e", p=128), writes=[t["w2s"]], stream="c2")
            sy.op("gpsimd", lambda e: e.tensor_copy(out=w2b[:], in_=w2s[:]), reads=[t["w2s"]], writes=[t["w2b"]])
            b = next_ps()
            for hc in range(2):
                for p in range(32):
                    sy.op("tensor", lambda e, b=b, hc=hc, p=p, PB=PB: e.matmul(
                        psb[b][:, hc:hc + 1], lhsT=w1r[PB:PB + 64, p, hc * 128:(hc + 1) * 128], rhs=posTb[PB:PB + 64, p:p + 1],
                        start=(p == 0 and hc == 0), stop=(p == 31), skip_group_check=True),
                        reads=[t["w1r"], t["posTb"]], writes=[pst[b]])
            sy.op("vector", lambda e, b=b: e.tensor_copy(out=hb[:], in_=psb[b][:, 0:2]), reads=[pst[b]], writes=[t["hb"]])
            rawr = [t[("rawK" if kv == "k" else "rawV", g, ch)] for g in range(2) for ch in range(16)]
            for g in range(2):
                for hc in range(2):
                    b = next_ps()
                    for p in range(32):
                        sy.op("tensor", lambda e, b=b, hc=hc, p=p, g=g, PB=PB: e.matmul(
                            psb[b][:, 0:511], lhsT=w1r[PB:PB + 64, p, hc * 128:(hc + 1) * 128], rhs=raw[PB:PB + 64, g, slice(p, p + 16 * 510 + 1, 16)],
                            start=(p == 0), stop=(p == 31)),
                            reads=[t["w1r"]] + rawr, writes=[pst[b]])
                    sy.op("vector", lambda e, b=b, hc=hc: e.tensor_scalar(out=h1[:, 0:511], in0=psb[b][:, 0:511], scalar1=hb[:, hc:hc + 1], scalar2=None, op0=ALU.add),
                          reads=[pst[b], t["hb"]], writes=[t["h1"]])
                    sy.op("vector", lambda e: e.tensor_tensor(out=h1x[:, 0:511], in0=h1[:, 0:511], in1=h1[:, 0:511], op=ALU.mult),
                          reads=[t["h1"]], writes=[t["h1x"]])
                    sy.op("vector", lambda e: e.tensor_scalar(out=h1x[:, 0:511], in0=h1x[:, 0:511], scalar1=0.044715, scalar2=1.0, op0=ALU.mult, op1=ALU.add),
                          reads=[t["h1x"]], writes=[t["h1x"]])
                    sy.op("vector", lambda e: e.tensor_tensor(out=h1x[:, 0:511], in0=h1x[:, 0:511], in1=h1[:, 0:511], op=ALU.mult),
                          reads=[t["h1"], t["h1x"]], writes=[t["h1x"]])
                    sy.op("scalar", lambda e: e.activation(out=h1x[:, 0:511], in_=h1x[:, 0:511], func=AF.Sigmoid, scale=1.5957691216057308),
                          reads=[t["h1x"]], writes=[t["h1x"]])
                    sy.op("vector", lambda e, hc=hc: e.tensor_tensor(out=h1T[:, hc, 0:511], in0=h1x[:, 0:511], in1=h1[:, 0:511], op=ALU.mult),
                          reads=[t["h1"], t["h1x"]], writes=[t["h1T"]])
                if kv == "k":
                    b = next_ps()
                    for hc in range(2):
                        sy.op("tensor", lambda e, b=b, hc=hc: e.matmul(psb[b][0:64, :], lhsT=w2b[:, hc, :], rhs=h1T[:, hc, :], start=(hc == 0), stop=(hc == 1)),
                              reads=[t["w2b"], t["h1T"]], writes=[pst[b]])
                    sy.op("vector", lambda e, b=b, g=g: e.tensor_copy(out=KcT[:, g, :], in_=psb[b][0:64, :]), reads=[pst[b]], writes=[t[("KcT", g)]])
                else:
                    b = next_ps()
                    for ct in range(4):
                        for hc in range(2):
                            sy.op("tensor", lambda e, b=b, hc=hc, ct=ct: e.matmul(
                                psb[b][:, ct * 64:(ct + 1) * 64], lhsT=h1T[:, hc, ct * 128:(ct + 1) * 128], rhs=w2b[:, hc, :],
                                start=(hc == 0 and ct == 0), stop=(hc == 1), skip_group_check=True),
                                reads=[t["w2b"], t["h1T"]], writes=[pst[b]])
                    for ct in range(4):
                        sy.op("vector", lambda e, b=b, g=g, ct=ct: e.tensor_scalar(
                            out=Vcm[:, ct, g, :], in0=psb[b][:, ct * 64:(ct + 1) * 64], scalar1=cvalid[:, ct:ct + 1], scalar2=None, op0=ALU.mult),
                            reads=[pst[b], t["cvalid"]], writes=[t[("Vcm", g)]])
        sy.barrier()
        es2.close()
        es1.close()
        if "B2" in dbg:
            o1 = P.dout("dbg_KcT", [64, 1024], BF16)
            sy.dma("sync", o1[:, :], KcT[:].rearrange("p a b -> p (a b)"), reads=t.all(), stream="o")
            o2 = P.dout("dbg_Vcm", [128, 512], BF16)
            sy.dma("sync", o2[:, :], Vcm[:].rearrange("p a b c -> p (a b c)"), reads=t.all(), stream="o")
            o3 = P.dout("dbg_slcK", [67, 2 * 2 * OWN], BF16)
            sy.dma("sync", o3[:, :], slcK[:].rearrange("p a b -> p (a b)"), reads=t.all(), stream="o")
            o4 = P.dout("dbg_winV", [128, 36 * 2 * 66], BF16)
            sy.dma("sync", o4[:, :], winV[:].rearrange("p a b c -> p (a b c)"), reads=t.all(), stream="o")
        if dbg.get("stopB2"):
            sy.barrier()
            es.close()
            return

        ps_hi[0] = 6
        wq = SB(es, "B_wq", [128, 8, 512], BF16)
        wg = SB(es, "B_wg", [128, 8, 24], BF16)
        load_w(wq, C_BQ, 512, "wq")
        load_w(wg, C_BG, 24, "wg")
        cs = {}
        for nm, shp, dt in (("mcb", [128, 17, 128], BF16), ("validrep", [128, 4, 128], BF16), ("ovl", [128, 4, 128], BF16),
                            ("wd", [128, 190], F32), ("fbvec", [128, 128], F32), ("indbig", [128, 64, 128], BF16),
                            ("tri_le", [128, 128], BF16), ("tri_gt", [128, 128], BF16)):
            cs[nm] = SB(es, "Bc_" + nm, shp, dt)
            dst = cs[nm][:]
            if len(shp) == 3:
                dst = dst.rearrange("p a b -> p (a b)")
            sy.dma("sync", dst, cd[nm][:, :], writes=[t["c_" + nm]], stream="c")
        xT1 = [SB(es, f"B_xT1{i}", [128, 8, 128], BF16) for i in range(2)]
        qTa = [SB(es, f"B_qTa{i}", [67, 8, 128], BF16) for i in range(2)]
        gates = [SB(es, f"B_gates{i}", [128, 24], F32) for i in range(2)]
        PcT = SB(es, "B_PcT", [128, 4, 512], BF16)
        Pn = SB(es, "B_Pn", [128, 4, 512], BF16)
        rden = SB(es, "B_rden", [128, 512], F32)
        score = SB(es, "B_score", [128, 128], F32)
        work = SB(es, "B_work", [128, 128], F32)
        pen = SB(es, "B_pen", [128, 128], F32)
        pen2 = SB(es, "B_pen2", [128, 128], F32)
        m8a = SB(es, "B_m8a", [128, 8], F32)
        m8b = SB(es, "B_m8b", [128, 8], F32)
        penT = [SB(es, f"B_penT{g}", [128, 4, 128], BF16) for g in range(2)]
        PT = [SB(es, f"B_PT{i}", [128, 512], BF16) for i in range(3)]
        oc_sb = SB(es, "B_oc", [128, 2, 4, 64], F32)
        os_sb = SB(es, "B_os", [128, 2, 4, 66], F32)
        ow_sb = SB(es, "B_ow", [128, 2, 4, 66], F32)
        rs = SB(es, "B_rs", [128, 2, 4, 2], F32)
        yb_tm = SB(es, "B_ybtm", [128, 512], F32)
        ytmp = SB(es, "B_ytmp", [128, 64], F32)
        OSB, OWB = 6, 7
        qaug3 = cd["qaug"].rearrange("r (h n) -> r h n", h=8)
        ptc = [0]

        def attn_tiles(i, g, kts, Ksrc, koff, Vsrc, accb, with_pen, qa, qk):
            first = True
            for kt in kts:
                sb = next_ps(0, 6)
                kl = kt - koff
                sy.op("tensor", lambda e, sb=sb, kl=kl: e.matmul(
                    psb[sb][:, :], lhsT=Ksrc[0:67, g, kl * 128:(kl + 1) * 128], rhs=qa[0:67, 4 * g:4 * g + 4, :], start=True, stop=False),
                    reads=[qk[0], qk[1]], writes=[pst[sb]])
                adds = []
                if with_pen:
                    adds.append((cs["indbig"][:, kt, :], penT[g][:], [t["c_indbig"], t[("penT", g)]]))
                if kt == 32 + i:
                    adds.append((identb[:], cs["tri_le"][:, None, :].to_broadcast([128, 4, 128]), [tC["identb"], t["c_tri_le"]]))
                if (not with_pen) and kt == 28 + i:
                    adds.append((identb[:], cs["tri_gt"][:, None, :].to_broadcast([128, 4, 128]), [tC["identb"], t["c_tri_gt"]]))
                for (l_, r_, rd) in adds:
                    sy.op("tensor", lambda e, sb=sb, l_=l_, r_=r_: e.matmul(psb[sb][:, :], lhsT=l_, rhs=r_, start=False, stop=True),
                          reads=rd, writes=[pst[sb]])
                pi = ptc[0] % 3
                ptc[0] += 1
                sy.op("scalar", lambda e, sb=sb, pi=pi: e.activation(out=PT[pi][:], in_=psb[sb][:, :], func=AF.Exp, scale=0.125),
                      reads=[pst[sb]], writes=[t[("PT", pi)]])
                for r in range(4):
                    sy.op("tensor", lambda e, r=r, pi=pi, kl=kl, st=(first and r == 0): e.matmul(
                        psb[accb][:, r * 66:(r + 1) * 66], lhsT=PT[pi][:, r * 128:(r + 1) * 128], rhs=Vsrc[:, kl, g, :],
                        start=st, stop=True, skip_group_check=True),
                        reads=[t[("PT", pi)]], writes=[pst[accb]])
                first = False

        for i in range(NT):
            xb_ = xT1[i % 2]
            xk = ("xT1", i % 2)
            emit_xT(32 + i, xb_, 0, xk)
            xr = [t[(xk, 0, 0)], t[(xk, 0, 1)]]
            qa = qTa[i % 2]
            qk = (t[("qTa", i % 2)], t[("qTaug", i % 2)])
            sy.dma("gpsimd", qa[64:67, :, :], qaug3[:, :, i * 128:(i + 1) * 128], writes=[qk[1]], stream="qa")
            for g in range(2):
                b = next_ps(0, 6)
                for r in range(4):
                    hh = 4 * g + r
                    for kc in range(8):
                        sy.op("tensor", lambda e, b=b, r=r, hh=hh, kc=kc, xb_=xb_: e.matmul(
                            psb[b][0:64, r * 128:(r + 1) * 128], lhsT=wq[:, kc, hh * 64:(hh + 1) * 64], rhs=xb_[:, kc, :],
                            start=(kc == 0 and r == 0), stop=(kc == 7), skip_group_check=True),
                            reads=[t["wq"]] + xr, writes=[pst[b]])
                sy.op("vector", lambda e, b=b, g=g, qa=qa: e.tensor_copy(
                    out=qa[0:64, 4 * g:4 * g + 4, :], in_=psb[b][0:64, :].rearrange("p (r n) -> p r n", r=4)),
                    reads=[pst[b]], writes=[qk[0]])
            b = next_ps(0, 6)
            for kc in range(8):
                sy.op("tensor", lambda e, b=b, kc=kc, xb_=xb_: e.matmul(psb[b][:, 0:24], lhsT=xb_[:, kc, :], rhs=wg[:, kc, :], start=(kc == 0), stop=(kc == 7)),
                      reads=[t["wg"]] + xr, writes=[pst[b]])
            gt = gates[i % 2]
            gk = t[("gates", i % 2)]
            sy.op("scalar", lambda e, b=b, gt=gt: e.activation(out=gt[:], in_=psb[b][:, 0:24], func=AF.Sigmoid), reads=[pst[b]], writes=[gk])
            for g in range(2):
                ctmax = (262 + 8 * i) // 128
                ncts = ctmax + 1
                for ct in range(ncts):
                    sb = next_ps(0, 6)
                    off = 254 + 8 * i - 128 * ct
                    need_mask = off < 127
                    sy.op("tensor", lambda e, sb=sb, ct=ct, nm=need_mask: e.matmul(
                        psb[sb][:, :], lhsT=KcT[:, g, ct * 128:(ct + 1) * 128], rhs=qa[0:64, 4 * g:4 * g + 4, :], start=True, stop=(not nm)),
                        reads=[t[("KcT", g)], qk[0]], writes=[pst[sb]])
                    if need_mask:
                        idx = (off + 2) // 8
                        assert 0 <= idx < 17, (i, ct, off)
                        sy.op("tensor", lambda e, sb=sb, idx=idx: e.matmul(
                            psb[sb][:, :], lhsT=identb[:], rhs=cs["mcb"][:, idx:idx + 1, :].to_broadcast([128, 4, 128]), start=False, stop=True),
                            reads=[tC["identb"], t["c_mcb"]], writes=[pst[sb]])
                    sy.op("scalar", lambda e, sb=sb, ct=ct: e.activation(out=PcT[:, ct, :], in_=psb[sb][:, :], func=AF.Exp, scale=0.125),
                          reads=[pst[sb]], writes=[t[("PcT", ct)]])
                db = next_ps(0, 6)
                for ct in range(ncts):
                    sy.op("tensor", lambda e, db=db, ct=ct: e.matmul(psb[db][:, :], lhsT=cs["validrep"][:, ct, :], rhs=PcT[:, ct, :], start=(ct == 0), stop=(ct == ncts - 1)),
                          reads=[t["c_validrep"], t[("PcT", ct)]], writes=[pst[db]])
                sy.op("vector", lambda e, db=db: e.tensor_scalar(out=rden[:], in0=psb[db][:, :], scalar1=1e-30, scalar2=None, op0=ALU.max),
                      reads=[pst[db]], writes=[t["rden"]])
                sy.op("vector", lambda e: e.reciprocal(out=rden[:], in_=rden[:]), reads=[t["rden"]], writes=[t["rden"]])
                for ct in range(ncts):
                    sy.op("vector" if ct % 2 == 0 else "gpsimd", lambda e, ct=ct: e.tensor_tensor(out=Pn[:, ct, :], in0=PcT[:, ct, :], in1=rden[:], op=ALU.mult),
                          reads=[t[("PcT", ct)], t["rden"]], writes=[t[("Pn", ct)]])
                ob = next_ps(0, 6)
                firstm = True
                for r in range(4):
                    for ct in range(ncts):
                        sy.op("tensor", lambda e, ob=ob, r=r, ct=ct, st=firstm: e.matmul(
                            psb[ob][:, r * 64:(r + 1) * 64], lhsT=Pn[:, ct, r * 128:(r + 1) * 128], rhs=Vcm[:, ct, g, :],
                            start=st, stop=True, skip_group_check=True),
                            reads=[t[("Pn", ct)], t[("Vcm", g)]], writes=[pst[ob]])
                        firstm = False
                for r in range(4):
                    for ct in range(ncts):
                        sy.op("tensor", lambda e, ob=ob, r=r, ct=ct: e.matmul(
                            psb[ob][:, 256:384], lhsT=Pn[:, ct, r * 128:(r + 1) * 128], rhs=cs["ovl"][:, ct, :],
                            start=False, stop=True, skip_group_check=True),
                            reads=[t[("Pn", ct)], t["c_ovl"]], writes=[pst[ob]])
                sy.op("scalar", lambda e, ob=ob, g=g: e.copy(out=oc_sb[:, g, :, :], in_=psb[ob][:, 0:256].rearrange("p (r e) -> p r e", r=4)),
                      reads=[pst[ob]], writes=[t[("oc", g)]])
                sy.op("vector", lambda e, ob=ob: e.tensor_tensor(out=score[:], in0=psb[ob][:, 256:384], in1=cs["wd"][:, 62 - 2 * i:190 - 2 * i], op=ALU.add),
                      reads=[pst[ob], t["c_wd"]], writes=[t["score"]])
                sy.op("vector", lambda e: e.tensor_tensor(out=score[:], in0=score[:], in1=cs["fbvec"][:], op=ALU.add),
                      reads=[t["c_fbvec"]], writes=[t["score"]])
                sy.op("vector", lambda e: e.max(out=m8a[:], in_=score[:]), reads=[t["score"]], writes=[t["m8a"]])
                sy.op("vector", lambda e: e.match_replace(out=work[:], in_to_replace=m8a[:], in_values=score[:], imm_value=-3.0e38),
                      reads=[t["score"], t["m8a"]], writes=[t["work"]])
                sy.op("vector", lambda e: e.max(out=m8b[:], in_=work[:]), reads=[t["work"]], writes=[t["m8b"]])
                sy.op("vector", lambda e: e.tensor_scalar(out=pen[:], in0=score[:], scalar1=m8b[:, 7:8], scalar2=NEG, op0=ALU.is_lt, op1=ALU.mult),
                      reads=[t["score"], t["m8b"]], writes=[t["pen"]])
                sy.op("vector", lambda e: e.tensor_scalar(out=pen2[:], in0=score[:], scalar1=-5.0e8, scalar2=NEG, op0=ALU.is_lt, op1=ALU.mult),
                      reads=[t["score"]], writes=[t["pen2"]])
                sy.op("vector", lambda e: e.tensor_tensor(out=pen[:], in0=pen[:], in1=pen2[:], op=ALU.min),
                      reads=[t["pen2"]], writes=[t["pen"]])
                tb = next_ps(0, 6)
                sy.op("tensor", lambda e, tb=tb: e.transpose(out=psb[tb][:, 0:128], in_=pen[:], identity=ident[:]),
                      reads=[t["pen"], tC["ident"]], writes=[pst[tb]])
                sy.op("vector", lambda e, tb=tb, g=g: e.tensor_copy(out=penT[g][:], in_=psb[tb][:, None, 0:128].to_broadcast([128, 4, 128])),
                      reads=[pst[tb]], writes=[t[("penT", g)]])
                attn_tiles(i, g, range(28 + i, 33 + i), winK, 28, winV, OWB, False, qa, qk)
                sy.op("vector", lambda e, g=g: e.tensor_copy(out=ow_sb[:, g, :, :], in_=psb[OWB][:, 0:264].rearrange("p (r e) -> p r e", r=4)),
                      reads=[pst[OWB]], writes=[t[("ow", g)]])
                attn_tiles(i, g, range(0, 33 + i), slcK, 0, slcV, OSB, True, qa, qk)
                sy.op("vector", lambda e, g=g: e.tensor_copy(out=os_sb[:, g, :, :], in_=psb[OSB][:, 0:264].rearrange("p (r e) -> p r e", r=4)),
                      reads=[pst[OSB]], writes=[t[("os", g)]])
            gt3 = gt[:].rearrange("p (g r b) -> p g r b", g=2, r=4)
            sy.op("vector", lambda e: e.reciprocal(out=rs[:, :, :, 0:1], in_=os_sb[:, :, :, 64:65]), reads=[t[("os", 0)], t[("os", 1)]], writes=[t["rs"]])
            sy.op("vector", lambda e: e.reciprocal(out=rs[:, :, :, 1:2], in_=ow_sb[:, :, :, 64:65]), reads=[t[("ow", 0)], t[("ow", 1)]], writes=[t["rs"]])
            sy.op("vector", lambda e, gt3=gt3: e.tensor_tensor(out=rs[:], in0=rs[:], in1=gt3[:, :, :, 1:3], op=ALU.mult), reads=[gk], writes=[t["rs"]])
            for g in range(2):
                for r in range(4):
                    col = (g * 4 + r) * 64
                    gc = gt[:, g * 12 + r * 3:g * 12 + r * 3 + 1]
                    sy.op("vector", lambda e, g=g, r=r, gc=gc: e.tensor_scalar(out=ytmp[:], in0=oc_sb[:, g, r, :], scalar1=gc, scalar2=None, op0=ALU.mult),
                          reads=[t[("oc", g)], gk], writes=[t["ytmp"]])
                    sy.op("vector", lambda e, g=g, r=r: e.scalar_tensor_tensor(out=ytmp[:], in0=os_sb[:, g, r, 0:64], scalar=rs[:, g, r, 0:1], in1=ytmp[:], op0=ALU.mult, op1=ALU.add),
                          reads=[t[("os", g)], t["rs"]], writes=[t["ytmp"]])
                    sy.op("vector", lambda e, g=g, r=r, col=col: e.scalar_tensor_tensor(out=yb_tm[:, col:col + 64], in0=ow_sb[:, g, r, 0:64], scalar=rs[:, g, r, 1:2], in1=ytmp[:], op0=ALU.mult, op1=ALU.add),
                          reads=[t[("ow", g)], t["rs"], t["ytmp"]], writes=[t["ybtm"]])
            tb = next_ps(0, 6)
            for c4 in range(4):
                sy.op("tensor", lambda e, tb=tb, c4=c4: e.transpose(out=psb[tb][:, c4 * 128:(c4 + 1) * 128], in_=yb_tm[:, c4 * 128:(c4 + 1) * 128], identity=ident[:]),
                      reads=[t["ybtm"], tC["ident"]], writes=[pst[tb]])
            sy.op("scalar", lambda e, tb=tb, i=i: e.copy(out=ybT[:, :, i * 128:(i + 1) * 128], in_=psb[tb][:, :].rearrange("p (c n) -> p c n", c=4)),
                  reads=[pst[tb]], writes=[tYb[i]])
        sy.barrier()
        es.close()

    sy.new_epoch()
    if not dbg.get("skipB"):
        stage_B()
    else:
        for i in range(NT):
            sy.op("gpsimd", lambda e, i=i: e.memset(ybT[:, :, i * 128:(i + 1) * 128], 0.0), writes=[tYb[i]])

    if "ybT" in dbg:
        o = P.dout("dbg_ybT", [128, 4 * OWN], BF16)
        sy.dma("sync", o[:, :], ybT[:].rearrange("p a b -> p (a b)"), reads=tYb.all(), stream="o")

    h1T_d = P.dscratch("h1T_scratch", [NT, 128, 8 * 128], BF16)
    h1_t = h1_d.rearrange("(n p) d -> n p d", p=128)

    def layer_norm(t, z, zk, lnrep, dst, dstk, tmp_stats, tmp_mv):
        for hh in range(2):
            sy.op("vector", lambda e, hh=hh: e.bn_stats(out=tmp_stats[:, hh * 6:(hh + 1) * 6], in_=z[:, hh * 512:(hh + 1) * 512]),
                  reads=[zk], writes=[t["lnst"]])
        sy.op("vector", lambda e: e.bn_aggr(out=tmp_mv[:, 0:2], in_=tmp_stats[:, 0:12]), reads=[t["lnst"]], writes=[t["lnmv"]])
        sy.op("vector", lambda e: e.tensor_scalar(out=tmp_mv[:, 2:3], in0=tmp_mv[:, 1:2], scalar1=LN_EPS, scalar2=None, op0=ALU.add),
              reads=[t["lnmv"]], writes=[t["lnmv"]])
        sy.op("scalar", lambda e: e.activation(out=tmp_mv[:, 2:3], in_=tmp_mv[:, 2:3], func=AF.Sqrt), reads=[t["lnmv"]], writes=[t["lnmv"]])
        sy.op("vector", lambda e: e.reciprocal(out=tmp_mv[:, 3:4], in_=tmp_mv[:, 2:3]), reads=[t["lnmv"]], writes=[t["lnmv"]])
        sy.op("vector", lambda e: e.tensor_scalar(out=dst[:], in0=z[:], scalar1=tmp_mv[:, 0:1], scalar2=tmp_mv[:, 3:4], op0=ALU.subtract, op1=ALU.mult),
              reads=[zk, t["lnmv"]], writes=[dstk])
        sy.op("gpsimd", lambda e: e.tensor_tensor(out=dst[:], in0=dst[:], in1=lnrep[:, 0, :], op=ALU.mult), reads=[t["ln"]], writes=[dstk])
        sy.op("gpsimd", lambda e: e.tensor_tensor(out=dst[:], in0=dst[:], in1=lnrep[:, 1, :], op=ALU.add), reads=[t["ln"]], writes=[dstk])

    def stage_C():
        es = contextlib.ExitStack()
        t = TK()
        xin_t = xin.rearrange("(n p) d -> n p d", p=128)
        ps_rot = [0]

        def next_ps():
            i = ps_rot[0]
            ps_rot[0] = i + 1
            return i % 8

        lnrep = SB(es, "C_lnrep", [128, 2, D], F32)
        sy.dma("sync", lnrep[:].rearrange("p a d -> p (a d)"), cd["lnrep"][:, 0:2 * D], writes=[t["ln"]], stream="c")
        wst = SB(es, "C_wst", [128, 2, 1024], F32)
        wM = SB(es, "C_wM", [128, 8, 2048], BF16)
        wAB = SB(es, "C_wAB", [128, 6, D], BF16)
        wO = SB(es, "C_wO", [128, 8, D], BF16)
        wR = SB(es, "C_wR", [128, 8, 36], F32)
        brep = SB(es, "C_brep", [128, 36], F32)
        xs = [SB(es, f"C_xs{i}", [128, D], F32) for i in range(2)]
        xT1 = SB(es, "C_xT1", [128, 8, 128], BF16)
        gT = SB(es, "C_gT", [128, 16, 128], F32)
        mT = SB(es, "C_mT", [128, 8, 128], BF16)
        tmp1 = SB(es, "C_tmp1", [128, 128], F32)
        tmp2 = SB(es, "C_tmp2", [128, 128], F32)
        z = SB(es, "C_z", [128, D], F32)
        h1 = [SB(es, f"C_h1{i}", [128, D], F32) for i in range(2)]
        h1T32 = SB(es, "C_h1T32", [128, 8, 128], F32)
        h1Tb = [SB(es, f"C_h1Tb{i}", [128, 8, 128], BF16) for i in range(2)]
        st6 = SB(es, "C_st6", [128, 12], F32)
        mv = SB(es, "C_mv", [128, 4], F32)
        lg = SB(es, "C_lg", [128, 36], F32)
        rt = SB(es, "C_rt", [128, 64], F32)
        m8 = SB(es, "C_m8", [128, 8], F32)

        def load_wgen(dst, src2d, rows, ncol, key):
            for r2 in range(rows // 256):
                sy.dma("gpsimd", wst[:, :, 0:ncol], src2d[r2 * 256:(r2 + 1) * 256, :].rearrange("(k p) c -> p k c", p=128),
                       writes=[t["wst"]], stream="w0")
                sy.op("gpsimd", lambda e, r2=r2: e.tensor_copy(out=dst[:, r2 * 2:r2 * 2 + 2, :], in_=wst[:, :, 0:ncol]),
                      reads=[t["wst"]], writes=[t[key]])

        load_wgen(wM[:, :, 0:1024], w_in[:, C_MG:C_MG + 1024], D, 1024, "wM")
        load_wgen(wM[:, :, 1024:2048], w_in[:, C_MG + 1024:C_MG + 2048], D, 1024, "wM")
        load_wgen(wAB[:, 0:2, :], cd["w_ba"], 256, D, "wAB")
        load_wgen(wAB[:, 2:6, :], cd["w_bb"], 512, D, "wAB")
        load_wgen(wO, cd["w_out"], D, D, "wO")
        sy.dma("sync", wR[:], cd["w_router"].rearrange("(k p) c -> p k c", p=128), writes=[t["wR"]], stream="c")
        sy.dma("sync", brep[:], cd["b_router"][:, :], writes=[t["brep"]], stream="c")

        for i in range(NT):
            sI = i % 2
            sy.dma("sync", xs[sI][:], xin_t[32 + i, :, :], writes=[t[("xs", sI)]], stream=f"x{sI}")
            for half in range(2):
                b = next_ps()
                for kk in range(4):
                    kc = half * 4 + kk
                    sy.op("tensor", lambda e, b=b, kk=kk, kc=kc, sI=sI: e.transpose(
                        out=psb[b][:, kk * 128:(kk + 1) * 128], in_=xs[sI][:, kc * 128:(kc + 1) * 128], identity=ident[:]),
                        reads=[t[("xs", sI)], tC["ident"]], writes=[pst[b]])
                sy.op("vector" if half == 0 else "scalar",
                      (lambda e, b=b, half=half: e.tensor_copy(out=xT1[:, half * 4:half * 4 + 4, :], in_=psb[b][:, :].rearrange("p (k c) -> p k c", k=4))) if half == 0 else
                      (lambda e, b=b, half=half: e.copy(out=xT1[:, half * 4:half * 4 + 4, :], in_=psb[b][:, :].rearrange("p (k c) -> p k c", k=4))),
                      reads=[pst[b]], writes=[t[("xT1", half)]])
            xr = [t[("xT1", 0)], t[("xT1", 1)]]
            tok = slice(i * 128, (i + 1) * 128)
            for c4 in range(4):
                b = next_ps()
                for cc in range(4):
                    ct = c4 * 4 + cc
                    for kc in range(8):
                        sy.op("tensor", lambda e, b=b, cc=cc, ct=ct, kc=kc: e.matmul(
                            psb[b][:, cc * 128:(cc + 1) * 128], lhsT=wM[:, kc, ct * 128:(ct + 1) * 128], rhs=xT1[:, kc, :],
                            start=(kc == 0 and cc == 0), stop=(kc == 7), skip_group_check=True),
                            reads=[t["wM"]] + xr, writes=[pst[b]])
                sy.op("scalar", lambda e, b=b, c4=c4: e.activation(out=gT[:, c4 * 4:c4 * 4 + 4, :], in_=psb[b][:, :].rearrange("p (c n) -> p c n", c=4), func=AF.Sigmoid),
                      reads=[pst[b]], writes=[t[("gT", c4)]])
            for c in range(8):
                b = next_ps()
                for k2 in range(2):
                    sy.op("tensor", lambda e, b=b, c=c, k2=k2: e.matmul(
                        psb[b][:, 0:128], lhsT=wAB[:, k2, c * 128:(c + 1) * 128], rhs=yaT[:, k2, tok], start=(k2 == 0), stop=(k2 == 1), skip_group_check=True),
                        reads=[t["wAB"]] + tYa.all(), writes=[pst[b]])
                for k4 in range(4):
                    sy.op("tensor", lambda e, b=b, c=c, k4=k4: e.matmul(
                        psb[b][:, 128:256], lhsT=wAB[:, 2 + k4, c * 128:(c + 1) * 128], rhs=ybT[:, k4, tok], start=False, stop=(k4 == 3), skip_group_check=True),
                        reads=[t["wAB"], tYb[i]], writes=[pst[b]])
                sy.op("vector", lambda e, b=b, c=c: e.tensor_tensor(out=tmp1[:], in0=psb[b][:, 0:128], in1=gT[:, c, :], op=ALU.mult),
                      reads=[pst[b], t[("gT", c // 4)]], writes=[t["tmp1"]])
                sy.op("vector", lambda e, b=b, c=c: e.tensor_tensor(out=tmp2[:], in0=psb[b][:, 128:256], in1=gT[:, 8 + c, :], op=ALU.mult),
                      reads=[pst[b], t[("gT", 2 + c // 4)]], writes=[t["tmp2"]])
                sy.op("gpsimd", lambda e, c=c: e.tensor_tensor(out=mT[:, c, :], in0=tmp1[:], in1=tmp2[:], op=ALU.add),
                      reads=[t["tmp1"], t["tmp2"]], writes=[t[("mT", c)]])
            for hf2 in range(2):
                b = next_ps()
                for c in range(8):
                    sy.op("tensor", lambda e, b=b, c=c, hf2=hf2: e.matmul(
                        psb[b][:, :], lhsT=mT[:, c, :], rhs=wO[:, c, hf2 * 512:(hf2 + 1) * 512], start=(c == 0), stop=(c == 7)),
                        reads=[t["wO"], t[("mT", c)]], writes=[pst[b]])
                sy.op("vector", lambda e, b=b, hf2=hf2, sI=sI: e.scalar_tensor_tensor(
                    out=z[:, hf2 * 512:(hf2 + 1) * 512], in0=xs[sI][:, hf2 * 512:(hf2 + 1) * 512], scalar=ALPHA, in1=psb[b][:, :], op0=ALU.mult, op1=ALU.add),
                    reads=[pst[b], t[("xs", sI)]], writes=[t["z"]])
            hb_ = h1[i % 2]
            hk = t[("h1", i % 2)]
            layer_norm(t, z, t["z"], lnrep, hb_, hk, st6, mv)
            sy.dma("sync", h1_t[i, :, :], hb_[:], reads=[hk], writes=[tH[("h1d", i)]], stream="h1w")
            for half in range(2):
                b = next_ps()
                for kk in range(4):
                    kc = half * 4 + kk
                    sy.op("tensor", lambda e, b=b, kk=kk, kc=kc, hb_=hb_: e.transpose(
                        out=psb[b][:, kk * 128:(kk + 1) * 128], in_=hb_[:, kc * 128:(kc + 1) * 128], identity=ident[:]),
                        reads=[hk, tC["ident"]], writes=[pst[b]])
                src = psb[b][:, :].rearrange("p (k c) -> p k c", k=4)
                sy.op("vector", lambda e, src=src, half=half: e.tensor_copy(out=h1T32[:, half * 4:half * 4 + 4, :], in_=src),
                      reads=[pst[b]], writes=[t[("h1T32", half)]])
                sy.op("scalar", lambda e, src=src, half=half: e.copy(out=h1Tb[i % 2][:, half * 4:half * 4 + 4, :], in_=src),
                      reads=[pst[b]], writes=[t[("h1Tb", i % 2, half)]])
            sy.dma("sync", h1T_d[i, :, :], h1Tb[i % 2][:].rearrange("p k n -> p (k n)"), reads=[t[("h1Tb", i % 2, 0)], t[("h1Tb", i % 2, 1)]],
                   writes=[tH[("h1T", i)]], stream="h1w")
            b = next_ps()
            for kc in range(8):
                sy.op("tensor", lambda e, b=b, kc=kc: e.matmul(psb[b][:, 0:36], lhsT=h1T32[:, kc, :], rhs=wR[:, kc, :], start=(kc == 0), stop=(kc == 7)),
                      reads=[t[("h1T32", 0)], t[("h1T32", 1)], t["wR"]], writes=[pst[b]])
            V = lambda fn, rd, wr: sy.op("vector", fn, reads=rd, writes=wr)
            rk = t["rt"]
            V(lambda e, b=b: e.tensor_tensor(out=lg[:], in0=psb[b][:, 0:36], in1=brep[:], op=ALU.add), [pst[b], t["brep"]], [rk])
            V(lambda e: e.tensor_reduce(out=rt[:, 0:1], in_=lg[:, 0:4], axis=AX.X, op=ALU.max), [rk], [rk])
            V(lambda e: e.tensor_scalar(out=rt[:, 4:8], in0=lg[:, 0:4], scalar1=rt[:, 0:1], scalar2=None, op0=ALU.is_ge), [rk], [rk])
            V(lambda e: e.tensor_scalar(out=rt[:, 1:2], in0=rt[:, 0:1], scalar1=-1.0, scalar2=None, op0=ALU.mult), [rk], [rk])
            sy.op("scalar", lambda e: e.activation(out=rt[:, 8:12], in_=lg[:, 0:4], func=AF.Exp, bias=rt[:, 1:2], scale=1.0), reads=[rk], writes=[rk])
            V(lambda e: e.tensor_reduce(out=rt[:, 2:3], in_=rt[:, 8:12], axis=AX.X, op=ALU.add), [rk], [rk])
            V(lambda e: e.reciprocal(out=rt[:, 3:4], in_=rt[:, 2:3]), [rk], [rk])
            V(lambda e: e.tensor_scalar(out=rt[:, 8:12], in0=rt[:, 4:8], scalar1=-1.0, scalar2=1.0e9, op0=ALU.add, op1=ALU.mult), [rk], [rk])
            V(lambda e: e.tensor_tensor(out=rt[:, 16:48].rearrange("p (g e) -> p g e", g=4), in0=lg[:, 4:36].rearrange("p (g e) -> p g e", g=4),
                                        in1=rt[:, 8:12].unsqueeze(2).to_broadcast([128, 4, 8]), op=ALU.add), [rk], [rk])
            V(lambda e: e.max(out=m8[:], in_=rt[:, 16:48]), [rk], [t["m8"]])
            V(lambda e: e.tensor_tensor(out=rt[:, 12:13], in0=m8[:, 0:1], in1=m8[:, 1:2], op=ALU.subtract), [t["m8"]], [rk])
            sy.op("scalar", lambda e: e.activation(out=rt[:, 12:13], in_=rt[:, 12:13], func=AF.Sigmoid), reads=[rk], writes=[rk])
            V(lambda e: e.tensor_scalar(out=rt[:, 13:14], in0=rt[:, 12:13], scalar1=-1.0, scalar2=1.0, op0=ALU.mult, op1=ALU.add), [rk], [rk])
            V(lambda e: e.tensor_scalar(out=rt[:, 12:14], in0=rt[:, 12:14], scalar1=rt[:, 3:4], scalar2=None, op0=ALU.mult), [rk], [rk])
            V(lambda e: e.tensor_scalar(out=rt[:, 48:64], in0=rt[:, 16:32], scalar1=0.0, scalar2=None, op0=ALU.mult), [rk], [rk])
            V(lambda e: e.tensor_scalar(out=Wt[:, i, :], in0=rt[:, 16:48], scalar1=m8[:, 1:2], scalar2=rt[:, 13:14], op0=ALU.is_ge, op1=ALU.mult),
              [rk, t["m8"]], [tH[("Wt", i)]])
            V(lambda e: e.tensor_scalar(out=lg[:, 4:36], in0=rt[:, 16:48], scalar1=m8[:, 0:1], scalar2=None, op0=ALU.is_ge), [rk, t["m8"]], [rk])
            V(lambda e: e.tensor_tensor(out=rt[:, 14:15], in0=rt[:, 12:13], in1=rt[:, 13:14], op=ALU.subtract), [rk], [rk])
            V(lambda e: e.scalar_tensor_tensor(out=Wt[:, i, :], in0=lg[:, 4:36], scalar=rt[:, 14:15], in1=Wt[:, i, :], op0=ALU.mult, op1=ALU.add),
              [rk], [tH[("Wt", i)]])
        sy.barrier()
        es.close()

    sy.new_epoch()
    if not dbg.get("skipC"):
        stage_C()
    es_y.close()
    if "h1" in dbg:
        o = P.dout("dbg_h1", [OWN, D], F32)
        sy.dma("sync", o[:, :], h1_d[:, :], reads=tH.all(), stream="o")
        o = P.dout("dbg_Wt", [128, NT * 32], F32)
        sy.dma("sync", o[:, :], Wt[:].rearrange("p a b -> p (a b)"), reads=tH.all(), stream="o")

    def stage_D():
        es = contextlib.ExitStack()
        t = TK()
        out_t = out.rearrange("(n p) d -> n p d", p=128)
        yacc = SB(es, "D_yacc", [128, 16, D], F32)
        lnrep = SB(es, "D_lnrep", [128, 2, D], F32)
        sy.dma("sync", lnrep[:].rearrange("p a d -> p (a d)"), cd["lnrep"][:, 2 * D:4 * D], writes=[t["ln"]], stream="c")
        h1T = SB(es, "D_h1T", [128, NT, 8, 128], BF16)
        for i in range(NT):
            sy.dma("sync", h1T[:, i, :, :].rearrange("p k n -> p (k n)"), h1T_d[i, :, :], reads=[tH[("h1T", i)]],
                   writes=[t[("h1T", i)]], stream="h1r")
        wgu = SB(es, "D_wgu", [128, 8, 2 * D_EXP], BF16)
        wdn = SB(es, "D_wdn", [128, 4, D], BF16)
        wst = [SB(es, f"D_wst{i}", [128, 2, 1024], F32) for i in range(2)]
        aT = SB(es, "D_aT", [128, 4, 512], BF16)
        sg = [SB(es, f"D_sg{i}", [128, 512], F32) for i in range(2)]
        hz = [SB(es, f"D_hz{i}", [128, D], F32) for i in range(2)]
        st6 = SB(es, "D_st6", [128, 12], F32)
        mv = SB(es, "D_mv", [128, 4], F32)
        ps_rot = [0]

        def next_ps():
            i = ps_rot[0]
            ps_rot[0] = i + 1
            return i % 8

        wcnt = [0]
        cast_eng = ("gpsimd", "vector", "gpsimd", "scalar")
        for hh in range(2):
            for j in range(16):
                sy.op("gpsimd", lambda e, j=j: e.memset(yacc[:, j, :], 0.0), writes=[t[("yacc", j)]])
            for ex in range(N_EXP):
                for r2 in range(6):
                    wi = wcnt[0] % 2
                    wcnt[0] += 1
                    if r2 < 4:
                        src = cd["w_gu"][ex, r2 * 256:(r2 + 1) * 256, :].rearrange("(k p) c -> p k c", p=128)
                        dst, dk = wgu[:, r2 * 2:r2 * 2 + 2, :], "wgu"
                    else:
                        src = cd["w_dn"][ex, (r2 - 4) * 256:(r2 - 3) * 256, :].rearrange("(k p) c -> p k c", p=128)
                        dst, dk = wdn[:, (r2 - 4) * 2:(r2 - 4) * 2 + 2, :], "wdn"
                    sy.dma("sync", wst[wi][:], src, writes=[t[("wst", wi)]], stream=f"e{wi}")
                    ce = cast_eng[r2 % 4]
                    if ce == "scalar":
                        sy.op("scalar", lambda e, dst=dst, wi=wi: e.copy(out=dst, in_=wst[wi][:]), reads=[t[("wst", wi)]], writes=[t[dk]])
                    else:
                        sy.op(ce, lambda e, dst=dst, wi=wi: e.tensor_copy(out=dst, in_=wst[wi][:]), reads=[t[("wst", wi)]], writes=[t[dk]])
                for c4 in range(4):
                    tok0 = hh * 2048 + c4 * 512
                    hr = [t[("h1T", (tok0 // 128) + jj)] for jj in range(4)]
                    for cc in range(4):
                        bg = next_ps()
                        bu = next_ps()
                        for (bb, ct) in ((bg, cc), (bu, 4 + cc)):
                            for kc in range(8):
                                sy.op("tensor", lambda e, bb=bb, ct=ct, kc=kc, tok0=tok0: e.matmul(
                                    psb[bb][:, :], lhsT=wgu[:, kc, ct * 128:(ct + 1) * 128], rhs=h1T[:, tok0 // 128:tok0 // 128 + 4, kc, :], start=(kc == 0), stop=(kc == 7)),
                                    reads=[t["wgu"]] + hr, writes=[pst[bb]])
                        si = cc % 2
                        sy.op("scalar", lambda e, bg=bg, si=si: e.activation(out=sg[si][:], in_=psb[bg][:, :], func=AF.Silu), reads=[pst[bg]], writes=[t[("sg", si)]])
                        sy.op("vector", lambda e, bu=bu, si=si, cc=cc: e.tensor_tensor(out=aT[:, cc, :], in0=psb[bu][:, :], in1=sg[si][:], op=ALU.mult),
                              reads=[pst[bu], t[("sg", si)]], writes=[t[("aT", cc)]])
                    for jj in range(4):
                        j = c4 * 4 + jj
                        for hf2 in range(2):
                            b = next_ps()
                            for k in range(4):
                                sy.op("tensor", lambda e, b=b, k=k, jj=jj, hf2=hf2: e.matmul(
                                    psb[b][:, :], lhsT=aT[:, k, jj * 128:(jj + 1) * 128], rhs=wdn[:, k, hf2 * 512:(hf2 + 1) * 512], start=(k == 0), stop=(k == 3)),
                                    reads=[t["wdn"], t[("aT", k)]], writes=[pst[b]])
                            sy.op("vector", lambda e, b=b, j=j, hf2=hf2, ex=ex: e.scalar_tensor_tensor(
                                out=yacc[:, j, hf2 * 512:(hf2 + 1) * 512], in0=psb[b][:, :], scalar=Wt[:, hh * 16 + j, ex:ex + 1],
                                in1=yacc[:, j, hf2 * 512:(hf2 + 1) * 512], op0=ALU.mult, op1=ALU.add),
                                reads=[pst[b], tH[("Wt", hh * 16 + j)]], writes=[t[("yacc", j)]])
            for j in range(16):
                i = hh * 16 + j
                hb_ = hz[j % 2]
                hk = t[("hz", j % 2)]
                sy.dma("sync", hb_[:], h1_t[i, :, :], reads=[tH[("h1d", i)]], writes=[hk], stream="h1r")
                sy.op("vector", lambda e, j=j, hb_=hb_: e.scalar_tensor_tensor(out=yacc[:, j, :], in0=hb_[:], scalar=ALPHA, in1=yacc[:, j, :], op0=ALU.mult, op1=ALU.add),
                      reads=[hk], writes=[t[("yacc", j)]])
                layer_norm(t, yacc[:, j, :], t[("yacc", j)], lnrep, hb_, hk, st6, mv)
                sy.dma("sync", out_t[i, :, :], hb_[:], reads=[hk], stream="o")
        sy.barrier()
        es.close()

    sy.new_epoch()
    if not dbg.get("skipD"):
        stage_D()
    sy.barrier()
    S = sy.dsem.get("o")
    if S is not None:
        nc.sync.wait_ge(S["sem"], 16 * S["cnt"])
    return P


def make_core_map(inputs, W, b, hf, names):
    x = np.asarray(inputs["x"][b], dtype=np.float32)
    if hf == 1:
        xin = x
    else:
        xin = np.concatenate([np.zeros((OWN, D), np.float32), x[:OWN]], axis=0)
    m = {"xin": np.ascontiguousarray(xin)}
    m.update(W)
    m.update(make_consts(hf))
    return {k: m[k] for k in names}


_PROG = None


def kernel(**inputs):
    global _PROG
    inputs = {k: np.asarray(v) for k, v in inputs.items()}
    if _PROG is None:
        _PROG = build_program()
    P = _PROG
    W = weight_layouts(inputs)
    names = list(P.ins)
    consts = [make_consts(0), make_consts(1)]
    maps = []
    for c in range(8):
        b, hf = c // 2, c % 2
        x = np.asarray(inputs["x"][b], dtype=np.float32)
        if hf == 1:
            xin = x
        else:
            xin = np.concatenate([np.zeros((OWN, D), np.float32), x[:OWN]], axis=0)
        m = {"xin": np.ascontiguousarray(xin)}
        m.update(W)
        m.update(consts[hf])
        maps.append({k: m[k] for k in names})
    res = run_bass_kernel_spmd(P.nc, maps, core_ids=list(range(8)))
    out = np.zeros((NB, SEQ, D), np.float32)
    for c in range(8):
        b, hf = c // 2, c % 2
        out[b, hf * OWN:(hf + 1) * OWN] = np.asarray(res.results[c]["out"], dtype=np.float32)
    return out
```

```python
import contextlib
import numpy as np
import ml_dtypes
import concourse.bass as bass
import concourse.mybir as mybir
from concourse.bass_utils import run_bass_kernel_spmd
from concourse.alu_op_type import AluOpType as ALU

F32 = mybir.dt.float32
BF16 = mybir.dt.bfloat16
AF = mybir.ActivationFunctionType
AX = mybir.AxisListType

D = 1024
SEQ = 8192
NB = 4
OWN = 4096
NT = OWN // 128
HD = 64
DIL = ((128, 1), (512, 4), (2048, 16))
IN_COLS = 5656
C_AQ, C_AK, C_AV = 0, 768, 1536
C_BQ = 2304
C_BKV = 2816
C_BG = 3584
C_MG = 3608
NEG = -30000.0
ALPHA = 2.0 ** 0.25
LN_EPS = 1e-5
N_EXP = 32
D_EXP = 512

DEBUG = {}


class Trk:
    __slots__ = ("w", "r", "x")

    def __init__(self, x=False):
        self.w = None
        self.r = {}
        self.x = x


class TK:
    def __init__(self):
        self.d = {}

    def __getitem__(self, k):
        t = self.d.get(k)
        if t is None:
            t = self.d[k] = Trk()
        return t

    def all(self):
        return list(self.d.values())


class Sy:
    def __init__(self, nc):
        self.nc = nc
        self.eng = {}
        for name in ("tensor", "vector", "scalar", "gpsimd", "sync"):
            self.eng[name] = dict(e=getattr(nc, name), sem=nc.alloc_semaphore(f"s_{name}"), cnt=0, known={})
        self.dsem = {}
        self.ninst = 0
        self.epoch = 0

    def new_epoch(self):
        self.barrier()
        self.epoch += 1
        for name, E in self.eng.items():
            E["sem"] = self.nc.alloc_semaphore(f"s_{name}_{self.epoch}")
            E["cnt"] = 0
            E["known"] = {}
        self.dsem = {}

    def _wait(self, E, deps):
        best = {}
        for sem, val in deps:
            k = id(sem)
            if k not in best or best[k][1] < val:
                best[k] = (sem, val)
        for k, (sem, val) in best.items():
            if E["known"].get(k, 0) >= val:
                continue
            E["e"].wait_ge(sem, val)
            E["known"][k] = val
            self.ninst += 1

    def _deps(self, E, reads, writes, skip_own):
        deps = []
        for t in reads:
            if t.w is not None:
                deps.append(t.w)
        for t in writes:
            if t.w is not None:
                deps.append(t.w)
            deps.extend(t.r.values())
        if skip_own:
            deps = [d for d in deps if d[0] is not E["sem"]]
        return deps

    def op(self, name, fn, reads=(), writes=()):
        E = self.eng[name]
        if any(t.x for t in reads):
            writes = list(writes) + [t for t in reads if t.x]
            reads = [t for t in reads if not t.x]
        self._wait(E, self._deps(E, reads, writes, name == "tensor"))
        ins = fn(E["e"])
        E["cnt"] += 1
        ins.then_inc(E["sem"], 1)
        self.ninst += 1
        tok = (E["sem"], E["cnt"])
        for t in writes:
            t.w = tok
            t.r = {}
        for t in reads:
            t.r[id(E["sem"])] = tok
        return tok

    def dma(self, qname, out, in_, reads=(), writes=(), stream="d"):
        E = self.eng[qname]
        S = self.dsem.get(stream)
        if S is None:
            S = self.dsem[stream] = dict(sem=self.nc.alloc_semaphore(f"d_{stream}_{self.epoch}"), cnt=0)
        self._wait(E, self._deps(E, reads, writes, False))
        ins = E["e"].dma_start(out=out, in_=in_)
        S["cnt"] += 1
        ins.then_inc(S["sem"], 16)
        self.ninst += 1
        tok = (S["sem"], 16 * S["cnt"])
        for t in writes:
            t.w = tok
            t.r = {}
        for t in reads:
            t.r[id(S["sem"])] = tok
        return tok

    def barrier(self):
        toks = [(E["sem"], E["cnt"]) for E in self.eng.values() if E["cnt"] > 0]
        toks += [(S["sem"], 16 * S["cnt"]) for S in self.dsem.values()]
        for E in self.eng.values():
            self._wait(E, [t for t in toks if t[0] is not E["sem"]])


def SSL(base, d):
    return slice(base, base + 127 * d + 1, d)


def _bf(a):
    return np.asarray(a, dtype=np.float32).astype(ml_dtypes.bfloat16)


def alibi(n):
    return np.exp2(-8.0 * np.arange(1, n + 1, dtype=np.float32) / n).astype(np.float32)


def make_consts(hf):
    c = {}
    c["ident"] = np.eye(128, dtype=np.float32)
    c["identb"] = _bf(np.eye(128))
    k = np.arange(128)[:, None]
    q = np.arange(128)[None, :]
    sl = alibi(12).reshape(3, 4)
    bm = np.zeros((128, 3, 4, 2, 128), np.float32)
    for g, (win, d) in enumerate(DIL):
        for h in range(4):
            dprev = (q - k + 128).astype(np.float32)
            dcur = (q - k).astype(np.float32)
            bm[:, g, h, 0, :] = np.where(dprev <= 128, -8.0 * sl[g, h] * d * dprev, 8.0 * NEG)
            bm[:, g, h, 1, :] = np.where(dcur >= 0, -8.0 * sl[g, h] * d * dcur, 8.0 * NEG)
    c["bmA"] = bm.reshape(128, -1)
    c["vflag"] = np.tile(np.array([[float(hf), 1.0]], np.float32), (128, 1))
    slb = alibi(8)
    u = np.arange(2 * OWN)
    c["kaug"] = _bf(np.stack([(u % 128) - 64.0, u // 128, np.ones_like(u)]).astype(np.float32))
    qa = np.zeros((3, 8, OWN), np.float32)
    qt = 32 + np.arange(OWN) // 128
    for h in range(8):
        qa[0, h] = 8.0 * slb[h]
        qa[1, h] = 1024.0 * slb[h]
        qa[2, h] = -1024.0 * slb[h] * qt
    c["qaug"] = _bf(qa.reshape(3, 8 * OWN))
    cup = np.arange(128)[:, None]
    mcb = np.zeros((128, 17, 128), np.float32)
    for idx in range(17):
        off = idx * 8 - 2
        mcb[:, idx, :] = np.where(cup <= off + (q + 1) // 16, 0.0, NEG)
    c["mcb"] = _bf(mcb.reshape(128, -1))
    cu = np.arange(512)
    cval = ((cu <= 510) & ((cu >= 256) | (hf == 1))).astype(np.float32)
    c["cvalid"] = np.ascontiguousarray(cval.reshape(4, 128).T)
    c["validrep"] = _bf(np.repeat(cval.reshape(4, 128).T[:, :, None], 128, axis=2).reshape(128, -1))
    jb = np.arange(128)
    ovl = ((16 * cu[:, None] < 64 * jb[None, :] + 64) & (16 * cu[:, None] + 32 > 64 * jb[None, :])).astype(np.float32)
    ovl = ovl * cval[:, None]
    c["ovl"] = _bf(ovl.reshape(4, 128, 128).transpose(1, 0, 2).reshape(128, -1))
    wd = np.zeros((128, 190), np.float32)
    qq = np.arange(128)[:, None]
    jj = np.arange(190)[None, :] - 62
    cur = 64 + (qq >= 64)
    wd = np.where(jj > cur, -1.0e9, np.where((jj == cur) | (jj == cur - 1), 1.0e4, 0.0)).astype(np.float32)
    c["wd"] = wd
    fb = np.zeros((128,), np.float32)
    if hf == 1:
        fb[0] = 1.0e4
    else:
        fb[:64] = -1.0e9
        fb[64] = 1.0e4
    c["fbvec"] = np.tile(fb[None, :], (128, 1)).astype(np.float32)
    ind = np.zeros((128, 64, 128), np.float32)
    for kt in range(64):
        ind[2 * kt, kt, 0:64] = 1.0
        ind[2 * kt + 1, kt, 64:128] = 1.0
    c["indbig"] = _bf(ind.reshape(128, -1))
    c["tri_le"] = _bf(np.where(k <= q, 0.0, NEG))
    c["tri_gt"] = _bf(np.where(k > q, 0.0, NEG))
    return c


def weight_layouts(inp):
    w = {}
    w["w_in"] = np.ascontiguousarray(inp["w_in"][0])
    for kv in ("k", "v"):
        w1 = np.asarray(inp[f"cmp_w1_{kv}"][0])
        w[f"w1r_{kv}"] = np.ascontiguousarray(w1.reshape(32, 64, 256).transpose(1, 0, 2).reshape(64, 32 * 256))
        w[f"posT_{kv}"] = np.ascontiguousarray(np.asarray(inp[f"cmp_pos_{kv}"][0]).T)
        w[f"w2_{kv}"] = np.ascontiguousarray(inp[f"cmp_w2_{kv}"][0])
    w["w_ba"] = np.ascontiguousarray(inp["w_branch_a"][0])
    w["w_bb"] = np.ascontiguousarray(inp["w_branch_b"][0])
    w["w_out"] = np.ascontiguousarray(inp["w_out"][0])
    ln = np.concatenate([np.asarray(inp[k][0]).reshape(1, D) for k in ("ln1_g", "ln1_b", "ln2_g", "ln2_b")], axis=1)
    w["lnrep"] = np.ascontiguousarray(np.broadcast_to(ln, (128, 4 * D))).astype(np.float32)
    wf = np.asarray(inp["w_fine"][0]).transpose(1, 0, 2).reshape(D, 32)
    w["w_router"] = np.ascontiguousarray(np.concatenate([np.asarray(inp["w_coarse"][0]), wf], axis=1)).astype(np.float32)
    br = np.concatenate([np.asarray(inp["b_coarse"][0]).reshape(1, 4), np.asarray(inp["b_fine"][0]).reshape(1, 32)], axis=1)
    w["b_router"] = np.ascontiguousarray(np.broadcast_to(br, (128, 36))).astype(np.float32)
    if "w_gate_up" in inp:
        w["w_gu"] = np.ascontiguousarray(inp["w_gate_up"][0])
        w["w_dn"] = np.ascontiguousarray(inp["w_down"][0])
    return w


class Prog:
    def __init__(self, dbg=None):
        self.dbg = dbg or {}
        self.nc = bass.Bass("TRN2", target_bir_lowering=False)
        self.sy = Sy(self.nc)
        self.ins = {}
        self.outs = {}

    def din(self, name, shape, dt=F32):
        ap = self.nc.dram_tensor(name, list(shape), dt, kind="ExternalInput").ap()
        self.ins[name] = ap
        return ap

    def dout(self, name, shape, dt=F32):
        ap = self.nc.dram_tensor(name, list(shape), dt, kind="ExternalOutput").ap()
        self.outs[name] = ap
        return ap

    def dscratch(self, name, shape, dt=F32):
        return self.nc.dram_tensor(name, list(shape), dt, kind="Internal").ap()


def build_program(dbg=None):
    P = Prog(dbg)
    nc, sy = P.nc, P.sy
    dbg = P.dbg
    xin = P.din("xin", [2 * OWN, D])
    w_in = P.din("w_in", [D, IN_COLS])
    ident_d = P.din("ident", [128, 128])
    identb_d = P.din("identb", [128, 128], BF16)
    bmA_d = P.din("bmA", [128, 3 * 4 * 2 * 128])
    vflag_d = P.din("vflag", [128, 2])
    out = P.dout("out", [OWN, D])
    cd = {}
    for nm, shp, dt in (("kaug", [3, 2 * OWN], BF16), ("qaug", [3, 8 * OWN], BF16), ("mcb", [128, 17 * 128], BF16),
                        ("cvalid", [128, 4], F32), ("validrep", [128, 512], BF16), ("ovl", [128, 512], BF16),
                        ("wd", [128, 190], F32), ("fbvec", [128, 128], F32), ("indbig", [128, 64 * 128], BF16),
                        ("tri_le", [128, 128], BF16), ("tri_gt", [128, 128], BF16),
                        ("w1r_k", [64, 32 * 256], F32), ("posT_k", [64, 32], F32), ("w2_k", [256, 64], F32),
                        ("w1r_v", [64, 32 * 256], F32), ("posT_v", [64, 32], F32), ("w2_v", [256, 64], F32),
                        ("w_ba", [256, D], F32), ("w_bb", [512, D], F32), ("w_out", [D, D], F32),
                        ("lnrep", [128, 4 * D], F32), ("w_router", [D, 36], F32), ("b_router", [128, 36], F32),
                        ("w_gu", [N_EXP, D, 2 * D_EXP], F32), ("w_dn", [N_EXP, D_EXP, D], F32)):
        if nm in ("w_gu", "w_dn") and dbg.get("skipD"):
            continue
        cd[nm] = P.din(nm, shp, dt)
    h1_d = P.dscratch("h1_scratch", [OWN, D])

    es_glob = contextlib.ExitStack()
    SB = lambda es, name, shape, dt: es.enter_context(nc.sbuf_tensor(name, list(shape), dt))
    psb = [es_glob.enter_context(nc.psum_tensor(f"ps{i}", [128, 512], F32)) for i in range(8)]
    pst = [Trk(True) for _ in range(8)]

    ident = SB(es_glob, "ident_s", [128, 128], F32)
    identb = SB(es_glob, "identb_s", [128, 128], BF16)
    vflag = SB(es_glob, "vflag_s", [128, 2], F32)
    tC = TK()
    sy.dma("sync", ident[:], ident_d[:, :], writes=[tC["ident"]], stream="c")
    sy.dma("sync", identb[:], identb_d[:, :], writes=[tC["identb"]], stream="c")
    sy.dma("sync", vflag[:], vflag_d[:, :], writes=[tC["vflag"]], stream="c")

    Wt = SB(es_glob, "Wt", [128, NT, 32], F32)
    tH = TK()
    es_y = contextlib.ExitStack()
    yaT = SB(es_y, "yaT", [128, 2, OWN], BF16)
    tYa = TK()

    def stage_A():
        es = contextlib.ExitStack()
        xs = [SB(es, f"A_xs{i}", [128, D], F32) for i in range(2)]
        xT = SB(es, "A_xT", [128, 8, 2048], BF16)
        wst = [SB(es, "A_wst0", [128, 2, 768], F32)] * 2
        wA = SB(es, "A_w", [128, 8, 768], BF16)
        bmA = SB(es, "A_bm", [128, 4, 2, 128], F32)
        Kp = [SB(es, f"A_Kp{g}", [64, 4, 128 * d], BF16) for g, (_, d) in enumerate(DIL)]
        Vp = [SB(es, f"A_Vp{g}", [128, d, 4, 128], BF16) for g, (_, d) in enumerate(DIL)]
        Kc = SB(es, "A_Kc", [64, 4, 2048], BF16)
        Qc = SB(es, "A_Qc", [64, 4, 2048], BF16)
        Vc = SB(es, "A_Vc", [128, 16, 4, 128], BF16)
        acc = SB(es, "A_acc", [128, 4, 2048], F32)
        PT = [SB(es, f"A_PT{i}", [128, 512], BF16) for i in range(3)]
        t = TK()
        ps_rot = [0]

        def next_ps():
            i = ps_rot[0]
            ps_rot[0] = (i + 1) % 8
            return i

        xin_t = xin.rearrange("(n p) d -> n p d", p=128)
        nload = [0]

        def load_xT(tile0, ntiles):
            for j in range(ntiles):
                s = nload[0] % 2
                nload[0] += 1
                sy.dma("sync", xs[s][:], xin_t[tile0 + j, :, :], writes=[t[("xs", s)]], stream=f"x{s}")
                for half in range(2):
                    b = next_ps()
                    for kk in range(4):
                        kc = half * 4 + kk
                        sy.op("tensor", lambda e, b=b, kk=kk, kc=kc, s=s: e.transpose(
                            out=psb[b][:, kk * 128:(kk + 1) * 128], in_=xs[s][:, kc * 128:(kc + 1) * 128], identity=ident[:]),
                            reads=[t[("xs", s)], tC["ident"]], writes=[pst[b]])
                    eng = "vector" if half == 0 else "scalar"
                    if eng == "vector":
                        sy.op("vector", lambda e, b=b, half=half, j=j: e.tensor_copy(
                            out=xT[:, half * 4:half * 4 + 4, j * 128:(j + 1) * 128],
                            in_=psb[b][:, :].rearrange("p (k c) -> p k c", k=4)),
                            reads=[pst[b]], writes=[t[("xT", j, half)]])
                    else:
                        sy.op("scalar", lambda e, b=b, half=half, j=j: e.copy(
                            out=xT[:, half * 4:half * 4 + 4, j * 128:(j + 1) * 128],
                            in_=psb[b][:, :].rearrange("p (k c) -> p k c", k=4)),
                            reads=[pst[b]], writes=[t[("xT", j, half)]])

        def load_wA(g):
            sy.dma("gpsimd", bmA[:].rearrange("p b c d -> p (b c d)"), bmA_d[:, g * 1024:(g + 1) * 1024], writes=[t["bm"]], stream="c")
            for kc2 in range(4):
                s = 0
                for part, c0 in enumerate((C_AQ, C_AK, C_AV)):
                    col = c0 + g * 256
                    sy.dma("gpsimd", wst[s][:, :, part * 256:(part + 1) * 256],
                           w_in[kc2 * 256:(kc2 + 1) * 256, col:col + 256].rearrange("(k p) c -> p k c", p=128),
                           writes=[t[("wst", s)]], stream=f"w{s}")
                sy.op("gpsimd", lambda e, s=s, kc2=kc2: e.tensor_copy(out=wA[:, kc2 * 2:kc2 * 2 + 2, :], in_=wst[s][:]),
                      reads=[t[("wst", s)]], writes=[t["wA"]])

        xT_all = [t[("xT", j, hh)] for j in range(16) for hh in range(2)]
        aslopes = alibi(12).reshape(3, 4)

        for sc in (-1, 0, 1):
            own = sc >= 0
            tile0 = 32 + sc * 16
            load_xT(tile0, 16)
            for g, (win, d) in enumerate(DIL):
                nblk = 16 // d
                load_wA(g)
                for which, dst, cbase in (("q", Qc, 0), ("k", Kc, 256)):
                    if which == "q" and not own:
                        continue
                    for h in range(4):
                        for nck in range(4):
                            b = next_ps()
                            for kc in range(8):
                                sy.op("tensor", lambda e, b=b, kc=kc, h=h, nck=nck, cbase=cbase: e.matmul(
                                    psb[b][0:64, :], lhsT=wA[:, kc, cbase + h * 64:cbase + (h + 1) * 64],
                                    rhs=xT[:, kc, nck * 512:(nck + 1) * 512], start=(kc == 0), stop=(kc == 7)),
                                    reads=[t["wA"]] + xT_all[nck * 8:nck * 8 + 8], writes=[pst[b]])
                            eng = "vector" if (h + nck) % 2 == 0 else "scalar"
                            if eng == "vector":
                                sy.op("vector", lambda e, b=b, dst=dst, h=h, nck=nck: e.tensor_copy(
                                    out=dst[:, h, nck * 512:(nck + 1) * 512], in_=psb[b][0:64, :]),
                                    reads=[pst[b]], writes=[t[(which, h)]])
                            else:
                                sy.op("scalar", lambda e, b=b, dst=dst, h=h, nck=nck: e.copy(
                                    out=dst[:, h, nck * 512:(nck + 1) * 512], in_=psb[b][0:64, :]),
                                    reads=[pst[b]], writes=[t[(which, h)]])
                for n in range(nblk):
                    for r in range(d):
                        ti = n * d + r
                        b = next_ps()
                        base = n * 128 * d + r
                        for kc in range(8):
                            sy.op("tensor", lambda e, b=b, kc=kc, base=base, d=d: e.matmul(
                                psb[b][:, 0:256], lhsT=xT[:, kc, SSL(base, d)] if d > 1 else xT[:, kc, base:base + 128],
                                rhs=wA[:, kc, 512:768], start=(kc == 0), stop=(kc == 7)),
                                reads=[t["wA"]] + xT_all, writes=[pst[b]])
                        sy.op("vector", lambda e, b=b, ti=ti: e.tensor_copy(
                            out=Vc[:, ti, :, 0:64], in_=psb[b][:, 0:256].rearrange("p (h e) -> p h e", h=4)),
                            reads=[pst[b]], writes=[t[("V", ti)]])
                        fcol = 1 if own else 0
                        sy.op("gpsimd", lambda e, ti=ti, fcol=fcol: e.tensor_copy(
                            out=Vc[:, ti, :, 64:128], in_=vflag[:, None, fcol:fcol + 1].to_broadcast([128, 4, 64])),
                            reads=[tC["vflag"]], writes=[t[("Vf", ti)]])
                if own:
                    pairs = [(r, n) for n in range(nblk) for r in range(d)]
                    units = [(h, p0, half) for h in range(4) for p0 in range(0, 16, 4) for half in range(2)]
                    pvbank = {}
                    ptidx = {}

                    def ksl_of(h, r, n, pc):
                        if pc == 1:
                            return (Kc[:, h, SSL(n * 128 * d + r, d)] if d > 1 else Kc[:, h, n * 128:(n + 1) * 128]), [t[("k", h)]]
                        if n > 0:
                            return (Kc[:, h, SSL((n - 1) * 128 * d + r, d)] if d > 1 else Kc[:, h, (n - 1) * 128:n * 128]), [t[("k", h)]]
                        return (Kp[g][:, h, SSL(r, d)] if d > 1 else Kp[g][:, h, 0:128]), [t[("Kp", g)]]

                    def vsl_of(h, r, n, pc):
                        if pc == 1:
                            return Vc[:, n * d + r, h, :], [t[("V", n * d + r)], t[("Vf", n * d + r)]]
                        if n > 0:
                            return Vc[:, (n - 1) * d + r, h, :], [t[("V", (n - 1) * d + r)], t[("Vf", (n - 1) * d + r)]]
                        return Vp[g][:, r, h, :], [t[("Vp", g)]]

                    def emit_SA(u):
                        h, p0, half = u
                        sb = next_ps()
                        sub = pairs[p0:p0 + 4][half * 2:half * 2 + 2]
                        first = True
                        for si, (r, n) in enumerate(sub):
                            qsl = Qc[:, h, SSL(n * 128 * d + r, d)] if d > 1 else Qc[:, h, n * 128:(n + 1) * 128]
                            for pc in range(2):
                                col = (si * 2 + pc) * 128
                                ksl, kr = ksl_of(h, r, n, pc)
                                sy.op("tensor", lambda e, sb=sb, col=col, ksl=ksl, qsl=qsl, first=first: e.matmul(
                                    psb[sb][:, col:col + 128], lhsT=ksl, rhs=qsl, start=first, stop=False, skip_group_check=True),
                                    reads=kr + [t[("q", h)]], writes=[pst[sb]])
                                first = False
                                sy.op("tensor", lambda e, sb=sb, col=col, h=h, pc=pc: e.matmul(
                                    psb[sb][:, col:col + 128], lhsT=ident[:], rhs=bmA[:, h, pc, :], start=False, stop=True, skip_group_check=True),
                                    reads=[tC["ident"], t["bm"]], writes=[pst[sb]])
                        return sb

                    pcount = [0]

                    def emit_restA(u, sb):
                        h, p0, half = u
                        grp = pairs[p0:p0 + 4]
                        sub = grp[half * 2:half * 2 + 2]
                        pi = pcount[0] % 3
                        pcount[0] += 1
                        pt, ptk = PT[pi], t[("PT", pi)]
                        if half == 0:
                            pvbank[(h, p0)] = next_ps()
                        pvb = pvbank[(h, p0)]
                        sy.op("scalar", lambda e, sb=sb, pt=pt: e.activation(out=pt[:], in_=psb[sb][:, :], func=AF.Exp, scale=0.125),
                              reads=[pst[sb]], writes=[ptk])
                        for si, (r, n) in enumerate(sub):
                            reg = (half * 2 + si) * 128
                            for pc in range(2):
                                col = (si * 2 + pc) * 128
                                vsl, vr = vsl_of(h, r, n, pc)
                                sy.op("tensor", lambda e, pvb=pvb, reg=reg, vsl=vsl, pt=pt, col=col, st=(half == 0 and si == 0 and pc == 0): e.matmul(
                                    psb[pvb][:, reg:reg + 128], lhsT=vsl, rhs=pt[:, col:col + 128], start=st, stop=True, skip_group_check=True),
                                    reads=vr + [ptk], writes=[pst[pvb]])
                        if half == 1:
                            r0, n0 = grp[0]
                            if d == 1:
                                dst = acc[:, h, n0 * 128:(n0 + 4) * 128]
                                src = psb[pvb][:, :]
                            else:
                                dst = acc[:, h, n0 * 128 * d:(n0 + 1) * 128 * d].rearrange("p (l r) -> p l r", r=d)[:, :, r0:r0 + 4]
                                src = psb[pvb][:, :].rearrange("p (r l) -> p l r", r=4)
                            if g == 0:
                                sy.op("vector", lambda e, dst=dst, src=src: e.tensor_copy(out=dst, in_=src),
                                      reads=[pst[pvb]], writes=[t[("acc", h)]])
                            else:
                                sy.op("vector", lambda e, dst=dst, src=src: e.tensor_tensor(out=dst, in0=dst, in1=src, op=ALU.add),
                                      reads=[pst[pvb]], writes=[t[("acc", h)]])

                    LOOKA = 2
                    pend = [emit_SA(u) for u in units[:LOOKA]]
                    for ui, u in enumerate(units):
                        if ui + LOOKA < len(units):
                            pend.append(emit_SA(units[ui + LOOKA]))
                        emit_restA(u, pend.pop(0))
                sy.op("gpsimd", lambda e, g=g, d=d: e.tensor_copy(out=Kp[g][:], in_=Kc[:, :, 2048 - 128 * d:2048]),
                      reads=[t[("k", h)] for h in range(4)], writes=[t[("Kp", g)]])
                sy.op("gpsimd", lambda e, g=g, d=d: e.tensor_copy(out=Vp[g][:], in_=Vc[:, 16 - d:16, :, :]),
                      reads=[t[("V", i)] for i in range(16)] + [t[("Vf", i)] for i in range(16)], writes=[t[("Vp", g)]])
            if own:
                for h in range(4):
                    hp, lo = h // 2, (h % 2) * 64
                    for s2 in range(2):
                        sy.op("vector", lambda e, h=h, s2=s2: e.reciprocal(out=xs[s2][0:64, :], in_=acc[64:128, h, s2 * 1024:(s2 + 1) * 1024]),
                              reads=[t[("acc", h)]], writes=[t[("xs", s2)]])
                        sy.op("vector", lambda e, h=h, hp=hp, lo=lo, s2=s2: e.tensor_tensor(
                            out=yaT[lo:lo + 64, hp, sc * 2048 + s2 * 1024:sc * 2048 + (s2 + 1) * 1024],
                            in0=acc[0:64, h, s2 * 1024:(s2 + 1) * 1024], in1=xs[s2][0:64, :], op=ALU.mult),
                            reads=[t[("acc", h)], t[("xs", s2)]], writes=[tYa[(hp, sc, lo, s2)]])
        sy.barrier()
        es.close()

    if not dbg.get("skipA"):
        stage_A()
    else:
        sy.op("gpsimd", lambda e: e.memset(yaT[:], 0.0), writes=[tYa["z"]])

    if "yaT" in dbg:
        o = P.dout("dbg_yaT", [128, 2 * OWN], BF16)
        sy.dma("sync", o[:, :], yaT[:].rearrange("p a b -> p (a b)"), reads=tYa.all(), stream="o")


    ybT = SB(es_y, "ybT", [128, 4, OWN], BF16)
    tYb = TK()

    def stage_B():
        es = contextlib.ExitStack()
        t = TK()
        xin_t = xin.rearrange("(n p) d -> n p d", p=128)
        ps_rot = [0]

        def next_ps(lo=0, hi=8):
            i = ps_rot[0]
            ps_rot[0] = i + 1
            return lo + i % (hi - lo)

        xs = [SB(es, "B_xs0", [128, D], F32)] * 2
        nload = [0]

        ps_hi = [8]

        def emit_xT(tile_u, dst, j, key):
            sI = 0
            nload[0] += 1
            sy.dma("sync", xs[sI][:], xin_t[tile_u, :, :], writes=[t[("xs", sI)]], stream=f"x{sI}")
            for half in range(2):
                b = next_ps(0, ps_hi[0])
                for kk in range(4):
                    kc = half * 4 + kk
                    sy.op("tensor", lambda e, b=b, kk=kk, kc=kc, sI=sI: e.transpose(
                        out=psb[b][:, kk * 128:(kk + 1) * 128], in_=xs[sI][:, kc * 128:(kc + 1) * 128], identity=ident[:]),
                        reads=[t[("xs", sI)], tC["ident"]], writes=[pst[b]])
                src = psb[b][:, :].rearrange("p (k c) -> p k c", k=4)
                o = dst[:, half * 4:half * 4 + 4, j * 128:(j + 1) * 128]
                if half == 0:
                    sy.op("vector", lambda e, o=o, src=src: e.tensor_copy(out=o, in_=src), reads=[pst[b]], writes=[t[(key, j, half)]])
                else:
                    sy.op("scalar", lambda e, o=o, src=src: e.copy(out=o, in_=src), reads=[pst[b]], writes=[t[(key, j, half)]])

        wst = SB(es, "B_wst", [128, 2, 768], F32)

        def load_w(dst, c0, ncol, key):
            for kc2 in range(4):
                sy.dma("gpsimd", wst[:, :, 0:ncol], w_in[kc2 * 256:(kc2 + 1) * 256, c0:c0 + ncol].rearrange("(k p) c -> p k c", p=128),
                       writes=[t["wst"]], stream="w0")
                sy.op("gpsimd", lambda e, kc2=kc2: e.tensor_copy(out=dst[:, kc2 * 2:kc2 * 2 + 2, :], in_=wst[:, :, 0:ncol]),
                      reads=[t["wst"]], writes=[t[key]])

        slcK = SB(es, "B_slcK", [67, 2, 2 * OWN], BF16)
        winK = SB(es, "B_winK", [67, 2, 36 * 128], BF16)
        slcV = SB(es, "B_slcV", [128, 64, 2, 66], BF16)
        winV = SB(es, "B_winV", [128, 36, 2, 66], BF16)
        KcT = SB(es, "B_KcT", [64, 2, 512], BF16)
        Vcm = SB(es, "B_Vcm", [128, 4, 2, 64], BF16)
        for g in range(2):
            sy.dma("gpsimd", slcK[64:67, g, :], cd["kaug"][:, :], writes=[t[("slcKaug", g)]], stream="c")
            sy.dma("gpsimd", winK[64:67, g, :], cd["kaug"][:, 28 * 128:], writes=[t[("winKaug", g)]], stream="c")
        sy.op("gpsimd", lambda e: e.tensor_copy(out=slcV[:, 0:32, :, 64:66], in_=vflag[:, None, None, 0:1].to_broadcast([128, 32, 2, 2])),
              reads=[tC["vflag"]], writes=[t["slcVf"]])
        sy.op("gpsimd", lambda e: e.tensor_copy(out=slcV[:, 32:64, :, 64:66], in_=vflag[:, None, None, 1:2].to_broadcast([128, 32, 2, 2])),
              reads=[tC["vflag"]], writes=[t["slcVf"]])
        sy.op("gpsimd", lambda e: e.tensor_copy(out=winV[:, 0:4, :, 64:66], in_=vflag[:, None, None, 0:1].to_broadcast([128, 4, 2, 2])),
              reads=[tC["vflag"]], writes=[t["winVf"]])
        sy.op("gpsimd", lambda e: e.tensor_copy(out=winV[:, 4:36, :, 64:66], in_=vflag[:, None, None, 1:2].to_broadcast([128, 32, 2, 2])),
              reads=[tC["vflag"]], writes=[t["winVf"]])

        es1 = contextlib.ExitStack()
        raw = SB(es1, "B_raw", [128, 2, 2 * OWN], BF16)
        es1b = contextlib.ExitStack()
        wB = SB(es1b, "B_wB", [128, 8, 768], BF16)
        xTc = [SB(es1b, f"B_xTc{i}", [128, 8, 512], BF16) for i in range(2)]
        load_w(wB, C_BKV, 768, "wB")
        for ch in range(16):
            xb_ = xTc[ch % 2]
            xk = ("xTc", ch % 2)
            for j in range(4):
                emit_xT(ch * 4 + j, xb_, j, xk)
            xr = [t[(xk, j, hh)] for j in range(4) for hh in range(2)]
            for (ii, dst, off, key) in () if dbg.get("noFM") else ((0, raw, 0, "rawK"), (1, raw, 0, "rawV"), (2, slcK, 0, "slcK"), (4, winK, -28 * 128, "winK")):
                if ii == 4 and ch < 7:
                    continue
                for g in range(2):
                    b = next_ps()
                    c0 = ii * 128 + g * 64
                    for kc in range(8):
                        sy.op("tensor", lambda e, b=b, kc=kc, c0=c0, xb_=xb_: e.matmul(
                            psb[b][0:64, :], lhsT=wB[:, kc, c0:c0 + 64], rhs=xb_[:, kc, :], start=(kc == 0), stop=(kc == 7)),
                            reads=[t["wB"]] + xr, writes=[pst[b]])
                    pb = 64 if ii == 1 else 0
                    o = dst[pb:pb + 64, g, ch * 512 + off:ch * 512 + off + 512]
                    if g == 0:
                        sy.op("vector", lambda e, o=o, b=b: e.tensor_copy(out=o, in_=psb[b][0:64, :]), reads=[pst[b]], writes=[t[(key, g, ch)]])
                    else:
                        sy.op("scalar", lambda e, o=o, b=b: e.copy(out=o, in_=psb[b][0:64, :]), reads=[pst[b]], writes=[t[(key, g, ch)]])
            for j in range(0 if dbg.get("noTM") else 4):
                tu = ch * 4 + j
                b = next_ps()
                for kc in range(8):
                    sy.op("tensor", lambda e, b=b, kc=kc, j=j, xb_=xb_: e.matmul(
                        psb[b][:, 0:384], lhsT=xb_[:, kc, j * 128:(j + 1) * 128], rhs=wB[:, kc, 384:768], start=(kc == 0), stop=(kc == 7)),
                        reads=[t["wB"]] + xr, writes=[pst[b]])
                sy.op("vector", lambda e, b=b, tu=tu: e.tensor_copy(
                    out=slcV[:, tu, :, 0:64], in_=psb[b][:, 0:128].rearrange("p (g e) -> p g e", g=2)),
                    reads=[pst[b]], writes=[t[("slcV", tu)]])
                if tu >= 28:
                    sy.op("scalar", lambda e, b=b, tu=tu: e.copy(
                        out=winV[:, tu - 28, :, 0:64], in_=psb[b][:, 256:384].rearrange("p (g e) -> p g e", g=2)),
                        reads=[pst[b]], writes=[t[("winV", tu - 28)]])
        sy.barrier()
        es1b.close()
        if dbg.get("stopB1"):
            if "B1" in dbg and not dbg.get("noOut"):
                o3 = P.dout("dbg_slcK", [67, 2 * 2 * OWN], BF16)
                sy.dma("sync", o3[:, :], slcK[:].rearrange("p a b -> p (a b)"), stream="o")
                o4 = P.dout("dbg_winV", [128, 36 * 2 * 66], BF16)
                sy.dma("sync", o4[:, :], winV[:].rearrange("p a b c -> p (a b c)"), stream="o")
                o5 = P.dout("dbg_raw", [128, 2 * 2 * OWN], BF16)
                sy.dma("sync", o5[:, :], raw[:].rearrange("p a b -> p (a b)"), stream="o")
            sy.barrier()
            es1.close()
            es.close()
            return
        es2 = contextlib.ExitStack()
        w1r = SB(es2, "B_w1r", [128, 32, 256], BF16)
        w1st = SB(es2, "B_w1st", [128, 4, 256], F32)
        posT = SB(es2, "B_posT", [128, 32], F32)
        posTb = SB(es2, "B_posTb", [128, 32], BF16)
        w2s = SB(es2, "B_w2s", [128, 2, 64], F32)
        w2b = SB(es2, "B_w2b", [128, 2, 64], BF16)
        hb = SB(es2, "B_hb", [128, 2], F32)
        h1 = SB(es2, "B_h1", [128, 512], F32)
        h1x = SB(es2, "B_h1x", [128, 512], F32)
        h1T = SB(es2, "B_h1T", [128, 2, 512], BF16)
        cvalid = SB(es2, "B_cvalid", [128, 4], F32)
        sy.dma("sync", cvalid[:], cd["cvalid"][:, :], writes=[t["cvalid"]], stream="c")
        sy.op("gpsimd", lambda e: e.memset(h1T[:], 0.0), writes=[t["h1T"]])
        for kv, PB in (("k", 0), ("v", 64)):
            for pp in range(8):
                sy.dma("sync", w1st[PB:PB + 64].rearrange("e p h -> e (p h)"), cd[f"w1r_{kv}"][:, pp * 1024:(pp + 1) * 1024], writes=[t["w1st"]], stream="c2")
                sy.op("gpsimd", lambda e, pp=pp, PB=PB: e.tensor_copy(out=w1r[PB:PB + 64, pp * 4:pp * 4 + 4, :], in_=w1st[PB:PB + 64]), reads=[t["w1st"]], writes=[t["w1r"]])
            sy.dma("sync", posT[PB:PB + 64, :], cd[f"posT_{kv}"][:, :], writes=[t["posT"]], stream="c2")
            sy.op("gpsimd", lambda e, PB=PB: e.tensor_copy(out=posTb[PB:PB + 64, :], in_=posT[PB:PB + 64, :]), reads=[t["posT"]], writes=[t["posTb"]])
            sy.dma("sync", w2s[:], cd[f"w2_{kv}"].rearrange("(c p) e -> p c e", p=128), writes=[t["w2s"]], stream="c2")
            sy.op("gpsimd", lambda e: e.tensor_copy(out=w2b[:], in_=w2s[:]), reads=[t["w2s"]], writes=[t["w2b"]])
            b = next_ps()
            for hc in range(2):
                for p in range(32):
                    sy.op("tensor", lambda e, b=b, hc=hc, p=p, PB=PB: e.matmul(
                        psb[b][:, hc:hc + 1], lhsT=w1r[PB:PB + 64, p, hc * 128:(hc + 1) * 128], rhs=posTb[PB:PB + 64, p:p + 1],
                        start=(p == 0 and hc == 0), stop=(p == 31), skip_group_check=True),
                        reads=[t["w1r"], t["posTb"]], writes=[pst[b]])
            sy.op("vector", lambda e, b=b: e.tensor_copy(out=hb[:], in_=psb[b][:, 0:2]), reads=[pst[b]], writes=[t["hb"]])
            rawr = [t[("rawK" if kv == "k" else "rawV", g, ch)] for g in range(2) for ch in range(16)]
            for g in range(2):
                for hc in range(2):
                    b = next_ps()
                    for p in range(32):
                        sy.op("tensor", lambda e, b=b, hc=hc, p=p, g=g, PB=PB: e.matmul(
                            psb[b][:, 0:511], lhsT=w1r[PB:PB + 64, p, hc * 128:(hc + 1) * 128], rhs=raw[PB:PB + 64, g, slice(p, p + 16 * 510 + 1, 16)],
                            start=(p == 0), stop=(p == 31)),
                            reads=[t["w1r"]] + rawr, writes=[pst[b]])
                    sy.op("vector", lambda e, b=b, hc=hc: e.tensor_scalar(out=h1[:, 0:511], in0=psb[b][:, 0:511], scalar1=hb[:, hc:hc + 1], scalar2=None, op0=ALU.add),
                          reads=[pst[b], t["hb"]], writes=[t["h1"]])
                    sy.op("vector", lambda e: e.tensor_tensor(out=h1x[:, 0:511], in0=h1[:, 0:511], in1=h1[:, 0:511], op=ALU.mult),
                          reads=[t["h1"]], writes=[t["h1x"]])
                    sy.op("vector", lambda e: e.tensor_scalar(out=h1x[:, 0:511], in0=h1x[:, 0:511], scalar1=0.044715, scalar2=1.0, op0=ALU.mult, op1=ALU.add),
                          reads=[t["h1x"]], writes=[t["h1x"]])
                    sy.op("vector", lambda e: e.tensor_tensor(out=h1x[:, 0:511], in0=h1x[:, 0:511], in1=h1[:, 0:511], op=ALU.mult),
                          reads=[t["h1"], t["h1x"]], writes=[t["h1x"]])
                    sy.op("scalar", lambda e: e.activation(out=h1x[:, 0:511], in_=h1x[:, 0:511], func=AF.Sigmoid, scale=1.5957691216057308),
                          reads=[t["h1x"]], writes=[t["h1x"]])
                    sy.op("vector", lambda e, hc=hc: e.tensor_tensor(out=h1T[:, hc, 0:511], in0=h1x[:, 0:511], in1=h1[:, 0:511], op=ALU.mult),
                          reads=[t["h1"], t["h1x"]], writes=[t["h1T"]])
                if kv == "k":
                    b = next_ps()
                    for hc in range(2):
                        sy.op("tensor", lambda e, b=b, hc=hc: e.matmul(psb[b][0:64, :], lhsT=w2b[:, hc, :], rhs=h1T[:, hc, :], start=(hc == 0), stop=(hc == 1)),
                              reads=[t["w2b"], t["h1T"]], writes=[pst[b]])
                    sy.op("vector", lambda e, b=b, g=g: e.tensor_copy(out=KcT[:, g, :], in_=psb[b][0:64, :]), reads=[pst[b]], writes=[t[("KcT", g)]])
                else:
                    b = next_ps()
                    for ct in range(4):
                        for hc in range(2):
                            sy.op("tensor", lambda e, b=b, hc=hc, ct=ct: e.matmul(
                                psb[b][:, ct * 64:(ct + 1) * 64], lhsT=h1T[:, hc, ct * 128:(ct + 1) * 128], rhs=w2b[:, hc, :],
                                start=(hc == 0 and ct == 0), stop=(hc == 1), skip_group_check=True),
                                reads=[t["w2b"], t["h1T"]], writes=[pst[b]])
                    for ct in range(4):
                        sy.op("vector", lambda e, b=b, g=g, ct=ct: e.tensor_scalar(
                            out=Vcm[:, ct, g, :], in0=psb[b][:, ct * 64:(ct + 1) * 64], scalar1=cvalid[:, ct:ct + 1], scalar2=None, op0=ALU.mult),
                            reads=[pst[b], t["cvalid"]], writes=[t[("Vcm", g)]])
        sy.barrier()
        es2.close()
        es1.close()
        if "B2" in dbg:
            o1 = P.dout("dbg_KcT", [64, 1024], BF16)
            sy.dma("sync", o1[:, :], KcT[:].rearrange("p a b -> p (a b)"), reads=t.all(), stream="o")
            o2 = P.dout("dbg_Vcm", [128, 512], BF16)
            sy.dma("sync", o2[:, :], Vcm[:].rearrange("p a b c -> p (a b c)"), reads=t.all(), stream="o")
            o3 = P.dout("dbg_slcK", [67, 2 * 2 * OWN], BF16)
            sy.dma("sync", o3[:, :], slcK[:].rearrange("p a b -> p (a b)"), reads=t.all(), stream="o")
            o4 = P.dout("dbg_winV", [128, 36 * 2 * 66], BF16)
            sy.dma("sync", o4[:, :], winV[:].rearrange("p a b c -> p (a b c)"), reads=t.all(), stream="o")
        if dbg.get("stopB2"):
            sy.barrier()
            es.close()
            return

        ps_hi[0] = 6
        wq = SB(es, "B_wq", [128, 8, 512], BF16)
        wg = SB(es, "B_wg", [128, 8, 24], BF16)
        load_w(wq, C_BQ, 512, "wq")
        load_w(wg, C_BG, 24, "wg")
        cs = {}
        for nm, shp, dt in (("mcb", [128, 17, 128], BF16), ("validrep", [128, 4, 128], BF16), ("ovl", [128, 4, 128], BF16),
                            ("wd", [128, 190], F32), ("fbvec", [128, 128], F32), ("indbig", [128, 64, 128], BF16),
                            ("tri_le", [128, 128], BF16), ("tri_gt", [128, 128], BF16)):
            cs[nm] = SB(es, "Bc_" + nm, shp, dt)
            dst = cs[nm][:]
            if len(shp) == 3:
                dst = dst.rearrange("p a b -> p (a b)")
            sy.dma("sync", dst, cd[nm][:, :], writes=[t["c_" + nm]], stream="c")
        xT1 = [SB(es, f"B_xT1{i}", [128, 8, 128], BF16) for i in range(2)]
        qTa = [SB(es, f"B_qTa{i}", [67, 8, 128], BF16) for i in range(2)]
        gates = [SB(es, f"B_gates{i}", [128, 24], F32) for i in range(2)]
        PcT = SB(es, "B_PcT", [128, 4, 512], BF16)
        Pn = SB(es, "B_Pn", [128, 4, 512], BF16)
        rden = SB(es, "B_rden", [128, 512], F32)
        score = SB(es, "B_score", [128, 128], F32)
        work = SB(es, "B_work", [128, 128], F32)
        pen = SB(es, "B_pen", [128, 128], F32)
        pen2 = SB(es, "B_pen2", [128, 128], F32)
        m8a = SB(es, "B_m8a", [128, 8], F32)
        m8b = SB(es, "B_m8b", [128, 8], F32)
        penT = [SB(es, f"B_penT{g}", [128, 4, 128], BF16) for g in range(2)]
        PT = [SB(es, f"B_PT{i}", [128, 512], BF16) for i in range(3)]
        oc_sb = SB(es, "B_oc", [128, 2, 4, 64], F32)
        os_sb = SB(es, "B_os", [128, 2, 4, 66], F32)
        ow_sb = SB(es, "B_ow", [128, 2, 4, 66], F32)
        rs = SB(es, "B_rs", [128, 2, 4, 2], F32)
        yb_tm = SB(es, "B_ybtm", [128, 512], F32)
        ytmp = SB(es, "B_ytmp", [128, 64], F32)
        OSB, OWB = 6, 7
        qaug3 = cd["qaug"].rearrange("r (h n) -> r h n", h=8)
        ptc = [0]

        def attn_tiles(i, g, kts, Ksrc, koff, Vsrc, accb, with_pen, qa, qk):
            kts = list(kts)
            LOOK = 2

            def emit_S(kt):
                sb = next_ps(0, 6)
                kl = kt - koff
                sy.op("tensor", lambda e, sb=sb, kl=kl: e.matmul(
                    psb[sb][:, :], lhsT=Ksrc[0:67, g, kl * 128:(kl + 1) * 128], rhs=qa[0:67, 4 * g:4 * g + 4, :], start=True, stop=False),
                    reads=[qk[0], qk[1]], writes=[pst[sb]])
                adds = []
                if with_pen:
                    adds.append((cs["indbig"][:, kt, :], penT[g][:], [t["c_indbig"], t[("penT", g)]]))
                if kt == 32 + i:
                    adds.append((identb[:], cs["tri_le"][:, None, :].to_broadcast([128, 4, 128]), [tC["identb"], t["c_tri_le"]]))
                if (not with_pen) and kt == 28 + i:
                    adds.append((identb[:], cs["tri_gt"][:, None, :].to_broadcast([128, 4, 128]), [tC["identb"], t["c_tri_gt"]]))
                for (l_, r_, rd) in adds:
                    sy.op("tensor", lambda e, sb=sb, l_=l_, r_=r_: e.matmul(psb[sb][:, :], lhsT=l_, rhs=r_, start=False, stop=True),
                          reads=rd, writes=[pst[sb]])
                return sb

            def emit_rest(kt, sb, first):
                kl = kt - koff
                pi = ptc[0] % 3
                ptc[0] += 1
                sy.op("scalar", lambda e, sb=sb, pi=pi: e.activation(out=PT[pi][:], in_=psb[sb][:, :], func=AF.Exp, scale=0.125),
                      reads=[pst[sb]], writes=[t[("PT", pi)]])
                for r in range(4):
                    sy.op("tensor", lambda e, r=r, pi=pi, kl=kl, st=(first and r == 0): e.matmul(
                        psb[accb][:, r * 66:(r + 1) * 66], lhsT=PT[pi][:, r * 128:(r + 1) * 128], rhs=Vsrc[:, kl, g, :],
                        start=st, stop=True, skip_group_check=True),
                        reads=[t[("PT", pi)]], writes=[pst[accb]])

            pend = [emit_S(kt) for kt in kts[:LOOK]]
            for n, kt in enumerate(kts):
                if n + LOOK < len(kts):
                    pend.append(emit_S(kts[n + LOOK]))
                emit_rest(kt, pend.pop(0), n == 0)

        for i in range(NT):
            xb_ = xT1[i % 2]
            xk = ("xT1", i % 2)
            emit_xT(32 + i, xb_, 0, xk)
            xr = [t[(xk, 0, 0)], t[(xk, 0, 1)]]
            qa = qTa[i % 2]
            qk = (t[("qTa", i % 2)], t[("qTaug", i % 2)])
            sy.dma("gpsimd", qa[64:67, :, :], qaug3[:, :, i * 128:(i + 1) * 128], writes=[qk[1]], stream="qa")
            for g in range(2):
                b = next_ps(0, 6)
                for r in range(4):
                    hh = 4 * g + r
                    for kc in range(8):
                        sy.op("tensor", lambda e, b=b, r=r, hh=hh, kc=kc, xb_=xb_: e.matmul(
                            psb[b][0:64, r * 128:(r + 1) * 128], lhsT=wq[:, kc, hh * 64:(hh + 1) * 64], rhs=xb_[:, kc, :],
                            start=(kc == 0 and r == 0), stop=(kc == 7), skip_group_check=True),
                            reads=[t["wq"]] + xr, writes=[pst[b]])
                sy.op("vector", lambda e, b=b, g=g, qa=qa: e.tensor_copy(
                    out=qa[0:64, 4 * g:4 * g + 4, :], in_=psb[b][0:64, :].rearrange("p (r n) -> p r n", r=4)),
                    reads=[pst[b]], writes=[qk[0]])
            b = next_ps(0, 6)
            for kc in range(8):
                sy.op("tensor", lambda e, b=b, kc=kc, xb_=xb_: e.matmul(psb[b][:, 0:24], lhsT=xb_[:, kc, :], rhs=wg[:, kc, :], start=(kc == 0), stop=(kc == 7)),
                      reads=[t["wg"]] + xr, writes=[pst[b]])
            gt = gates[i % 2]
            gk = t[("gates", i % 2)]
            sy.op("scalar", lambda e, b=b, gt=gt: e.activation(out=gt[:], in_=psb[b][:, 0:24], func=AF.Sigmoid), reads=[pst[b]], writes=[gk])
            for g in range(2):
                ctmax = (262 + 8 * i) // 128
                ncts = ctmax + 1
                for ct in range(ncts):
                    sb = next_ps(0, 6)
                    off = 254 + 8 * i - 128 * ct
                    need_mask = off < 127
                    sy.op("tensor", lambda e, sb=sb, ct=ct, nm=need_mask: e.matmul(
                        psb[sb][:, :], lhsT=KcT[:, g, ct * 128:(ct + 1) * 128], rhs=qa[0:64, 4 * g:4 * g + 4, :], start=True, stop=(not nm)),
                        reads=[t[("KcT", g)], qk[0]], writes=[pst[sb]])
                    if need_mask:
                        idx = (off + 2) // 8
                        assert 0 <= idx < 17, (i, ct, off)
                        sy.op("tensor", lambda e, sb=sb, idx=idx: e.matmul(
                            psb[sb][:, :], lhsT=identb[:], rhs=cs["mcb"][:, idx:idx + 1, :].to_broadcast([128, 4, 128]), start=False, stop=True),
                            reads=[tC["identb"], t["c_mcb"]], writes=[pst[sb]])
                    sy.op("scalar", lambda e, sb=sb, ct=ct: e.activation(out=PcT[:, ct, :], in_=psb[sb][:, :], func=AF.Exp, scale=0.125),
                          reads=[pst[sb]], writes=[t[("PcT", ct)]])
                db = next_ps(0, 6)
                for ct in range(ncts):
                    sy.op("tensor", lambda e, db=db, ct=ct: e.matmul(psb[db][:, :], lhsT=cs["validrep"][:, ct, :], rhs=PcT[:, ct, :], start=(ct == 0), stop=(ct == ncts - 1)),
                          reads=[t["c_validrep"], t[("PcT", ct)]], writes=[pst[db]])
                sy.op("vector", lambda e, db=db: e.tensor_scalar(out=rden[:], in0=psb[db][:, :], scalar1=1e-30, scalar2=None, op0=ALU.max),
                      reads=[pst[db]], writes=[t["rden"]])
                sy.op("vector", lambda e: e.reciprocal(out=rden[:], in_=rden[:]), reads=[t["rden"]], writes=[t["rden"]])
                for ct in range(ncts):
                    sy.op("vector" if ct % 2 == 0 else "gpsimd", lambda e, ct=ct: e.tensor_tensor(out=Pn[:, ct, :], in0=PcT[:, ct, :], in1=rden[:], op=ALU.mult),
                          reads=[t[("PcT", ct)], t["rden"]], writes=[t[("Pn", ct)]])
                ob = next_ps(0, 6)
                firstm = True
                for r in range(4):
                    for ct in range(ncts):
                        sy.op("tensor", lambda e, ob=ob, r=r, ct=ct, st=firstm: e.matmul(
                            psb[ob][:, r * 64:(r + 1) * 64], lhsT=Pn[:, ct, r * 128:(r + 1) * 128], rhs=Vcm[:, ct, g, :],
                            start=st, stop=True, skip_group_check=True),
                            reads=[t[("Pn", ct)], t[("Vcm", g)]], writes=[pst[ob]])
                        firstm = False
                for r in range(4):
                    for ct in range(ncts):
                        sy.op("tensor", lambda e, ob=ob, r=r, ct=ct: e.matmul(
                            psb[ob][:, 256:384], lhsT=Pn[:, ct, r * 128:(r + 1) * 128], rhs=cs["ovl"][:, ct, :],
                            start=False, stop=True, skip_group_check=True),
                            reads=[t[("Pn", ct)], t["c_ovl"]], writes=[pst[ob]])
                sy.op("scalar", lambda e, ob=ob, g=g: e.copy(out=oc_sb[:, g, :, :], in_=psb[ob][:, 0:256].rearrange("p (r e) -> p r e", r=4)),
                      reads=[pst[ob]], writes=[t[("oc", g)]])
                sy.op("vector", lambda e, ob=ob: e.tensor_tensor(out=score[:], in0=psb[ob][:, 256:384], in1=cs["wd"][:, 62 - 2 * i:190 - 2 * i], op=ALU.add),
                      reads=[pst[ob], t["c_wd"]], writes=[t["score"]])
                sy.op("vector", lambda e: e.tensor_tensor(out=score[:], in0=score[:], in1=cs["fbvec"][:], op=ALU.add),
                      reads=[t["c_fbvec"]], writes=[t["score"]])
                sy.op("vector", lambda e: e.max(out=m8a[:], in_=score[:]), reads=[t["score"]], writes=[t["m8a"]])
                sy.op("vector", lambda e: e.match_replace(out=work[:], in_to_replace=m8a[:], in_values=score[:], imm_value=-3.0e38),
                      reads=[t["score"], t["m8a"]], writes=[t["work"]])
                sy.op("vector", lambda e: e.max(out=m8b[:], in_=work[:]), reads=[t["work"]], writes=[t["m8b"]])
                sy.op("vector", lambda e: e.tensor_scalar(out=pen[:], in0=score[:], scalar1=m8b[:, 7:8], scalar2=NEG, op0=ALU.is_lt, op1=ALU.mult),
                      reads=[t["score"], t["m8b"]], writes=[t["pen"]])
                sy.op("vector", lambda e: e.tensor_scalar(out=pen2[:], in0=score[:], scalar1=-5.0e8, scalar2=NEG, op0=ALU.is_lt, op1=ALU.mult),
                      reads=[t["score"]], writes=[t["pen2"]])
                sy.op("vector", lambda e: e.tensor_tensor(out=pen[:], in0=pen[:], in1=pen2[:], op=ALU.min),
                      reads=[t["pen2"]], writes=[t["pen"]])
                tb = next_ps(0, 6)
                sy.op("tensor", lambda e, tb=tb: e.transpose(out=psb[tb][:, 0:128], in_=pen[:], identity=ident[:]),
                      reads=[t["pen"], tC["ident"]], writes=[pst[tb]])
                sy.op("vector", lambda e, tb=tb, g=g: e.tensor_copy(out=penT[g][:], in_=psb[tb][:, None, 0:128].to_broadcast([128, 4, 128])),
                      reads=[pst[tb]], writes=[t[("penT", g)]])
                attn_tiles(i, g, range(28 + i, 33 + i), winK, 28, winV, OWB, False, qa, qk)
                sy.op("vector", lambda e, g=g: e.tensor_copy(out=ow_sb[:, g, :, :], in_=psb[OWB][:, 0:264].rearrange("p (r e) -> p r e", r=4)),
                      reads=[pst[OWB]], writes=[t[("ow", g)]])
                attn_tiles(i, g, range(0, 33 + i), slcK, 0, slcV, OSB, True, qa, qk)
                sy.op("vector", lambda e, g=g: e.tensor_copy(out=os_sb[:, g, :, :], in_=psb[OSB][:, 0:264].rearrange("p (r e) -> p r e", r=4)),
                      reads=[pst[OSB]], writes=[t[("os", g)]])
            gt3 = gt[:].rearrange("p (g r b) -> p g r b", g=2, r=4)
            sy.op("vector", lambda e: e.reciprocal(out=rs[:, :, :, 0:1], in_=os_sb[:, :, :, 64:65]), reads=[t[("os", 0)], t[("os", 1)]], writes=[t["rs"]])
            sy.op("vector", lambda e: e.reciprocal(out=rs[:, :, :, 1:2], in_=ow_sb[:, :, :, 64:65]), reads=[t[("ow", 0)], t[("ow", 1)]], writes=[t["rs"]])
            sy.op("vector", lambda e, gt3=gt3: e.tensor_tensor(out=rs[:], in0=rs[:], in1=gt3[:, :, :, 1:3], op=ALU.mult), reads=[gk], writes=[t["rs"]])
            for g in range(2):
                for r in range(4):
                    col = (g * 4 + r) * 64
                    gc = gt[:, g * 12 + r * 3:g * 12 + r * 3 + 1]
                    sy.op("vector", lambda e, g=g, r=r, gc=gc: e.tensor_scalar(out=ytmp[:], in0=oc_sb[:, g, r, :], scalar1=gc, scalar2=None, op0=ALU.mult),
                          reads=[t[("oc", g)], gk], writes=[t["ytmp"]])
                    sy.op("vector", lambda e, g=g, r=r: e.scalar_tensor_tensor(out=ytmp[:], in0=os_sb[:, g, r, 0:64], scalar=rs[:, g, r, 0:1], in1=ytmp[:], op0=ALU.mult, op1=ALU.add),
                          reads=[t[("os", g)], t["rs"]], writes=[t["ytmp"]])
                    sy.op("vector", lambda e, g=g, r=r, col=col: e.scalar_tensor_tensor(out=yb_tm[:, col:col + 64], in0=ow_sb[:, g, r, 0:64], scalar=rs[:, g, r, 1:2], in1=ytmp[:], op0=ALU.mult, op1=ALU.add),
                          reads=[t[("ow", g)], t["rs"], t["ytmp"]], writes=[t["ybtm"]])
            tb = next_ps(0, 6)
            for c4 in range(4):
                sy.op("tensor", lambda e, tb=tb, c4=c4: e.transpose(out=psb[tb][:, c4 * 128:(c4 + 1) * 128], in_=yb_tm[:, c4 * 128:(c4 + 1) * 128], identity=ident[:]),
                      reads=[t["ybtm"], tC["ident"]], writes=[pst[tb]])
            sy.op("scalar", lambda e, tb=tb, i=i: e.copy(out=ybT[:, :, i * 128:(i + 1) * 128], in_=psb[tb][:, :].rearrange("p (c n) -> p c n", c=4)),
                  reads=[pst[tb]], writes=[tYb[i]])
        sy.barrier()
        es.close()

    sy.new_epoch()
    if not dbg.get("skipB"):
        stage_B()
    else:
        for i in range(NT):
            sy.op("gpsimd", lambda e, i=i: e.memset(ybT[:, :, i * 128:(i + 1) * 128], 0.0), writes=[tYb[i]])

    if "ybT" in dbg:
        o = P.dout("dbg_ybT", [128, 4 * OWN], BF16)
        sy.dma("sync", o[:, :], ybT[:].rearrange("p a b -> p (a b)"), reads=tYb.all(), stream="o")

    h1T_d = P.dscratch("h1T_scratch", [NT, 128, 8 * 128], BF16)
    h1_t = h1_d.rearrange("(n p) d -> n p d", p=128)

    def layer_norm(t, z, zk, lnrep, dst, dstk, tmp_stats, tmp_mv):
        for hh in range(2):
            sy.op("vector", lambda e, hh=hh: e.bn_stats(out=tmp_stats[:, hh * 6:(hh + 1) * 6], in_=z[:, hh * 512:(hh + 1) * 512]),
                  reads=[zk], writes=[t["lnst"]])
        sy.op("vector", lambda e: e.bn_aggr(out=tmp_mv[:, 0:2], in_=tmp_stats[:, 0:12]), reads=[t["lnst"]], writes=[t["lnmv"]])
        sy.op("vector", lambda e: e.tensor_scalar(out=tmp_mv[:, 2:3], in0=tmp_mv[:, 1:2], scalar1=LN_EPS, scalar2=None, op0=ALU.add),
              reads=[t["lnmv"]], writes=[t["lnmv"]])
        sy.op("scalar", lambda e: e.activation(out=tmp_mv[:, 2:3], in_=tmp_mv[:, 2:3], func=AF.Sqrt), reads=[t["lnmv"]], writes=[t["lnmv"]])
        sy.op("vector", lambda e: e.reciprocal(out=tmp_mv[:, 3:4], in_=tmp_mv[:, 2:3]), reads=[t["lnmv"]], writes=[t["lnmv"]])
        sy.op("vector", lambda e: e.tensor_scalar(out=dst[:], in0=z[:], scalar1=tmp_mv[:, 0:1], scalar2=tmp_mv[:, 3:4], op0=ALU.subtract, op1=ALU.mult),
              reads=[zk, t["lnmv"]], writes=[dstk])
        sy.op("gpsimd", lambda e: e.tensor_tensor(out=dst[:], in0=dst[:], in1=lnrep[:, 0, :], op=ALU.mult), reads=[t["ln"]], writes=[dstk])
        sy.op("gpsimd", lambda e: e.tensor_tensor(out=dst[:], in0=dst[:], in1=lnrep[:, 1, :], op=ALU.add), reads=[t["ln"]], writes=[dstk])

    def stage_C():
        es = contextlib.ExitStack()
        t = TK()
        xin_t = xin.rearrange("(n p) d -> n p d", p=128)
        ps_rot = [0]

        def next_ps():
            i = ps_rot[0]
            ps_rot[0] = i + 1
            return i % 8

        lnrep = SB(es, "C_lnrep", [128, 2, D], F32)
        sy.dma("sync", lnrep[:].rearrange("p a d -> p (a d)"), cd["lnrep"][:, 0:2 * D], writes=[t["ln"]], stream="c")
        wst = SB(es, "C_wst", [128, 2, 1024], F32)
        wM = SB(es, "C_wM", [128, 8, 2048], BF16)
        wAB = SB(es, "C_wAB", [128, 6, D], BF16)
        wO = SB(es, "C_wO", [128, 8, D], BF16)
        wR = SB(es, "C_wR", [128, 8, 36], F32)
        brep = SB(es, "C_brep", [128, 36], F32)
        xs = [SB(es, f"C_xs{i}", [128, D], F32) for i in range(2)]
        xT1 = SB(es, "C_xT1", [128, 8, 128], BF16)
        gT = SB(es, "C_gT", [128, 16, 128], F32)
        mT = SB(es, "C_mT", [128, 8, 128], BF16)
        tmp1 = SB(es, "C_tmp1", [128, 128], F32)
        tmp2 = SB(es, "C_tmp2", [128, 128], F32)
        z = SB(es, "C_z", [128, D], F32)
        h1 = [SB(es, f"C_h1{i}", [128, D], F32) for i in range(2)]
        h1T32 = SB(es, "C_h1T32", [128, 8, 128], F32)
        h1Tb = [SB(es, f"C_h1Tb{i}", [128, 8, 128], BF16) for i in range(2)]
        st6 = SB(es, "C_st6", [128, 12], F32)
        mv = SB(es, "C_mv", [128, 4], F32)
        lg = SB(es, "C_lg", [128, 36], F32)
        rt = SB(es, "C_rt", [128, 64], F32)
        m8 = SB(es, "C_m8", [128, 8], F32)

        def load_wgen(dst, src2d, rows, ncol, key):
            for r2 in range(rows // 256):
                sy.dma("gpsimd", wst[:, :, 0:ncol], src2d[r2 * 256:(r2 + 1) * 256, :].rearrange("(k p) c -> p k c", p=128),
                       writes=[t["wst"]], stream="w0")
                sy.op("gpsimd", lambda e, r2=r2: e.tensor_copy(out=dst[:, r2 * 2:r2 * 2 + 2, :], in_=wst[:, :, 0:ncol]),
                      reads=[t["wst"]], writes=[t[key]])

        load_wgen(wM[:, :, 0:1024], w_in[:, C_MG:C_MG + 1024], D, 1024, "wM")
        load_wgen(wM[:, :, 1024:2048], w_in[:, C_MG + 1024:C_MG + 2048], D, 1024, "wM")
        load_wgen(wAB[:, 0:2, :], cd["w_ba"], 256, D, "wAB")
        load_wgen(wAB[:, 2:6, :], cd["w_bb"], 512, D, "wAB")
        load_wgen(wO, cd["w_out"], D, D, "wO")
        sy.dma("sync", wR[:], cd["w_router"].rearrange("(k p) c -> p k c", p=128), writes=[t["wR"]], stream="c")
        sy.dma("sync", brep[:], cd["b_router"][:, :], writes=[t["brep"]], stream="c")

        for i in range(NT):
            sI = i % 2
            sy.dma("sync", xs[sI][:], xin_t[32 + i, :, :], writes=[t[("xs", sI)]], stream=f"x{sI}")
            for half in range(2):
                b = next_ps()
                for kk in range(4):
                    kc = half * 4 + kk
                    sy.op("tensor", lambda e, b=b, kk=kk, kc=kc, sI=sI: e.transpose(
                        out=psb[b][:, kk * 128:(kk + 1) * 128], in_=xs[sI][:, kc * 128:(kc + 1) * 128], identity=ident[:]),
                        reads=[t[("xs", sI)], tC["ident"]], writes=[pst[b]])
                sy.op("vector" if half == 0 else "scalar",
                      (lambda e, b=b, half=half: e.tensor_copy(out=xT1[:, half * 4:half * 4 + 4, :], in_=psb[b][:, :].rearrange("p (k c) -> p k c", k=4))) if half == 0 else
                      (lambda e, b=b, half=half: e.copy(out=xT1[:, half * 4:half * 4 + 4, :], in_=psb[b][:, :].rearrange("p (k c) -> p k c", k=4))),
                      reads=[pst[b]], writes=[t[("xT1", half)]])
            xr = [t[("xT1", 0)], t[("xT1", 1)]]
            tok = slice(i * 128, (i + 1) * 128)
            for c4 in range(4):
                b = next_ps()
                for cc in range(4):
                    ct = c4 * 4 + cc
                    for kc in range(8):
                        sy.op("tensor", lambda e, b=b, cc=cc, ct=ct, kc=kc: e.matmul(
                            psb[b][:, cc * 128:(cc + 1) * 128], lhsT=wM[:, kc, ct * 128:(ct + 1) * 128], rhs=xT1[:, kc, :],
                            start=(kc == 0 and cc == 0), stop=(kc == 7), skip_group_check=True),
                            reads=[t["wM"]] + xr, writes=[pst[b]])
                sy.op("scalar", lambda e, b=b, c4=c4: e.activation(out=gT[:, c4 * 4:c4 * 4 + 4, :], in_=psb[b][:, :].rearrange("p (c n) -> p c n", c=4), func=AF.Sigmoid),
                      reads=[pst[b]], writes=[t[("gT", c4)]])
            for c in range(8):
                b = next_ps()
                for k2 in range(2):
                    sy.op("tensor", lambda e, b=b, c=c, k2=k2: e.matmul(
                        psb[b][:, 0:128], lhsT=wAB[:, k2, c * 128:(c + 1) * 128], rhs=yaT[:, k2, tok], start=(k2 == 0), stop=(k2 == 1), skip_group_check=True),
                        reads=[t["wAB"]] + tYa.all(), writes=[pst[b]])
                for k4 in range(4):
                    sy.op("tensor", lambda e, b=b, c=c, k4=k4: e.matmul(
                        psb[b][:, 128:256], lhsT=wAB[:, 2 + k4, c * 128:(c + 1) * 128], rhs=ybT[:, k4, tok], start=False, stop=(k4 == 3), skip_group_check=True),
                        reads=[t["wAB"], tYb[i]], writes=[pst[b]])
                sy.op("vector", lambda e, b=b, c=c: e.tensor_tensor(out=tmp1[:], in0=psb[b][:, 0:128], in1=gT[:, c, :], op=ALU.mult),
                      reads=[pst[b], t[("gT", c // 4)]], writes=[t["tmp1"]])
                sy.op("vector", lambda e, b=b, c=c: e.tensor_tensor(out=tmp2[:], in0=psb[b][:, 128:256], in1=gT[:, 8 + c, :], op=ALU.mult),
                      reads=[pst[b], t[("gT", 2 + c // 4)]], writes=[t["tmp2"]])
                sy.op("gpsimd", lambda e, c=c: e.tensor_tensor(out=mT[:, c, :], in0=tmp1[:], in1=tmp2[:], op=ALU.add),
                      reads=[t["tmp1"], t["tmp2"]], writes=[t[("mT", c)]])
            for hf2 in range(2):
                b = next_ps()
                for c in range(8):
                    sy.op("tensor", lambda e, b=b, c=c, hf2=hf2: e.matmul(
                        psb[b][:, :], lhsT=mT[:, c, :], rhs=wO[:, c, hf2 * 512:(hf2 + 1) * 512], start=(c == 0), stop=(c == 7)),
                        reads=[t["wO"], t[("mT", c)]], writes=[pst[b]])
                sy.op("vector", lambda e, b=b, hf2=hf2, sI=sI: e.scalar_tensor_tensor(
                    out=z[:, hf2 * 512:(hf2 + 1) * 512], in0=xs[sI][:, hf2 * 512:(hf2 + 1) * 512], scalar=ALPHA, in1=psb[b][:, :], op0=ALU.mult, op1=ALU.add),
                    reads=[pst[b], t[("xs", sI)]], writes=[t["z"]])
            hb_ = h1[i % 2]
            hk = t[("h1", i % 2)]
            layer_norm(t, z, t["z"], lnrep, hb_, hk, st6, mv)
            sy.dma("sync", h1_t[i, :, :], hb_[:], reads=[hk], writes=[tH[("h1d", i)]], stream="h1w")
            for half in range(2):
                b = next_ps()
                for kk in range(4):
                    kc = half * 4 + kk
                    sy.op("tensor", lambda e, b=b, kk=kk, kc=kc, hb_=hb_: e.transpose(
                        out=psb[b][:, kk * 128:(kk + 1) * 128], in_=hb_[:, kc * 128:(kc + 1) * 128], identity=ident[:]),
                        reads=[hk, tC["ident"]], writes=[pst[b]])
                src = psb[b][:, :].rearrange("p (k c) -> p k c", k=4)
                sy.op("vector", lambda e, src=src, half=half: e.tensor_copy(out=h1T32[:, half * 4:half * 4 + 4, :], in_=src),
                      reads=[pst[b]], writes=[t[("h1T32", half)]])
                sy.op("scalar", lambda e, src=src, half=half: e.copy(out=h1Tb[i % 2][:, half * 4:half * 4 + 4, :], in_=src),
                      reads=[pst[b]], writes=[t[("h1Tb", i % 2, half)]])
            sy.dma("sync", h1T_d[i, :, :], h1Tb[i % 2][:].rearrange("p k n -> p (k n)"), reads=[t[("h1Tb", i % 2, 0)], t[("h1Tb", i % 2, 1)]],
                   writes=[tH[("h1T", i)]], stream="h1w")
            b = next_ps()
            for kc in range(8):
                sy.op("tensor", lambda e, b=b, kc=kc: e.matmul(psb[b][:, 0:36], lhsT=h1T32[:, kc, :], rhs=wR[:, kc, :], start=(kc == 0), stop=(kc == 7)),
                      reads=[t[("h1T32", 0)], t[("h1T32", 1)], t["wR"]], writes=[pst[b]])
            V = lambda fn, rd, wr: sy.op("vector", fn, reads=rd, writes=wr)
            rk = t["rt"]
            V(lambda e, b=b: e.tensor_tensor(out=lg[:], in0=psb[b][:, 0:36], in1=brep[:], op=ALU.add), [pst[b], t["brep"]], [rk])
            V(lambda e: e.tensor_reduce(out=rt[:, 0:1], in_=lg[:, 0:4], axis=AX.X, op=ALU.max), [rk], [rk])
            V(lambda e: e.tensor_scalar(out=rt[:, 4:8], in0=lg[:, 0:4], scalar1=rt[:, 0:1], scalar2=None, op0=ALU.is_ge), [rk], [rk])
            V(lambda e: e.tensor_scalar(out=rt[:, 1:2], in0=rt[:, 0:1], scalar1=-1.0, scalar2=None, op0=ALU.mult), [rk], [rk])
            sy.op("scalar", lambda e: e.activation(out=rt[:, 8:12], in_=lg[:, 0:4], func=AF.Exp, bias=rt[:, 1:2], scale=1.0), reads=[rk], writes=[rk])
            V(lambda e: e.tensor_reduce(out=rt[:, 2:3], in_=rt[:, 8:12], axis=AX.X, op=ALU.add), [rk], [rk])
            V(lambda e: e.reciprocal(out=rt[:, 3:4], in_=rt[:, 2:3]), [rk], [rk])
            V(lambda e: e.tensor_scalar(out=rt[:, 8:12], in0=rt[:, 4:8], scalar1=-1.0, scalar2=1.0e9, op0=ALU.add, op1=ALU.mult), [rk], [rk])
            V(lambda e: e.tensor_tensor(out=rt[:, 16:48].rearrange("p (g e) -> p g e", g=4), in0=lg[:, 4:36].rearrange("p (g e) -> p g e", g=4),
                                        in1=rt[:, 8:12].unsqueeze(2).to_broadcast([128, 4, 8]), op=ALU.add), [rk], [rk])
            V(lambda e: e.max(out=m8[:], in_=rt[:, 16:48]), [rk], [t["m8"]])
            V(lambda e: e.tensor_tensor(out=rt[:, 12:13], in0=m8[:, 0:1], in1=m8[:, 1:2], op=ALU.subtract), [t["m8"]], [rk])
            sy.op("scalar", lambda e: e.activation(out=rt[:, 12:13], in_=rt[:, 12:13], func=AF.Sigmoid), reads=[rk], writes=[rk])
            V(lambda e: e.tensor_scalar(out=rt[:, 13:14], in0=rt[:, 12:13], scalar1=-1.0, scalar2=1.0, op0=ALU.mult, op1=ALU.add), [rk], [rk])
            V(lambda e: e.tensor_scalar(out=rt[:, 12:14], in0=rt[:, 12:14], scalar1=rt[:, 3:4], scalar2=None, op0=ALU.mult), [rk], [rk])
            V(lambda e: e.tensor_scalar(out=rt[:, 48:64], in0=rt[:, 16:32], scalar1=0.0, scalar2=None, op0=ALU.mult), [rk], [rk])
            V(lambda e: e.tensor_scalar(out=Wt[:, i, :], in0=rt[:, 16:48], scalar1=m8[:, 1:2], scalar2=rt[:, 13:14], op0=ALU.is_ge, op1=ALU.mult),
              [rk, t["m8"]], [tH[("Wt", i)]])
            V(lambda e: e.tensor_scalar(out=lg[:, 4:36], in0=rt[:, 16:48], scalar1=m8[:, 0:1], scalar2=None, op0=ALU.is_ge), [rk, t["m8"]], [rk])
            V(lambda e: e.tensor_tensor(out=rt[:, 14:15], in0=rt[:, 12:13], in1=rt[:, 13:14], op=ALU.subtract), [rk], [rk])
            V(lambda e: e.scalar_tensor_tensor(out=Wt[:, i, :], in0=lg[:, 4:36], scalar=rt[:, 14:15], in1=Wt[:, i, :], op0=ALU.mult, op1=ALU.add),
              [rk], [tH[("Wt", i)]])
        sy.barrier()
        es.close()

    sy.new_epoch()
    if not dbg.get("skipC"):
        stage_C()
    es_y.close()
    if "h1" in dbg:
        o = P.dout("dbg_h1", [OWN, D], F32)
        sy.dma("sync", o[:, :], h1_d[:, :], reads=tH.all(), stream="o")
        o = P.dout("dbg_Wt", [128, NT * 32], F32)
        sy.dma("sync", o[:, :], Wt[:].rearrange("p a b -> p (a b)"), reads=tH.all(), stream="o")

    def stage_D():
        es = contextlib.ExitStack()
        t = TK()
        out_t = out.rearrange("(n p) d -> n p d", p=128)
        yacc = SB(es, "D_yacc", [128, 16, D], F32)
        lnrep = SB(es, "D_lnrep", [128, 2, D], F32)
        sy.dma("sync", lnrep[:].rearrange("p a d -> p (a d)"), cd["lnrep"][:, 2 * D:4 * D], writes=[t["ln"]], stream="c")
        h1T = SB(es, "D_h1T", [128, NT, 8, 128], BF16)
        for i in range(NT):
            sy.dma("sync", h1T[:, i, :, :].rearrange("p k n -> p (k n)"), h1T_d[i, :, :], reads=[tH[("h1T", i)]],
                   writes=[t[("h1T", i)]], stream="h1r")
        wgu = SB(es, "D_wgu", [128, 8, 2 * D_EXP], BF16)
        wdn = SB(es, "D_wdn", [128, 4, D], BF16)
        wst = [SB(es, f"D_wst{i}", [128, 2, 1024], F32) for i in range(2)]
        aT = SB(es, "D_aT", [128, 4, 512], BF16)
        sg = [SB(es, f"D_sg{i}", [128, 512], F32) for i in range(2)]
        hz = [SB(es, f"D_hz{i}", [128, D], F32) for i in range(2)]
        st6 = SB(es, "D_st6", [128, 12], F32)
        mv = SB(es, "D_mv", [128, 4], F32)
        ps_rot = [0]

        def next_ps():
            i = ps_rot[0]
            ps_rot[0] = i + 1
            return i % 8

        wcnt = [0]
        cast_eng = ("gpsimd", "vector", "gpsimd", "scalar")
        for hh in range(2):
            for j in range(16):
                sy.op("gpsimd", lambda e, j=j: e.memset(yacc[:, j, :], 0.0), writes=[t[("yacc", j)]])
            for ex in range(N_EXP):
                for r2 in range(6):
                    wi = wcnt[0] % 2
                    wcnt[0] += 1
                    if r2 < 4:
                        src = cd["w_gu"][ex, r2 * 256:(r2 + 1) * 256, :].rearrange("(k p) c -> p k c", p=128)
                        dst, dk = wgu[:, r2 * 2:r2 * 2 + 2, :], "wgu"
                    else:
                        src = cd["w_dn"][ex, (r2 - 4) * 256:(r2 - 3) * 256, :].rearrange("(k p) c -> p k c", p=128)
                        dst, dk = wdn[:, (r2 - 4) * 2:(r2 - 4) * 2 + 2, :], "wdn"
                    sy.dma("sync", wst[wi][:], src, writes=[t[("wst", wi)]], stream=f"e{wi}")
                    ce = cast_eng[r2 % 4]
                    if ce == "scalar":
                        sy.op("scalar", lambda e, dst=dst, wi=wi: e.copy(out=dst, in_=wst[wi][:]), reads=[t[("wst", wi)]], writes=[t[dk]])
                    else:
                        sy.op(ce, lambda e, dst=dst, wi=wi: e.tensor_copy(out=dst, in_=wst[wi][:]), reads=[t[("wst", wi)]], writes=[t[dk]])
                for c4 in range(4):
                    tok0 = hh * 2048 + c4 * 512
                    hr = [t[("h1T", (tok0 // 128) + jj)] for jj in range(4)]
                    for cc in range(4):
                        bg = next_ps()
                        bu = next_ps()
                        for (bb, ct) in ((bg, cc), (bu, 4 + cc)):
                            for kc in range(8):
                                sy.op("tensor", lambda e, bb=bb, ct=ct, kc=kc, tok0=tok0: e.matmul(
                                    psb[bb][:, :], lhsT=wgu[:, kc, ct * 128:(ct + 1) * 128], rhs=h1T[:, tok0 // 128:tok0 // 128 + 4, kc, :], start=(kc == 0), stop=(kc == 7)),
                                    reads=[t["wgu"]] + hr, writes=[pst[bb]])
                        si = cc % 2
                        sy.op("scalar", lambda e, bg=bg, si=si: e.activation(out=sg[si][:], in_=psb[bg][:, :], func=AF.Silu), reads=[pst[bg]], writes=[t[("sg", si)]])
                        sy.op("vector", lambda e, bu=bu, si=si, cc=cc: e.tensor_tensor(out=aT[:, cc, :], in0=psb[bu][:, :], in1=sg[si][:], op=ALU.mult),
                              reads=[pst[bu], t[("sg", si)]], writes=[t[("aT", cc)]])
                    for jj in range(4):
                        j = c4 * 4 + jj
                        for hf2 in range(2):
                            b = next_ps()
                            for k in range(4):
                                sy.op("tensor", lambda e, b=b, k=k, jj=jj, hf2=hf2: e.matmul(
                                    psb[b][:, :], lhsT=aT[:, k, jj * 128:(jj + 1) * 128], rhs=wdn[:, k, hf2 * 512:(hf2 + 1) * 512], start=(k == 0), stop=(k == 3)),
                                    reads=[t["wdn"], t[("aT", k)]], writes=[pst[b]])
                            sy.op("vector", lambda e, b=b, j=j, hf2=hf2, ex=ex: e.scalar_tensor_tensor(
                                out=yacc[:, j, hf2 * 512:(hf2 + 1) * 512], in0=psb[b][:, :], scalar=Wt[:, hh * 16 + j, ex:ex + 1],
                                in1=yacc[:, j, hf2 * 512:(hf2 + 1) * 512], op0=ALU.mult, op1=ALU.add),
                                reads=[pst[b], tH[("Wt", hh * 16 + j)]], writes=[t[("yacc", j)]])
            for j in range(16):
                i = hh * 16 + j
                hb_ = hz[j % 2]
                hk = t[("hz", j % 2)]
                sy.dma("sync", hb_[:], h1_t[i, :, :], reads=[tH[("h1d", i)]], writes=[hk], stream="h1r")
                sy.op("vector", lambda e, j=j, hb_=hb_: e.scalar_tensor_tensor(out=yacc[:, j, :], in0=hb_[:], scalar=ALPHA, in1=yacc[:, j, :], op0=ALU.mult, op1=ALU.add),
                      reads=[hk], writes=[t[("yacc", j)]])
                layer_norm(t, yacc[:, j, :], t[("yacc", j)], lnrep, hb_, hk, st6, mv)
                sy.dma("sync", out_t[i, :, :], hb_[:], reads=[hk], stream="o")
        sy.barrier()
        es.close()

    sy.new_epoch()
    if not dbg.get("skipD"):
        stage_D()
    sy.barrier()
    S = sy.dsem.get("o")
    if S is not None:
        nc.sync.wait_ge(S["sem"], 16 * S["cnt"])
    return P


def make_core_map(inputs, W, b, hf, names):
    x = np.asarray(inputs["x"][b], dtype=np.float32)
    if hf == 1:
        xin = x
    else:
        xin = np.concatenate([np.zeros((OWN, D), np.float32), x[:OWN]], axis=0)
    m = {"xin": np.ascontiguousarray(xin)}
    m.update(W)
    m.update(make_consts(hf))
    return {k: m[k] for k in names}


_PROG = None


def kernel(**inputs):
    global _PROG
    inputs = {k: np.asarray(v) for k, v in inputs.items()}
    if _PROG is None:
        _PROG = build_program()
    P = _PROG
    W = weight_layouts(inputs)
    names = list(P.ins)
    consts = [make_consts(0), make_consts(1)]
    maps = []
    for c in range(8):
        b, hf = c // 2, c % 2
        x = np.asarray(inputs["x"][b], dtype=np.float32)
        if hf == 1:
            xin = x
        else:
            xin = np.concatenate([np.zeros((OWN, D), np.float32), x[:OWN]], axis=0)
        m = {"xin": np.ascontiguousarray(xin)}
        m.update(W)
        m.update(consts[hf])
        maps.append({k: m[k] for k in names})
    res = run_bass_kernel_spmd(P.nc, maps, core_ids=list(range(8)))
    out = np.zeros((NB, SEQ, D), np.float32)
    for c in range(8):
        b, hf = c // 2, c % 2
        out[b, hf * OWN:(hf + 1) * OWN] = np.asarray(res.results[c]["out"], dtype=np.float32)
    return out
```

```python
import contextlib
import numpy as np
import ml_dtypes
import concourse.bass as bass
import concourse.mybir as mybir
from concourse.bass_utils import run_bass_kernel_spmd
from concourse.alu_op_type import AluOpType as ALU

F32 = mybir.dt.float32
BF16 = mybir.dt.bfloat16
AF = mybir.ActivationFunctionType
AX = mybir.AxisListType

D = 1024
SEQ = 8192
NB = 4
OWN = 4096
NT = OWN // 128
HD = 64
DIL = ((128, 1), (512, 4), (2048, 16))
IN_COLS = 5656
C_AQ, C_AK, C_AV = 0, 768, 1536
C_BQ = 2304
C_BKV = 2816
C_BG = 3584
C_MG = 3608
NEG = -30000.0
ALPHA = 2.0 ** 0.25
LN_EPS = 1e-5
N_EXP = 32
D_EXP = 512

DEBUG = {}


class Trk:
    __slots__ = ("w", "r", "x")

    def __init__(self, x=False):
        self.w = None
        self.r = {}
        self.x = x


class TK:
    def __init__(self):
        self.d = {}

    def __getitem__(self, k):
        t = self.d.get(k)
        if t is None:
            t = self.d[k] = Trk()
        return t

    def all(self):
        return list(self.d.values())


class Sy:
    def __init__(self, nc):
        self.nc = nc
        self.eng = {}
        for name in ("tensor", "vector", "scalar", "gpsimd", "sync"):
            self.eng[name] = dict(e=getattr(nc, name), sem=nc.alloc_semaphore(f"s_{name}"), cnt=0, known={})
        self.dsem = {}
        self.ninst = 0
        self.epoch = 0

    def new_epoch(self):
        self.barrier()
        self.epoch += 1
        for name, E in self.eng.items():
            E["sem"] = self.nc.alloc_semaphore(f"s_{name}_{self.epoch}")
            E["cnt"] = 0
            E["known"] = {}
        self.dsem = {}

    def _wait(self, E, deps):
        best = {}
        for sem, val in deps:
            k = id(sem)
            if k not in best or best[k][1] < val:
                best[k] = (sem, val)
        for k, (sem, val) in best.items():
            if E["known"].get(k, 0) >= val:
                continue
            E["e"].wait_ge(sem, val)
            E["known"][k] = val
            self.ninst += 1

    def _deps(self, E, reads, writes, skip_own):
        deps = []
        for t in reads:
            if t.w is not None:
                deps.append(t.w)
        for t in writes:
            if t.w is not None:
                deps.append(t.w)
            deps.extend(t.r.values())
        if skip_own:
            deps = [d for d in deps if d[0] is not E["sem"]]
        return deps

    def op(self, name, fn, reads=(), writes=()):
        E = self.eng[name]
        if any(t.x for t in reads):
            writes = list(writes) + [t for t in reads if t.x]
            reads = [t for t in reads if not t.x]
        self._wait(E, self._deps(E, reads, writes, name == "tensor"))
        ins = fn(E["e"])
        E["cnt"] += 1
        ins.then_inc(E["sem"], 1)
        self.ninst += 1
        tok = (E["sem"], E["cnt"])
        for t in writes:
            t.w = tok
            t.r = {}
        for t in reads:
            t.r[id(E["sem"])] = tok
        return tok

    def dma(self, qname, out, in_, reads=(), writes=(), stream="d"):
        E = self.eng[qname]
        skey = (qname, stream)
        S = self.dsem.get(skey)
        if S is None:
            S = self.dsem[skey] = dict(sem=self.nc.alloc_semaphore(f"d_{qname}_{stream}_{self.epoch}"), cnt=0)
        self._wait(E, self._deps(E, reads, writes, False))
        ins = E["e"].dma_start(out=out, in_=in_)
        S["cnt"] += 1
        ins.then_inc(S["sem"], 16)
        self.ninst += 1
        tok = (S["sem"], 16 * S["cnt"])
        for t in writes:
            t.w = tok
            t.r = {}
        for t in reads:
            t.r[id(S["sem"])] = tok
        return tok

    def barrier(self):
        toks = [(E["sem"], E["cnt"]) for E in self.eng.values() if E["cnt"] > 0]
        toks += [(S["sem"], 16 * S["cnt"]) for S in self.dsem.values()]
        for E in self.eng.values():
            self._wait(E, [t for t in toks if t[0] is not E["sem"]])


def SSL(base, d):
    return slice(base, base + 127 * d + 1, d)


def _bf(a):
    return np.asarray(a, dtype=np.float32).astype(ml_dtypes.bfloat16)


def alibi(n):
    return np.exp2(-8.0 * np.arange(1, n + 1, dtype=np.float32) / n).astype(np.float32)


def make_consts(hf):
    c = {}
    c["ident"] = np.eye(128, dtype=np.float32)
    c["identb"] = _bf(np.eye(128))
    k = np.arange(128)[:, None]
    q = np.arange(128)[None, :]
    sl = alibi(12).reshape(3, 4)
    bm = np.zeros((128, 3, 4, 2, 128), np.float32)
    for g, (win, d) in enumerate(DIL):
        for h in range(4):
            dprev = (q - k + 128).astype(np.float32)
            dcur = (q - k).astype(np.float32)
            bm[:, g, h, 0, :] = np.where(dprev <= 128, -8.0 * sl[g, h] * d * dprev, 8.0 * NEG)
            bm[:, g, h, 1, :] = np.where(dcur >= 0, -8.0 * sl[g, h] * d * dcur, 8.0 * NEG)
    c["bmA"] = bm.reshape(128, -1)
    c["vflag"] = np.tile(np.array([[float(hf), 1.0]], np.float32), (128, 1))
    slb = alibi(8)
    u = np.arange(2 * OWN)
    c["kaug"] = _bf(np.stack([(u % 128) - 64.0, u // 128, np.ones_like(u)]).astype(np.float32))
    qa = np.zeros((3, 8, OWN), np.float32)
    qt = 32 + np.arange(OWN) // 128
    for h in range(8):
        qa[0, h] = 8.0 * slb[h]
        qa[1, h] = 1024.0 * slb[h]
        qa[2, h] = -1024.0 * slb[h] * qt
    c["qaug"] = _bf(qa.reshape(3, 8 * OWN))
    cup = np.arange(128)[:, None]
    mcb = np.zeros((128, 17, 128), np.float32)
    for idx in range(17):
        off = idx * 8 - 2
        mcb[:, idx, :] = np.where(cup <= off + (q + 1) // 16, 0.0, NEG)
    c["mcb"] = _bf(mcb.reshape(128, -1))
    cu = np.arange(512)
    cval = ((cu <= 510) & ((cu >= 256) | (hf == 1))).astype(np.float32)
    c["cvalid"] = np.ascontiguousarray(cval.reshape(4, 128).T)
    c["validrep"] = _bf(np.repeat(cval.reshape(4, 128).T[:, :, None], 128, axis=2).reshape(128, -1))
    jb = np.arange(128)
    ovl = ((16 * cu[:, None] < 64 * jb[None, :] + 64) & (16 * cu[:, None] + 32 > 64 * jb[None, :])).astype(np.float32)
    ovl = ovl * cval[:, None]
    c["ovl"] = _bf(ovl.reshape(4, 128, 128).transpose(1, 0, 2).reshape(128, -1))
    wd = np.zeros((128, 190), np.float32)
    qq = np.arange(128)[:, None]
    jj = np.arange(190)[None, :] - 62
    cur = 64 + (qq >= 64)
    wd = np.where(jj > cur, -1.0e9, np.where((jj == cur) | (jj == cur - 1), 1.0e4, 0.0)).astype(np.float32)
    c["wd"] = wd
    fb = np.zeros((128,), np.float32)
    if hf == 1:
        fb[0] = 1.0e4
    else:
        fb[:64] = -1.0e9
        fb[64] = 1.0e4
    c["fbvec"] = np.tile(fb[None, :], (128, 1)).astype(np.float32)
    ind = np.zeros((128, 64, 128), np.float32)
    for kt in range(64):
        ind[2 * kt, kt, 0:64] = 1.0
        ind[2 * kt + 1, kt, 64:128] = 1.0
    c["indbig"] = _bf(ind.reshape(128, -1))
    c["tri_le"] = _bf(np.where(k <= q, 0.0, NEG))
    c["tri_gt"] = _bf(np.where(k > q, 0.0, NEG))
    return c


def weight_layouts(inp):
    w = {}
    w["w_in"] = np.ascontiguousarray(inp["w_in"][0])
    for kv in ("k", "v"):
        w1 = np.asarray(inp[f"cmp_w1_{kv}"][0])
        w[f"w1r_{kv}"] = np.ascontiguousarray(w1.reshape(32, 64, 256).transpose(1, 0, 2).reshape(64, 32 * 256))
        w[f"posT_{kv}"] = np.ascontiguousarray(np.asarray(inp[f"cmp_pos_{kv}"][0]).T)
        w[f"w2_{kv}"] = np.ascontiguousarray(inp[f"cmp_w2_{kv}"][0])
    w["w_ba"] = np.ascontiguousarray(inp["w_branch_a"][0])
    w["w_bb"] = np.ascontiguousarray(inp["w_branch_b"][0])
    w["w_out"] = np.ascontiguousarray(inp["w_out"][0])
    ln = np.concatenate([np.asarray(inp[k][0]).reshape(1, D) for k in ("ln1_g", "ln1_b", "ln2_g", "ln2_b")], axis=1)
    w["lnrep"] = np.ascontiguousarray(np.broadcast_to(ln, (128, 4 * D))).astype(np.float32)
    wf = np.asarray(inp["w_fine"][0]).transpose(1, 0, 2).reshape(D, 32)
    w["w_router"] = np.ascontiguousarray(np.concatenate([np.asarray(inp["w_coarse"][0]), wf], axis=1)).astype(np.float32)
    br = np.concatenate([np.asarray(inp["b_coarse"][0]).reshape(1, 4), np.asarray(inp["b_fine"][0]).reshape(1, 32)], axis=1)
    w["b_router"] = np.ascontiguousarray(np.broadcast_to(br, (128, 36))).astype(np.float32)
    if "w_gate_up" in inp:
        w["w_gu"] = np.ascontiguousarray(inp["w_gate_up"][0])
        w["w_dn"] = np.ascontiguousarray(inp["w_down"][0])
    return w


class Prog:
    def __init__(self, dbg=None):
        self.dbg = dbg or {}
        self.nc = bass.Bass("TRN2", target_bir_lowering=False)
        self.sy = Sy(self.nc)
        self.ins = {}
        self.outs = {}

    def din(self, name, shape, dt=F32):
        ap = self.nc.dram_tensor(name, list(shape), dt, kind="ExternalInput").ap()
        self.ins[name] = ap
        return ap

    def dout(self, name, shape, dt=F32):
        ap = self.nc.dram_tensor(name, list(shape), dt, kind="ExternalOutput").ap()
        self.outs[name] = ap
        return ap

    def dscratch(self, name, shape, dt=F32):
        return self.nc.dram_tensor(name, list(shape), dt, kind="Internal").ap()


def build_program(dbg=None):
    P = Prog(dbg)
    nc, sy = P.nc, P.sy
    dbg = P.dbg
    xin = P.din("xin", [2 * OWN, D])
    w_in = P.din("w_in", [D, IN_COLS])
    ident_d = P.din("ident", [128, 128])
    identb_d = P.din("identb", [128, 128], BF16)
    bmA_d = P.din("bmA", [128, 3 * 4 * 2 * 128])
    vflag_d = P.din("vflag", [128, 2])
    out = P.dout("out", [OWN, D])
    cd = {}
    for nm, shp, dt in (("kaug", [3, 2 * OWN], BF16), ("qaug", [3, 8 * OWN], BF16), ("mcb", [128, 17 * 128], BF16),
                        ("cvalid", [128, 4], F32), ("validrep", [128, 512], BF16), ("ovl", [128, 512], BF16),
                        ("wd", [128, 190], F32), ("fbvec", [128, 128], F32), ("indbig", [128, 64 * 128], BF16),
                        ("tri_le", [128, 128], BF16), ("tri_gt", [128, 128], BF16),
                        ("w1r_k", [64, 32 * 256], F32), ("posT_k", [64, 32], F32), ("w2_k", [256, 64], F32),
                        ("w1r_v", [64, 32 * 256], F32), ("posT_v", [64, 32], F32), ("w2_v", [256, 64], F32),
                        ("w_ba", [256, D], F32), ("w_bb", [512, D], F32), ("w_out", [D, D], F32),
                        ("lnrep", [128, 4 * D], F32), ("w_router", [D, 36], F32), ("b_router", [128, 36], F32),
                        ("w_gu", [N_EXP, D, 2 * D_EXP], F32), ("w_dn", [N_EXP, D_EXP, D], F32)):
        if nm in ("w_gu", "w_dn") and dbg.get("skipD"):
            continue
        cd[nm] = P.din(nm, shp, dt)
    h1_d = P.dscratch("h1_scratch", [OWN, D])

    es_glob = contextlib.ExitStack()
    SB = lambda es, name, shape, dt: es.enter_context(nc.sbuf_tensor(name, list(shape), dt))
    psb = [es_glob.enter_context(nc.psum_tensor(f"ps{i}", [128, 512], F32)) for i in range(8)]
    pst = [Trk(True) for _ in range(8)]

    ident = SB(es_glob, "ident_s", [128, 128], F32)
    identb = SB(es_glob, "identb_s", [128, 128], BF16)
    vflag = SB(es_glob, "vflag_s", [128, 2], F32)
    tC = TK()
    sy.dma("sync", ident[:], ident_d[:, :], writes=[tC["ident"]], stream="c")
    sy.dma("sync", identb[:], identb_d[:, :], writes=[tC["identb"]], stream="c")
    sy.dma("sync", vflag[:], vflag_d[:, :], writes=[tC["vflag"]], stream="c")

    Wt = SB(es_glob, "Wt", [128, NT, 32], F32)
    tH = TK()
    es_y = contextlib.ExitStack()
    yaT = SB(es_y, "yaT", [128, 2, OWN], BF16)
    tYa = TK()

    def stage_A():
        es = contextlib.ExitStack()
        xs = [SB(es, f"A_xs{i}", [128, D], F32) for i in range(2)]
        xT = SB(es, "A_xT", [128, 8, 2048], BF16)
        wst = [SB(es, "A_wst0", [128, 2, 768], F32)] * 2
        wA = SB(es, "A_w", [128, 8, 768], BF16)
        bmA = SB(es, "A_bm", [128, 4, 2, 128], F32)
        Kp = [SB(es, f"A_Kp{g}", [64, 4, 128 * d], BF16) for g, (_, d) in enumerate(DIL)]
        Vp = [SB(es, f"A_Vp{g}", [128, d, 4, 128], BF16) for g, (_, d) in enumerate(DIL)]
        Kc = SB(es, "A_Kc", [64, 4, 2048], BF16)
        Qc = SB(es, "A_Qc", [64, 4, 2048], BF16)
        Vc = SB(es, "A_Vc", [128, 16, 4, 128], BF16)
        acc = SB(es, "A_acc", [128, 4, 2048], F32)
        PT = [SB(es, f"A_PT{i}", [128, 512], BF16) for i in range(3)]
        t = TK()
        ps_rot = [0]

        def next_ps():
            i = ps_rot[0]
            ps_rot[0] = (i + 1) % 8
            return i

        xin_t = xin.rearrange("(n p) d -> n p d", p=128)
        nload = [0]

        def load_xT(tile0, ntiles):
            for j in range(ntiles):
                s = nload[0] % 2
                nload[0] += 1
                sy.dma("sync", xs[s][:], xin_t[tile0 + j, :, :], writes=[t[("xs", s)]], stream=f"x{s}")
                for half in range(2):
                    b = next_ps()
                    for kk in range(4):
                        kc = half * 4 + kk
                        sy.op("tensor", lambda e, b=b, kk=kk, kc=kc, s=s: e.transpose(
                            out=psb[b][:, kk * 128:(kk + 1) * 128], in_=xs[s][:, kc * 128:(kc + 1) * 128], identity=ident[:]),
                            reads=[t[("xs", s)], tC["ident"]], writes=[pst[b]])
                    eng = "vector" if half == 0 else "scalar"
                    if eng == "vector":
                        sy.op("vector", lambda e, b=b, half=half, j=j: e.tensor_copy(
                            out=xT[:, half * 4:half * 4 + 4, j * 128:(j + 1) * 128],
                            in_=psb[b][:, :].rearrange("p (k c) -> p k c", k=4)),
                            reads=[pst[b]], writes=[t[("xT", j, half)]])
                    else:
                        sy.op("scalar", lambda e, b=b, half=half, j=j: e.copy(
                            out=xT[:, half * 4:half * 4 + 4, j * 128:(j + 1) * 128],
                            in_=psb[b][:, :].rearrange("p (k c) -> p k c", k=4)),
                            reads=[pst[b]], writes=[t[("xT", j, half)]])

        def load_wA(g):
            sy.dma("gpsimd", bmA[:].rearrange("p b c d -> p (b c d)"), bmA_d[:, g * 1024:(g + 1) * 1024], writes=[t["bm"]], stream="c")
            for kc2 in range(4):
                s = 0
                for part, c0 in enumerate((C_AQ, C_AK, C_AV)):
                    col = c0 + g * 256
                    sy.dma("gpsimd", wst[s][:, :, part * 256:(part + 1) * 256],
                           w_in[kc2 * 256:(kc2 + 1) * 256, col:col + 256].rearrange("(k p) c -> p k c", p=128),
                           writes=[t[("wst", s)]], stream=f"w{s}")
                sy.op("gpsimd", lambda e, s=s, kc2=kc2: e.tensor_copy(out=wA[:, kc2 * 2:kc2 * 2 + 2, :], in_=wst[s][:]),
                      reads=[t[("wst", s)]], writes=[t["wA"]])

        xT_all = [t[("xT", j, hh)] for j in range(16) for hh in range(2)]
        aslopes = alibi(12).reshape(3, 4)

        for sc in (-1, 0, 1):
            own = sc >= 0
            tile0 = 32 + sc * 16
            load_xT(tile0, 16)
            for g, (win, d) in enumerate(DIL):
                nblk = 16 // d
                load_wA(g)
                for which, dst, cbase in (("q", Qc, 0), ("k", Kc, 256)):
                    if which == "q" and not own:
                        continue
                    for h in range(4):
                        for nck in range(4):
                            b = next_ps()
                            for kc in range(8):
                                sy.op("tensor", lambda e, b=b, kc=kc, h=h, nck=nck, cbase=cbase: e.matmul(
                                    psb[b][0:64, :], lhsT=wA[:, kc, cbase + h * 64:cbase + (h + 1) * 64],
                                    rhs=xT[:, kc, nck * 512:(nck + 1) * 512], start=(kc == 0), stop=(kc == 7)),
                                    reads=[t["wA"]] + xT_all[nck * 8:nck * 8 + 8], writes=[pst[b]])
                            eng = "vector" if (h + nck) % 2 == 0 else "scalar"
                            if eng == "vector":
                                sy.op("vector", lambda e, b=b, dst=dst, h=h, nck=nck: e.tensor_copy(
                                    out=dst[:, h, nck * 512:(nck + 1) * 512], in_=psb[b][0:64, :]),
                                    reads=[pst[b]], writes=[t[(which, h)]])
                            else:
                                sy.op("scalar", lambda e, b=b, dst=dst, h=h, nck=nck: e.copy(
                                    out=dst[:, h, nck * 512:(nck + 1) * 512], in_=psb[b][0:64, :]),
                                    reads=[pst[b]], writes=[t[(which, h)]])
                for n in range(nblk):
                    for r in range(d):
                        ti = n * d + r
                        b = next_ps()
                        base = n * 128 * d + r
                        for kc in range(8):
                            sy.op("tensor", lambda e, b=b, kc=kc, base=base, d=d: e.matmul(
                                psb[b][:, 0:256], lhsT=xT[:, kc, SSL(base, d)] if d > 1 else xT[:, kc, base:base + 128],
                                rhs=wA[:, kc, 512:768], start=(kc == 0), stop=(kc == 7)),
                                reads=[t["wA"]] + xT_all, writes=[pst[b]])
                        sy.op("vector", lambda e, b=b, ti=ti: e.tensor_copy(
                            out=Vc[:, ti, :, 0:64], in_=psb[b][:, 0:256].rearrange("p (h e) -> p h e", h=4)),
                            reads=[pst[b]], writes=[t[("V", ti)]])
                        fcol = 1 if own else 0
                        sy.op("gpsimd", lambda e, ti=ti, fcol=fcol: e.tensor_copy(
                            out=Vc[:, ti, :, 64:128], in_=vflag[:, None, fcol:fcol + 1].to_broadcast([128, 4, 64])),
                            reads=[tC["vflag"]], writes=[t[("Vf", ti)]])
                if own:
                    pairs = [(r, n) for n in range(nblk) for r in range(d)]
                    units = [(h, p0, half) for h in range(4) for p0 in range(0, 16, 4) for half in range(2)]
                    pvbank = {}
                    ptidx = {}

                    def ksl_of(h, r, n, pc):
                        if pc == 1:
                            return (Kc[:, h, SSL(n * 128 * d + r, d)] if d > 1 else Kc[:, h, n * 128:(n + 1) * 128]), [t[("k", h)]]
                        if n > 0:
                            return (Kc[:, h, SSL((n - 1) * 128 * d + r, d)] if d > 1 else Kc[:, h, (n - 1) * 128:n * 128]), [t[("k", h)]]
                        return (Kp[g][:, h, SSL(r, d)] if d > 1 else Kp[g][:, h, 0:128]), [t[("Kp", g)]]

                    def vsl_of(h, r, n, pc):
                        if pc == 1:
                            return Vc[:, n * d + r, h, :], [t[("V", n * d + r)], t[("Vf", n * d + r)]]
                        if n > 0:
                            return Vc[:, (n - 1) * d + r, h, :], [t[("V", (n - 1) * d + r)], t[("Vf", (n - 1) * d + r)]]
                        return Vp[g][:, r, h, :], [t[("Vp", g)]]

                    def emit_SA(u):
                        h, p0, half = u
                        sb = next_ps()
                        sub = pairs[p0:p0 + 4][half * 2:half * 2 + 2]
                        first = True
                        for si, (r, n) in enumerate(sub):
                            qsl = Qc[:, h, SSL(n * 128 * d + r, d)] if d > 1 else Qc[:, h, n * 128:(n + 1) * 128]
                            for pc in range(2):
                                col = (si * 2 + pc) * 128
                                ksl, kr = ksl_of(h, r, n, pc)
                                sy.op("tensor", lambda e, sb=sb, col=col, ksl=ksl, qsl=qsl, first=first: e.matmul(
                                    psb[sb][:, col:col + 128], lhsT=ksl, rhs=qsl, start=first, stop=False, skip_group_check=True),
                                    reads=kr + [t[("q", h)]], writes=[pst[sb]])
                                first = False
                                sy.op("tensor", lambda e, sb=sb, col=col, h=h, pc=pc: e.matmul(
                                    psb[sb][:, col:col + 128], lhsT=ident[:], rhs=bmA[:, h, pc, :], start=False, stop=True, skip_group_check=True),
                                    reads=[tC["ident"], t["bm"]], writes=[pst[sb]])
                        return sb

                    pcount = [0]

                    def emit_restA(u, sb):
                        h, p0, half = u
                        grp = pairs[p0:p0 + 4]
                        sub = grp[half * 2:half * 2 + 2]
                        pi = pcount[0] % 3
                        pcount[0] += 1
                        pt, ptk = PT[pi], t[("PT", pi)]
                        if half == 0:
                            pvbank[(h, p0)] = next_ps()
                        pvb = pvbank[(h, p0)]
                        sy.op("scalar", lambda e, sb=sb, pt=pt: e.activation(out=pt[:], in_=psb[sb][:, :], func=AF.Exp, scale=0.125),
                              reads=[pst[sb]], writes=[ptk])
                        for si, (r, n) in enumerate(sub):
                            reg = (half * 2 + si) * 128
                            for pc in range(2):
                                col = (si * 2 + pc) * 128
                                vsl, vr = vsl_of(h, r, n, pc)
                                sy.op("tensor", lambda e, pvb=pvb, reg=reg, vsl=vsl, pt=pt, col=col, st=(half == 0 and si == 0 and pc == 0): e.matmul(
                                    psb[pvb][:, reg:reg + 128], lhsT=vsl, rhs=pt[:, col:col + 128], start=st, stop=True, skip_group_check=True),
                                    reads=vr + [ptk], writes=[pst[pvb]])
                        if half == 1:
                            r0, n0 = grp[0]
                            if d == 1:
                                dst = acc[:, h, n0 * 128:(n0 + 4) * 128]
                                src = psb[pvb][:, :]
                            else:
                                dst = acc[:, h, n0 * 128 * d:(n0 + 1) * 128 * d].rearrange("p (l r) -> p l r", r=d)[:, :, r0:r0 + 4]
                                src = psb[pvb][:, :].rearrange("p (r l) -> p l r", r=4)
                            if g == 0:
                                sy.op("vector", lambda e, dst=dst, src=src: e.tensor_copy(out=dst, in_=src),
                                      reads=[pst[pvb]], writes=[t[("acc", h)]])
                            else:
                                sy.op("vector", lambda e, dst=dst, src=src: e.tensor_tensor(out=dst, in0=dst, in1=src, op=ALU.add),
                                      reads=[pst[pvb]], writes=[t[("acc", h)]])

                    LOOKA = 2
                    pend = [emit_SA(u) for u in units[:LOOKA]]
                    for ui, u in enumerate(units):
                        if ui + LOOKA < len(units):
                            pend.append(emit_SA(units[ui + LOOKA]))
                        emit_restA(u, pend.pop(0))
                sy.op("gpsimd", lambda e, g=g, d=d: e.tensor_copy(out=Kp[g][:], in_=Kc[:, :, 2048 - 128 * d:2048]),
                      reads=[t[("k", h)] for h in range(4)], writes=[t[("Kp", g)]])
                sy.op("gpsimd", lambda e, g=g, d=d: e.tensor_copy(out=Vp[g][:], in_=Vc[:, 16 - d:16, :, :]),
                      reads=[t[("V", i)] for i in range(16)] + [t[("Vf", i)] for i in range(16)], writes=[t[("Vp", g)]])
            if own:
                for h in range(4):
                    hp, lo = h // 2, (h % 2) * 64
                    for s2 in range(2):
                        sy.op("vector", lambda e, h=h, s2=s2: e.reciprocal(out=xs[s2][0:64, :], in_=acc[64:128, h, s2 * 1024:(s2 + 1) * 1024]),
                              reads=[t[("acc", h)]], writes=[t[("xs", s2)]])
                        sy.op("vector", lambda e, h=h, hp=hp, lo=lo, s2=s2: e.tensor_tensor(
                            out=yaT[lo:lo + 64, hp, sc * 2048 + s2 * 1024:sc * 2048 + (s2 + 1) * 1024],
                            in0=acc[0:64, h, s2 * 1024:(s2 + 1) * 1024], in1=xs[s2][0:64, :], op=ALU.mult),
                            reads=[t[("acc", h)], t[("xs", s2)]], writes=[tYa[(hp, sc, lo, s2)]])
        sy.barrier()
        es.close()

    if not dbg.get("skipA"):
        stage_A()
    else:
        sy.op("gpsimd", lambda e: e.memset(yaT[:], 0.0), writes=[tYa["z"]])

    if "yaT" in dbg:
        o = P.dout("dbg_yaT", [128, 2 * OWN], BF16)
        sy.dma("sync", o[:, :], yaT[:].rearrange("p a b -> p (a b)"), reads=tYa.all(), stream="o")


    ybT = SB(es_y, "ybT", [128, 4, OWN], BF16)
    tYb = TK()

    def stage_B():
        es = contextlib.ExitStack()
        t = TK()
        xin_t = xin.rearrange("(n p) d -> n p d", p=128)
        ps_rot = [0]

        def next_ps(lo=0, hi=8):
            i = ps_rot[0]
            ps_rot[0] = i + 1
            return lo + i % (hi - lo)

        xs = [SB(es, "B_xs0", [128, D], F32)] * 2
        nload = [0]

        ps_hi = [8]

        def emit_xT(tile_u, dst, j, key):
            sI = 0
            nload[0] += 1
            sy.dma("sync", xs[sI][:], xin_t[tile_u, :, :], writes=[t[("xs", sI)]], stream=f"x{sI}")
            for half in range(2):
                b = next_ps(0, ps_hi[0])
                for kk in range(4):
                    kc = half * 4 + kk
                    sy.op("tensor", lambda e, b=b, kk=kk, kc=kc, sI=sI: e.transpose(
                        out=psb[b][:, kk * 128:(kk + 1) * 128], in_=xs[sI][:, kc * 128:(kc + 1) * 128], identity=ident[:]),
                        reads=[t[("xs", sI)], tC["ident"]], writes=[pst[b]])
                src = psb[b][:, :].rearrange("p (k c) -> p k c", k=4)
                o = dst[:, half * 4:half * 4 + 4, j * 128:(j + 1) * 128]
                if half == 0:
                    sy.op("vector", lambda e, o=o, src=src: e.tensor_copy(out=o, in_=src), reads=[pst[b]], writes=[t[(key, j, half)]])
                else:
                    sy.op("scalar", lambda e, o=o, src=src: e.copy(out=o, in_=src), reads=[pst[b]], writes=[t[(key, j, half)]])

        wst = SB(es, "B_wst", [128, 2, 768], F32)

        def load_w(dst, c0, ncol, key):
            for kc2 in range(4):
                sy.dma("gpsimd", wst[:, :, 0:ncol], w_in[kc2 * 256:(kc2 + 1) * 256, c0:c0 + ncol].rearrange("(k p) c -> p k c", p=128),
                       writes=[t["wst"]], stream="w0")
                sy.op("gpsimd", lambda e, kc2=kc2: e.tensor_copy(out=dst[:, kc2 * 2:kc2 * 2 + 2, :], in_=wst[:, :, 0:ncol]),
                      reads=[t["wst"]], writes=[t[key]])

        slcK = SB(es, "B_slcK", [67, 2, 2 * OWN], BF16)
        winK = SB(es, "B_winK", [67, 2, 36 * 128], BF16)
        slcV = SB(es, "B_slcV", [128, 64, 2, 66], BF16)
        winV = SB(es, "B_winV", [128, 36, 2, 66], BF16)
        KcT = SB(es, "B_KcT", [64, 2, 512], BF16)
        Vcm = SB(es, "B_Vcm", [128, 4, 2, 64], BF16)
        for g in range(2):
            sy.dma("gpsimd", slcK[64:67, g, :], cd["kaug"][:, :], writes=[t[("slcKaug", g)]], stream="c")
            sy.dma("gpsimd", winK[64:67, g, :], cd["kaug"][:, 28 * 128:], writes=[t[("winKaug", g)]], stream="c")
        sy.op("gpsimd", lambda e: e.tensor_copy(out=slcV[:, 0:32, :, 64:66], in_=vflag[:, None, None, 0:1].to_broadcast([128, 32, 2, 2])),
              reads=[tC["vflag"]], writes=[t["slcVf"]])
        sy.op("gpsimd", lambda e: e.tensor_copy(out=slcV[:, 32:64, :, 64:66], in_=vflag[:, None, None, 1:2].to_broadcast([128, 32, 2, 2])),
              reads=[tC["vflag"]], writes=[t["slcVf"]])
        sy.op("gpsimd", lambda e: e.tensor_copy(out=winV[:, 0:4, :, 64:66], in_=vflag[:, None, None, 0:1].to_broadcast([128, 4, 2, 2])),
              reads=[tC["vflag"]], writes=[t["winVf"]])
        sy.op("gpsimd", lambda e: e.tensor_copy(out=winV[:, 4:36, :, 64:66], in_=vflag[:, None, None, 1:2].to_broadcast([128, 32, 2, 2])),
              reads=[tC["vflag"]], writes=[t["winVf"]])

        es1 = contextlib.ExitStack()
        raw = SB(es1, "B_raw", [128, 2, 2 * OWN], BF16)
        es1b = contextlib.ExitStack()
        wB = SB(es1b, "B_wB", [128, 8, 768], BF16)
        xTc = [SB(es1b, f"B_xTc{i}", [128, 8, 512], BF16) for i in range(2)]
        load_w(wB, C_BKV, 768, "wB")
        for ch in range(16):
            xb_ = xTc[ch % 2]
            xk = ("xTc", ch % 2)
            for j in range(4):
                emit_xT(ch * 4 + j, xb_, j, xk)
            xr = [t[(xk, j, hh)] for j in range(4) for hh in range(2)]
            for (ii, dst, off, key) in () if dbg.get("noFM") else ((0, raw, 0, "rawK"), (1, raw, 0, "rawV"), (2, slcK, 0, "slcK"), (4, winK, -28 * 128, "winK")):
                if ii == 4 and ch < 7:
                    continue
                for g in range(2):
                    b = next_ps()
                    c0 = ii * 128 + g * 64
                    for kc in range(8):
                        sy.op("tensor", lambda e, b=b, kc=kc, c0=c0, xb_=xb_: e.matmul(
                            psb[b][0:64, :], lhsT=wB[:, kc, c0:c0 + 64], rhs=xb_[:, kc, :], start=(kc == 0), stop=(kc == 7)),
                            reads=[t["wB"]] + xr, writes=[pst[b]])
                    pb = 64 if ii == 1 else 0
                    o = dst[pb:pb + 64, g, ch * 512 + off:ch * 512 + off + 512]
                    if g == 0:
                        sy.op("vector", lambda e, o=o, b=b: e.tensor_copy(out=o, in_=psb[b][0:64, :]), reads=[pst[b]], writes=[t[(key, g, ch)]])
                    else:
                        sy.op("scalar", lambda e, o=o, b=b: e.copy(out=o, in_=psb[b][0:64, :]), reads=[pst[b]], writes=[t[(key, g, ch)]])
            for j in range(0 if dbg.get("noTM") else 4):
                tu = ch * 4 + j
                b = next_ps()
                for kc in range(8):
                    sy.op("tensor", lambda e, b=b, kc=kc, j=j, xb_=xb_: e.matmul(
                        psb[b][:, 0:384], lhsT=xb_[:, kc, j * 128:(j + 1) * 128], rhs=wB[:, kc, 384:768], start=(kc == 0), stop=(kc == 7)),
                        reads=[t["wB"]] + xr, writes=[pst[b]])
                sy.op("vector", lambda e, b=b, tu=tu: e.tensor_copy(
                    out=slcV[:, tu, :, 0:64], in_=psb[b][:, 0:128].rearrange("p (g e) -> p g e", g=2)),
                    reads=[pst[b]], writes=[t[("slcV", tu)]])
                if tu >= 28:
                    sy.op("scalar", lambda e, b=b, tu=tu: e.copy(
                        out=winV[:, tu - 28, :, 0:64], in_=psb[b][:, 256:384].rearrange("p (g e) -> p g e", g=2)),
                        reads=[pst[b]], writes=[t[("winV", tu - 28)]])
        sy.barrier()
        es1b.close()
        if dbg.get("stopB1"):
            if "B1" in dbg and not dbg.get("noOut"):
                o3 = P.dout("dbg_slcK", [67, 2 * 2 * OWN], BF16)
                sy.dma("sync", o3[:, :], slcK[:].rearrange("p a b -> p (a b)"), stream="o")
                o4 = P.dout("dbg_winV", [128, 36 * 2 * 66], BF16)
                sy.dma("sync", o4[:, :], winV[:].rearrange("p a b c -> p (a b c)"), stream="o")
                o5 = P.dout("dbg_raw", [128, 2 * 2 * OWN], BF16)
                sy.dma("sync", o5[:, :], raw[:].rearrange("p a b -> p (a b)"), stream="o")
            sy.barrier()
            es1.close()
            es.close()
            return
        es2 = contextlib.ExitStack()
        w1r = SB(es2, "B_w1r", [128, 32, 256], BF16)
        w1st = SB(es2, "B_w1st", [128, 4, 256], F32)
        posT = SB(es2, "B_posT", [128, 32], F32)
        posTb = SB(es2, "B_posTb", [128, 32], BF16)
        w2s = SB(es2, "B_w2s", [128, 2, 64], F32)
        w2b = SB(es2, "B_w2b", [128, 2, 64], BF16)
        hb = SB(es2, "B_hb", [128, 2], F32)
        h1 = SB(es2, "B_h1", [128, 512], F32)
        h1x = SB(es2, "B_h1x", [128, 512], F32)
        h1T = SB(es2, "B_h1T", [128, 2, 512], BF16)
        cvalid = SB(es2, "B_cvalid", [128, 4], F32)
        sy.dma("sync", cvalid[:], cd["cvalid"][:, :], writes=[t["cvalid"]], stream="c")
        sy.op("gpsimd", lambda e: e.memset(h1T[:], 0.0), writes=[t["h1T"]])
        for kv, PB in (("k", 0), ("v", 64)):
            for pp in range(8):
                sy.dma("sync", w1st[PB:PB + 64].rearrange("e p h -> e (p h)"), cd[f"w1r_{kv}"][:, pp * 1024:(pp + 1) * 1024], writes=[t["w1st"]], stream="c2")
                sy.op("gpsimd", lambda e, pp=pp, PB=PB: e.tensor_copy(out=w1r[PB:PB + 64, pp * 4:pp * 4 + 4, :], in_=w1st[PB:PB + 64]), reads=[t["w1st"]], writes=[t["w1r"]])
            sy.dma("sync", posT[PB:PB + 64, :], cd[f"posT_{kv}"][:, :], writes=[t["posT"]], stream="c2")
            sy.op("gpsimd", lambda e, PB=PB: e.tensor_copy(out=posTb[PB:PB + 64, :], in_=posT[PB:PB + 64, :]), reads=[t["posT"]], writes=[t["posTb"]])
            sy.dma("sync", w2s[:], cd[f"w2_{kv}"].rearrange("(c p) e -> p c e", p=128), writes=[t["w2s"]], stream="c2")
            sy.op("gpsimd", lambda e: e.tensor_copy(out=w2b[:], in_=w2s[:]), reads=[t["w2s"]], writes=[t["w2b"]])
            b = next_ps()
            for hc in range(2):
                for p in range(32):
                    sy.op("tensor", lambda e, b=b, hc=hc, p=p, PB=PB: e.matmul(
                        psb[b][:, hc:hc + 1], lhsT=w1r[PB:PB + 64, p, hc * 128:(hc + 1) * 128], rhs=posTb[PB:PB + 64, p:p + 1],
                        start=(p == 0 and hc == 0), stop=(p == 31), skip_group_check=True),
                        reads=[t["w1r"], t["posTb"]], writes=[pst[b]])
            sy.op("vector", lambda e, b=b: e.tensor_copy(out=hb[:], in_=psb[b][:, 0:2]), reads=[pst[b]], writes=[t["hb"]])
            rawr = [t[("rawK" if kv == "k" else "rawV", g, ch)] for g in range(2) for ch in range(16)]
            for g in range(2):
                for hc in range(2):
                    b = next_ps()
                    for p in range(32):
                        sy.op("tensor", lambda e, b=b, hc=hc, p=p, g=g, PB=PB: e.matmul(
                            psb[b][:, 0:511], lhsT=w1r[PB:PB + 64, p, hc * 128:(hc + 1) * 128], rhs=raw[PB:PB + 64, g, slice(p, p + 16 * 510 + 1, 16)],
                            start=(p == 0), stop=(p == 31)),
                            reads=[t["w1r"]] + rawr, writes=[pst[b]])
                    sy.op("vector", lambda e, b=b, hc=hc: e.tensor_scalar(out=h1[:, 0:511], in0=psb[b][:, 0:511], scalar1=hb[:, hc:hc + 1], scalar2=None, op0=ALU.add),
                          reads=[pst[b], t["hb"]], writes=[t["h1"]])
                    sy.op("vector", lambda e: e.tensor_tensor(out=h1x[:, 0:511], in0=h1[:, 0:511], in1=h1[:, 0:511], op=ALU.mult),
                          reads=[t["h1"]], writes=[t["h1x"]])
                    sy.op("vector", lambda e: e.tensor_scalar(out=h1x[:, 0:511], in0=h1x[:, 0:511], scalar1=0.044715, scalar2=1.0, op0=ALU.mult, op1=ALU.add),
                          reads=[t["h1x"]], writes=[t["h1x"]])
                    sy.op("vector", lambda e: e.tensor_tensor(out=h1x[:, 0:511], in0=h1x[:, 0:511], in1=h1[:, 0:511], op=ALU.mult),
                          reads=[t["h1"], t["h1x"]], writes=[t["h1x"]])
                    sy.op("scalar", lambda e: e.activation(out=h1x[:, 0:511], in_=h1x[:, 0:511], func=AF.Sigmoid, scale=1.5957691216057308),
                          reads=[t["h1x"]], writes=[t["h1x"]])
                    sy.op("vector", lambda e, hc=hc: e.tensor_tensor(out=h1T[:, hc, 0:511], in0=h1x[:, 0:511], in1=h1[:, 0:511], op=ALU.mult),
                          reads=[t["h1"], t["h1x"]], writes=[t["h1T"]])
                if kv == "k":
                    b = next_ps()
                    for hc in range(2):
                        sy.op("tensor", lambda e, b=b, hc=hc: e.matmul(psb[b][0:64, :], lhsT=w2b[:, hc, :], rhs=h1T[:, hc, :], start=(hc == 0), stop=(hc == 1)),
                              reads=[t["w2b"], t["h1T"]], writes=[pst[b]])
                    sy.op("vector", lambda e, b=b, g=g: e.tensor_copy(out=KcT[:, g, :], in_=psb[b][0:64, :]), reads=[pst[b]], writes=[t[("KcT", g)]])
                else:
                    b = next_ps()
                    for ct in range(4):
                        for hc in range(2):
                            sy.op("tensor", lambda e, b=b, hc=hc, ct=ct: e.matmul(
                                psb[b][:, ct * 64:(ct + 1) * 64], lhsT=h1T[:, hc, ct * 128:(ct + 1) * 128], rhs=w2b[:, hc, :],
                                start=(hc == 0 and ct == 0), stop=(hc == 1), skip_group_check=True),
                                reads=[t["w2b"], t["h1T"]], writes=[pst[b]])
                    for ct in range(4):
                        sy.op("vector", lambda e, b=b, g=g, ct=ct: e.tensor_scalar(
                            out=Vcm[:, ct, g, :], in0=psb[b][:, ct * 64:(ct + 1) * 64], scalar1=cvalid[:, ct:ct + 1], scalar2=None, op0=ALU.mult),
                            reads=[pst[b], t["cvalid"]], writes=[t[("Vcm", g)]])
        sy.barrier()
        es2.close()
        es1.close()
        if "B2" in dbg:
            o1 = P.dout("dbg_KcT", [64, 1024], BF16)
            sy.dma("sync", o1[:, :], KcT[:].rearrange("p a b -> p (a b)"), reads=t.all(), stream="o")
            o2 = P.dout("dbg_Vcm", [128, 512], BF16)
            sy.dma("sync", o2[:, :], Vcm[:].rearrange("p a b c -> p (a b c)"), reads=t.all(), stream="o")
            o3 = P.dout("dbg_slcK", [67, 2 * 2 * OWN], BF16)
            sy.dma("sync", o3[:, :], slcK[:].rearrange("p a b -> p (a b)"), reads=t.all(), stream="o")
            o4 = P.dout("dbg_winV", [128, 36 * 2 * 66], BF16)
            sy.dma("sync", o4[:, :], winV[:].rearrange("p a b c -> p (a b c)"), reads=t.all(), stream="o")
        if dbg.get("stopB2"):
            sy.barrier()
            es.close()
            return

        ps_hi[0] = 6
        wq = SB(es, "B_wq", [128, 8, 512], BF16)
        wg = SB(es, "B_wg", [128, 8, 24], BF16)
        load_w(wq, C_BQ, 512, "wq")
        load_w(wg, C_BG, 24, "wg")
        cs = {}
        for nm, shp, dt in (("mcb", [128, 17, 128], BF16), ("validrep", [128, 4, 128], BF16), ("ovl", [128, 4, 128], BF16),
                            ("wd", [128, 190], F32), ("fbvec", [128, 128], F32), ("indbig", [128, 64, 128], BF16),
                            ("tri_le", [128, 128], BF16), ("tri_gt", [128, 128], BF16)):
            cs[nm] = SB(es, "Bc_" + nm, shp, dt)
            dst = cs[nm][:]
            if len(shp) == 3:
                dst = dst.rearrange("p a b -> p (a b)")
            sy.dma("sync", dst, cd[nm][:, :], writes=[t["c_" + nm]], stream="c")
        xT1 = [SB(es, f"B_xT1{i}", [128, 8, 128], BF16) for i in range(2)]
        qTa = [SB(es, f"B_qTa{i}", [67, 8, 128], BF16) for i in range(2)]
        gates = [SB(es, f"B_gates{i}", [128, 24], F32) for i in range(2)]
        PcT = SB(es, "B_PcT", [128, 4, 512], BF16)
        Pn = SB(es, "B_Pn", [128, 4, 512], BF16)
        rden = SB(es, "B_rden", [128, 512], F32)
        score = SB(es, "B_score", [128, 128], F32)
        work = SB(es, "B_work", [128, 128], F32)
        pen = SB(es, "B_pen", [128, 128], F32)
        pen2 = SB(es, "B_pen2", [128, 128], F32)
        m8a = SB(es, "B_m8a", [128, 8], F32)
        m8b = SB(es, "B_m8b", [128, 8], F32)
        penT = [SB(es, f"B_penT{g}", [128, 4, 128], BF16) for g in range(2)]
        PT = [SB(es, f"B_PT{i}", [128, 512], BF16) for i in range(3)]
        oc_sb = SB(es, "B_oc", [128, 2, 4, 64], F32)
        os_sb = SB(es, "B_os", [128, 2, 4, 66], F32)
        ow_sb = SB(es, "B_ow", [128, 2, 4, 66], F32)
        rs = SB(es, "B_rs", [128, 2, 4, 2], F32)
        yb_tm = SB(es, "B_ybtm", [128, 512], F32)
        ytmp = SB(es, "B_ytmp", [128, 64], F32)
        OSB, OWB = 6, 7
        qaug3 = cd["qaug"].rearrange("r (h n) -> r h n", h=8)
        ptc = [0]

        def attn_tiles(i, g, kts, Ksrc, koff, Vsrc, accb, with_pen, qa, qk):
            kts = list(kts)
            LOOK = 2

            def emit_S(kt):
                sb = next_ps(0, 6)
                kl = kt - koff
                sy.op("tensor", lambda e, sb=sb, kl=kl: e.matmul(
                    psb[sb][:, :], lhsT=Ksrc[0:67, g, kl * 128:(kl + 1) * 128], rhs=qa[0:67, 4 * g:4 * g + 4, :], start=True, stop=False),
                    reads=[qk[0], qk[1]], writes=[pst[sb]])
                adds = []
                if with_pen:
                    adds.append((cs["indbig"][:, kt, :], penT[g][:], [t["c_indbig"], t[("penT", g)]]))
                if kt == 32 + i:
                    adds.append((identb[:], cs["tri_le"][:, None, :].to_broadcast([128, 4, 128]), [tC["identb"], t["c_tri_le"]]))
                if (not with_pen) and kt == 28 + i:
                    adds.append((identb[:], cs["tri_gt"][:, None, :].to_broadcast([128, 4, 128]), [tC["identb"], t["c_tri_gt"]]))
                for (l_, r_, rd) in adds:
                    sy.op("tensor", lambda e, sb=sb, l_=l_, r_=r_: e.matmul(psb[sb][:, :], lhsT=l_, rhs=r_, start=False, stop=True),
                          reads=rd, writes=[pst[sb]])
                return sb

            def emit_rest(kt, sb, first):
                kl = kt - koff
                pi = ptc[0] % 3
                ptc[0] += 1
                sy.op("scalar", lambda e, sb=sb, pi=pi: e.activation(out=PT[pi][:], in_=psb[sb][:, :], func=AF.Exp, scale=0.125),
                      reads=[pst[sb]], writes=[t[("PT", pi)]])
                for r in range(4):
                    sy.op("tensor", lambda e, r=r, pi=pi, kl=kl, st=(first and r == 0): e.matmul(
                        psb[accb][:, r * 66:(r + 1) * 66], lhsT=PT[pi][:, r * 128:(r + 1) * 128], rhs=Vsrc[:, kl, g, :],
                        start=st, stop=True, skip_group_check=True),
                        reads=[t[("PT", pi)]], writes=[pst[accb]])

            pend = [emit_S(kt) for kt in kts[:LOOK]]
            for n, kt in enumerate(kts):
                if n + LOOK < len(kts):
                    pend.append(emit_S(kts[n + LOOK]))
                emit_rest(kt, pend.pop(0), n == 0)

        for i in range(NT):
            xb_ = xT1[i % 2]
            xk = ("xT1", i % 2)
            emit_xT(32 + i, xb_, 0, xk)
            xr = [t[(xk, 0, 0)], t[(xk, 0, 1)]]
            qa = qTa[i % 2]
            qk = (t[("qTa", i % 2)], t[("qTaug", i % 2)])
            sy.dma("gpsimd", qa[64:67, :, :], qaug3[:, :, i * 128:(i + 1) * 128], writes=[qk[1]], stream="qa")
            for g in range(2):
                b = next_ps(0, 6)
                for r in range(4):
                    hh = 4 * g + r
                    for kc in range(8):
                        sy.op("tensor", lambda e, b=b, r=r, hh=hh, kc=kc, xb_=xb_: e.matmul(
                            psb[b][0:64, r * 128:(r + 1) * 128], lhsT=wq[:, kc, hh * 64:(hh + 1) * 64], rhs=xb_[:, kc, :],
                            start=(kc == 0 and r == 0), stop=(kc == 7), skip_group_check=True),
                            reads=[t["wq"]] + xr, writes=[pst[b]])
                sy.op("vector", lambda e, b=b, g=g, qa=qa: e.tensor_copy(
                    out=qa[0:64, 4 * g:4 * g + 4, :], in_=psb[b][0:64, :].rearrange("p (r n) -> p r n", r=4)),
                    reads=[pst[b]], writes=[qk[0]])
            b = next_ps(0, 6)
            for kc in range(8):
                sy.op("tensor", lambda e, b=b, kc=kc, xb_=xb_: e.matmul(psb[b][:, 0:24], lhsT=xb_[:, kc, :], rhs=wg[:, kc, :], start=(kc == 0), stop=(kc == 7)),
                      reads=[t["wg"]] + xr, writes=[pst[b]])
            gt = gates[i % 2]
            gk = t[("gates", i % 2)]
            sy.op("scalar", lambda e, b=b, gt=gt: e.activation(out=gt[:], in_=psb[b][:, 0:24], func=AF.Sigmoid), reads=[pst[b]], writes=[gk])
            for g in range(2):
                ctmax = (262 + 8 * i) // 128
                ncts = ctmax + 1
                for ct in range(ncts):
                    sb = next_ps(0, 6)
                    off = 254 + 8 * i - 128 * ct
                    need_mask = off < 127
                    sy.op("tensor", lambda e, sb=sb, ct=ct, nm=need_mask: e.matmul(
                        psb[sb][:, :], lhsT=KcT[:, g, ct * 128:(ct + 1) * 128], rhs=qa[0:64, 4 * g:4 * g + 4, :], start=True, stop=(not nm)),
                        reads=[t[("KcT", g)], qk[0]], writes=[pst[sb]])
                    if need_mask:
                        idx = (off + 2) // 8
                        assert 0 <= idx < 17, (i, ct, off)
                        sy.op("tensor", lambda e, sb=sb, idx=idx: e.matmul(
                            psb[sb][:, :], lhsT=identb[:], rhs=cs["mcb"][:, idx:idx + 1, :].to_broadcast([128, 4, 128]), start=False, stop=True),
                            reads=[tC["identb"], t["c_mcb"]], writes=[pst[sb]])
                    sy.op("scalar", lambda e, sb=sb, ct=ct: e.activation(out=PcT[:, ct, :], in_=psb[sb][:, :], func=AF.Exp, scale=0.125),
                          reads=[pst[sb]], writes=[t[("PcT", ct)]])
                db = next_ps(0, 6)
                for ct in range(ncts):
                    sy.op("tensor", lambda e, db=db, ct=ct: e.matmul(psb[db][:, :], lhsT=cs["validrep"][:, ct, :], rhs=PcT[:, ct, :], start=(ct == 0), stop=(ct == ncts - 1)),
                          reads=[t["c_validrep"], t[("PcT", ct)]], writes=[pst[db]])
                sy.op("vector", lambda e, db=db: e.tensor_scalar(out=rden[:], in0=psb[db][:, :], scalar1=1e-30, scalar2=None, op0=ALU.max),
                      reads=[pst[db]], writes=[t["rden"]])
                sy.op("vector", lambda e: e.reciprocal(out=rden[:], in_=rden[:]), reads=[t["rden"]], writes=[t["rden"]])
                for ct in range(ncts):
                    sy.op("vector" if ct % 2 == 0 else "gpsimd", lambda e, ct=ct: e.tensor_tensor(out=Pn[:, ct, :], in0=PcT[:, ct, :], in1=rden[:], op=ALU.mult),
                          reads=[t[("PcT", ct)], t["rden"]], writes=[t[("Pn", ct)]])
                ob = next_ps(0, 6)
                firstm = True
                for r in range(4):
                    for ct in range(ncts):
                        sy.op("tensor", lambda e, ob=ob, r=r, ct=ct, st=firstm: e.matmul(
                            psb[ob][:, r * 64:(r + 1) * 64], lhsT=Pn[:, ct, r * 128:(r + 1) * 128], rhs=Vcm[:, ct, g, :],
                            start=st, stop=True, skip_group_check=True),
                            reads=[t[("Pn", ct)], t[("Vcm", g)]], writes=[pst[ob]])
                        firstm = False
                for r in range(4):
                    for ct in range(ncts):
                        sy.op("tensor", lambda e, ob=ob, r=r, ct=ct: e.matmul(
                            psb[ob][:, 256:384], lhsT=Pn[:, ct, r * 128:(r + 1) * 128], rhs=cs["ovl"][:, ct, :],
                            start=False, stop=True, skip_group_check=True),
                            reads=[t[("Pn", ct)], t["c_ovl"]], writes=[pst[ob]])
                sy.op("scalar", lambda e, ob=ob, g=g: e.copy(out=oc_sb[:, g, :, :], in_=psb[ob][:, 0:256].rearrange("p (r e) -> p r e", r=4)),
                      reads=[pst[ob]], writes=[t[("oc", g)]])
                sy.op("vector", lambda e, ob=ob: e.tensor_tensor(out=score[:], in0=psb[ob][:, 256:384], in1=cs["wd"][:, 62 - 2 * i:190 - 2 * i], op=ALU.add),
                      reads=[pst[ob], t["c_wd"]], writes=[t["score"]])
                sy.op("vector", lambda e: e.tensor_tensor(out=score[:], in0=score[:], in1=cs["fbvec"][:], op=ALU.add),
                      reads=[t["c_fbvec"]], writes=[t["score"]])
                sy.op("vector", lambda e: e.max(out=m8a[:], in_=score[:]), reads=[t["score"]], writes=[t["m8a"]])
                sy.op("vector", lambda e: e.match_replace(out=work[:], in_to_replace=m8a[:], in_values=score[:], imm_value=-3.0e38),
                      reads=[t["score"], t["m8a"]], writes=[t["work"]])
                sy.op("vector", lambda e: e.max(out=m8b[:], in_=work[:]), reads=[t["work"]], writes=[t["m8b"]])
                sy.op("vector", lambda e: e.tensor_scalar(out=pen[:], in0=score[:], scalar1=m8b[:, 7:8], scalar2=NEG, op0=ALU.is_lt, op1=ALU.mult),
                      reads=[t["score"], t["m8b"]], writes=[t["pen"]])
                sy.op("vector", lambda e: e.tensor_scalar(out=pen2[:], in0=score[:], scalar1=-5.0e8, scalar2=NEG, op0=ALU.is_lt, op1=ALU.mult),
                      reads=[t["score"]], writes=[t["pen2"]])
                sy.op("vector", lambda e: e.tensor_tensor(out=pen[:], in0=pen[:], in1=pen2[:], op=ALU.min),
                      reads=[t["pen2"]], writes=[t["pen"]])
                tb = next_ps(0, 6)
                sy.op("tensor", lambda e, tb=tb: e.transpose(out=psb[tb][:, 0:128], in_=pen[:], identity=ident[:]),
                      reads=[t["pen"], tC["ident"]], writes=[pst[tb]])
                sy.op("vector", lambda e, tb=tb, g=g: e.tensor_copy(out=penT[g][:], in_=psb[tb][:, None, 0:128].to_broadcast([128, 4, 128])),
                      reads=[pst[tb]], writes=[t[("penT", g)]])
                attn_tiles(i, g, range(28 + i, 33 + i), winK, 28, winV, OWB, False, qa, qk)
                sy.op("vector", lambda e, g=g: e.tensor_copy(out=ow_sb[:, g, :, :], in_=psb[OWB][:, 0:264].rearrange("p (r e) -> p r e", r=4)),
                      reads=[pst[OWB]], writes=[t[("ow", g)]])
                attn_tiles(i, g, range(0, 33 + i), slcK, 0, slcV, OSB, True, qa, qk)
                sy.op("vector", lambda e, g=g: e.tensor_copy(out=os_sb[:, g, :, :], in_=psb[OSB][:, 0:264].rearrange("p (r e) -> p r e", r=4)),
                      reads=[pst[OSB]], writes=[t[("os", g)]])
            gt3 = gt[:].rearrange("p (g r b) -> p g r b", g=2, r=4)
            sy.op("vector", lambda e: e.reciprocal(out=rs[:, :, :, 0:1], in_=os_sb[:, :, :, 64:65]), reads=[t[("os", 0)], t[("os", 1)]], writes=[t["rs"]])
            sy.op("vector", lambda e: e.reciprocal(out=rs[:, :, :, 1:2], in_=ow_sb[:, :, :, 64:65]), reads=[t[("ow", 0)], t[("ow", 1)]], writes=[t["rs"]])
            sy.op("vector", lambda e, gt3=gt3: e.tensor_tensor(out=rs[:], in0=rs[:], in1=gt3[:, :, :, 1:3], op=ALU.mult), reads=[gk], writes=[t["rs"]])
            for g in range(2):
                for r in range(4):
                    col = (g * 4 + r) * 64
                    gc = gt[:, g * 12 + r * 3:g * 12 + r * 3 + 1]
                    sy.op("vector", lambda e, g=g, r=r, gc=gc: e.tensor_scalar(out=ytmp[:], in0=oc_sb[:, g, r, :], scalar1=gc, scalar2=None, op0=ALU.mult),
                          reads=[t[("oc", g)], gk], writes=[t["ytmp"]])
                    sy.op("vector", lambda e, g=g, r=r: e.scalar_tensor_tensor(out=ytmp[:], in0=os_sb[:, g, r, 0:64], scalar=rs[:, g, r, 0:1], in1=ytmp[:], op0=ALU.mult, op1=ALU.add),
                          reads=[t[("os", g)], t["rs"]], writes=[t["ytmp"]])
                    sy.op("vector", lambda e, g=g, r=r, col=col: e.scalar_tensor_tensor(out=yb_tm[:, col:col + 64], in0=ow_sb[:, g, r, 0:64], scalar=rs[:, g, r, 1:2], in1=ytmp[:], op0=ALU.mult, op1=ALU.add),
                          reads=[t[("ow", g)], t["rs"], t["ytmp"]], writes=[t["ybtm"]])
            tb = next_ps(0, 6)
            for c4 in range(4):
                sy.op("tensor", lambda e, tb=tb, c4=c4: e.transpose(out=psb[tb][:, c4 * 128:(c4 + 1) * 128], in_=yb_tm[:, c4 * 128:(c4 + 1) * 128], identity=ident[:]),
                      reads=[t["ybtm"], tC["ident"]], writes=[pst[tb]])
            sy.op("scalar", lambda e, tb=tb, i=i: e.copy(out=ybT[:, :, i * 128:(i + 1) * 128], in_=psb[tb][:, :].rearrange("p (c n) -> p c n", c=4)),
                  reads=[pst[tb]], writes=[tYb[i]])
        sy.barrier()
        es.close()

    sy.new_epoch()
    if not dbg.get("skipB"):
        stage_B()
    else:
        for i in range(NT):
            sy.op("gpsimd", lambda e, i=i: e.memset(ybT[:, :, i * 128:(i + 1) * 128], 0.0), writes=[tYb[i]])

    if "ybT" in dbg:
        o = P.dout("dbg_ybT", [128, 4 * OWN], BF16)
        sy.dma("sync", o[:, :], ybT[:].rearrange("p a b -> p (a b)"), reads=tYb.all(), stream="o")

    h1T_d = P.dscratch("h1T_scratch", [NT, 128, 8 * 128], BF16)
    h1_t = h1_d.rearrange("(n p) d -> n p d", p=128)

    def layer_norm(t, z, zk, lnrep, dst, dstk, tmp_stats, tmp_mv):
        for hh in range(2):
            sy.op("vector", lambda e, hh=hh: e.bn_stats(out=tmp_stats[:, hh * 6:(hh + 1) * 6], in_=z[:, hh * 512:(hh + 1) * 512]),
                  reads=[zk], writes=[t["lnst"]])
        sy.op("vector", lambda e: e.bn_aggr(out=tmp_mv[:, 0:2], in_=tmp_stats[:, 0:12]), reads=[t["lnst"]], writes=[t["lnmv"]])
        sy.op("vector", lambda e: e.tensor_scalar(out=tmp_mv[:, 2:3], in0=tmp_mv[:, 1:2], scalar1=LN_EPS, scalar2=None, op0=ALU.add),
              reads=[t["lnmv"]], writes=[t["lnmv"]])
        sy.op("scalar", lambda e: e.activation(out=tmp_mv[:, 2:3], in_=tmp_mv[:, 2:3], func=AF.Sqrt), reads=[t["lnmv"]], writes=[t["lnmv"]])
        sy.op("vector", lambda e: e.reciprocal(out=tmp_mv[:, 3:4], in_=tmp_mv[:, 2:3]), reads=[t["lnmv"]], writes=[t["lnmv"]])
        sy.op("vector", lambda e: e.tensor_scalar(out=dst[:], in0=z[:], scalar1=tmp_mv[:, 0:1], scalar2=tmp_mv[:, 3:4], op0=ALU.subtract, op1=ALU.mult),
              reads=[zk, t["lnmv"]], writes=[dstk])
        sy.op("gpsimd", lambda e: e.tensor_tensor(out=dst[:], in0=dst[:], in1=lnrep[:, 0, :], op=ALU.mult), reads=[t["ln"]], writes=[dstk])
        sy.op("gpsimd", lambda e: e.tensor_tensor(out=dst[:], in0=dst[:], in1=lnrep[:, 1, :], op=ALU.add), reads=[t["ln"]], writes=[dstk])

    def stage_C():
        es = contextlib.ExitStack()
        t = TK()
        xin_t = xin.rearrange("(n p) d -> n p d", p=128)
        ps_rot = [0]

        def next_ps():
            i = ps_rot[0]
            ps_rot[0] = i + 1
            return i % 8

        lnrep = SB(es, "C_lnrep", [128, 2, D], F32)
        sy.dma("sync", lnrep[:].rearrange("p a d -> p (a d)"), cd["lnrep"][:, 0:2 * D], writes=[t["ln"]], stream="c")
        wst = SB(es, "C_wst", [128, 2, 1024], F32)
        wM = SB(es, "C_wM", [128, 8, 2048], BF16)
        wAB = SB(es, "C_wAB", [128, 6, D], BF16)
        wO = SB(es, "C_wO", [128, 8, D], BF16)
        wR = SB(es, "C_wR", [128, 8, 36], F32)
        brep = SB(es, "C_brep", [128, 36], F32)
        xs = [SB(es, f"C_xs{i}", [128, D], F32) for i in range(2)]
        xT1 = SB(es, "C_xT1", [128, 8, 128], BF16)
        gT = SB(es, "C_gT", [128, 16, 128], F32)
        mT = SB(es, "C_mT", [128, 8, 128], BF16)
        tmp1 = SB(es, "C_tmp1", [128, 128], F32)
        tmp2 = SB(es, "C_tmp2", [128, 128], F32)
        z = SB(es, "C_z", [128, D], F32)
        h1 = [SB(es, f"C_h1{i}", [128, D], F32) for i in range(2)]
        h1T32 = SB(es, "C_h1T32", [128, 8, 128], F32)
        h1Tb = [SB(es, f"C_h1Tb{i}", [128, 8, 128], BF16) for i in range(2)]
        st6 = SB(es, "C_st6", [128, 12], F32)
        mv = SB(es, "C_mv", [128, 4], F32)
        lg = SB(es, "C_lg", [128, 36], F32)
        rt = SB(es, "C_rt", [128, 64], F32)
        m8 = SB(es, "C_m8", [128, 8], F32)

        def load_wgen(dst, src2d, rows, ncol, key):
            for r2 in range(rows // 256):
                sy.dma("gpsimd", wst[:, :, 0:ncol], src2d[r2 * 256:(r2 + 1) * 256, :].rearrange("(k p) c -> p k c", p=128),
                       writes=[t["wst"]], stream="w0")
                sy.op("gpsimd", lambda e, r2=r2: e.tensor_copy(out=dst[:, r2 * 2:r2 * 2 + 2, :], in_=wst[:, :, 0:ncol]),
                      reads=[t["wst"]], writes=[t[key]])

        load_wgen(wM[:, :, 0:1024], w_in[:, C_MG:C_MG + 1024], D, 1024, "wM")
        load_wgen(wM[:, :, 1024:2048], w_in[:, C_MG + 1024:C_MG + 2048], D, 1024, "wM")
        load_wgen(wAB[:, 0:2, :], cd["w_ba"], 256, D, "wAB")
        load_wgen(wAB[:, 2:6, :], cd["w_bb"], 512, D, "wAB")
        load_wgen(wO, cd["w_out"], D, D, "wO")
        sy.dma("sync", wR[:], cd["w_router"].rearrange("(k p) c -> p k c", p=128), writes=[t["wR"]], stream="c")
        sy.dma("sync", brep[:], cd["b_router"][:, :], writes=[t["brep"]], stream="c")

        for i in range(NT):
            sI = i % 2
            sy.dma("sync", xs[sI][:], xin_t[32 + i, :, :], writes=[t[("xs", sI)]], stream=f"x{sI}")
            for half in range(2):
                b = next_ps()
                for kk in range(4):
                    kc = half * 4 + kk
                    sy.op("tensor", lambda e, b=b, kk=kk, kc=kc, sI=sI: e.transpose(
                        out=psb[b][:, kk * 128:(kk + 1) * 128], in_=xs[sI][:, kc * 128:(kc + 1) * 128], identity=ident[:]),
                        reads=[t[("xs", sI)], tC["ident"]], writes=[pst[b]])
                sy.op("vector" if half == 0 else "scalar",
                      (lambda e, b=b, half=half: e.tensor_copy(out=xT1[:, half * 4:half * 4 + 4, :], in_=psb[b][:, :].rearrange("p (k c) -> p k c", k=4))) if half == 0 else
                      (lambda e, b=b, half=half: e.copy(out=xT1[:, half * 4:half * 4 + 4, :], in_=psb[b][:, :].rearrange("p (k c) -> p k c", k=4))),
                      reads=[pst[b]], writes=[t[("xT1", half)]])
            xr = [t[("xT1", 0)], t[("xT1", 1)]]
            tok = slice(i * 128, (i + 1) * 128)
            for c4 in range(4):
                b = next_ps()
                for cc in range(4):
                    ct = c4 * 4 + cc
                    for kc in range(8):
                        sy.op("tensor", lambda e, b=b, cc=cc, ct=ct, kc=kc: e.matmul(
                            psb[b][:, cc * 128:(cc + 1) * 128], lhsT=wM[:, kc, ct * 128:(ct + 1) * 128], rhs=xT1[:, kc, :],
                            start=(kc == 0 and cc == 0), stop=(kc == 7), skip_group_check=True),
                            reads=[t["wM"]] + xr, writes=[pst[b]])
                sy.op("scalar", lambda e, b=b, c4=c4: e.activation(out=gT[:, c4 * 4:c4 * 4 + 4, :], in_=psb[b][:, :].rearrange("p (c n) -> p c n", c=4), func=AF.Sigmoid),
                      reads=[pst[b]], writes=[t[("gT", c4)]])
            for c in range(8):
                b = next_ps()
                for k2 in range(2):
                    sy.op("tensor", lambda e, b=b, c=c, k2=k2: e.matmul(
                        psb[b][:, 0:128], lhsT=wAB[:, k2, c * 128:(c + 1) * 128], rhs=yaT[:, k2, tok], start=(k2 == 0), stop=(k2 == 1), skip_group_check=True),
                        reads=[t["wAB"]] + tYa.all(), writes=[pst[b]])
                for k4 in range(4):
                    sy.op("tensor", lambda e, b=b, c=c, k4=k4: e.matmul(
                        psb[b][:, 128:256], lhsT=wAB[:, 2 + k4, c * 128:(c + 1) * 128], rhs=ybT[:, k4, tok], start=False, stop=(k4 == 3), skip_group_check=True),
                        reads=[t["wAB"], tYb[i]], writes=[pst[b]])
                sy.op("vector", lambda e, b=b, c=c: e.tensor_tensor(out=tmp1[:], in0=psb[b][:, 0:128], in1=gT[:, c, :], op=ALU.mult),
                      reads=[pst[b], t[("gT", c // 4)]], writes=[t["tmp1"]])
                sy.op("vector", lambda e, b=b, c=c: e.tensor_tensor(out=tmp2[:], in0=psb[b][:, 128:256], in1=gT[:, 8 + c, :], op=ALU.mult),
                      reads=[pst[b], t[("gT", 2 + c // 4)]], writes=[t["tmp2"]])
                sy.op("gpsimd", lambda e, c=c: e.tensor_tensor(out=mT[:, c, :], in0=tmp1[:], in1=tmp2[:], op=ALU.add),
                      reads=[t["tmp1"], t["tmp2"]], writes=[t[("mT", c)]])
            for hf2 in range(2):
                b = next_ps()
                for c in range(8):
                    sy.op("tensor", lambda e, b=b, c=c, hf2=hf2: e.matmul(
                        psb[b][:, :], lhsT=mT[:, c, :], rhs=wO[:, c, hf2 * 512:(hf2 + 1) * 512], start=(c == 0), stop=(c == 7)),
                        reads=[t["wO"], t[("mT", c)]], writes=[pst[b]])
                sy.op("vector", lambda e, b=b, hf2=hf2, sI=sI: e.scalar_tensor_tensor(
                    out=z[:, hf2 * 512:(hf2 + 1) * 512], in0=xs[sI][:, hf2 * 512:(hf2 + 1) * 512], scalar=ALPHA, in1=psb[b][:, :], op0=ALU.mult, op1=ALU.add),
                    reads=[pst[b], t[("xs", sI)]], writes=[t["z"]])
            hb_ = h1[i % 2]
            hk = t[("h1", i % 2)]
            layer_norm(t, z, t["z"], lnrep, hb_, hk, st6, mv)
            sy.dma("sync", h1_t[i, :, :], hb_[:], reads=[hk], writes=[tH[("h1d", i)]], stream="h1w")
            for half in range(2):
                b = next_ps()
                for kk in range(4):
                    kc = half * 4 + kk
                    sy.op("tensor", lambda e, b=b, kk=kk, kc=kc, hb_=hb_: e.transpose(
                        out=psb[b][:, kk * 128:(kk + 1) * 128], in_=hb_[:, kc * 128:(kc + 1) * 128], identity=ident[:]),
                        reads=[hk, tC["ident"]], writes=[pst[b]])
                src = psb[b][:, :].rearrange("p (k c) -> p k c", k=4)
                sy.op("vector", lambda e, src=src, half=half: e.tensor_copy(out=h1T32[:, half * 4:half * 4 + 4, :], in_=src),
                      reads=[pst[b]], writes=[t[("h1T32", half)]])
                sy.op("scalar", lambda e, src=src, half=half: e.copy(out=h1Tb[i % 2][:, half * 4:half * 4 + 4, :], in_=src),
                      reads=[pst[b]], writes=[t[("h1Tb", i % 2, half)]])
            sy.dma("sync", h1T_d[i, :, :], h1Tb[i % 2][:].rearrange("p k n -> p (k n)"), reads=[t[("h1Tb", i % 2, 0)], t[("h1Tb", i % 2, 1)]],
                   writes=[tH[("h1T", i)]], stream="h1w")
            b = next_ps()
            for kc in range(8):
                sy.op("tensor", lambda e, b=b, kc=kc: e.matmul(psb[b][:, 0:36], lhsT=h1T32[:, kc, :], rhs=wR[:, kc, :], start=(kc == 0), stop=(kc == 7)),
                      reads=[t[("h1T32", 0)], t[("h1T32", 1)], t["wR"]], writes=[pst[b]])
            V = lambda fn, rd, wr: sy.op("vector", fn, reads=rd, writes=wr)
            rk = t["rt"]
            V(lambda e, b=b: e.tensor_tensor(out=lg[:], in0=psb[b][:, 0:36], in1=brep[:], op=ALU.add), [pst[b], t["brep"]], [rk])
            V(lambda e: e.tensor_reduce(out=rt[:, 0:1], in_=lg[:, 0:4], axis=AX.X, op=ALU.max), [rk], [rk])
            V(lambda e: e.tensor_scalar(out=rt[:, 4:8], in0=lg[:, 0:4], scalar1=rt[:, 0:1], scalar2=None, op0=ALU.is_ge), [rk], [rk])
            V(lambda e: e.tensor_scalar(out=rt[:, 1:2], in0=rt[:, 0:1], scalar1=-1.0, scalar2=None, op0=ALU.mult), [rk], [rk])
            sy.op("scalar", lambda e: e.activation(out=rt[:, 8:12], in_=lg[:, 0:4], func=AF.Exp, bias=rt[:, 1:2], scale=1.0), reads=[rk], writes=[rk])
            V(lambda e: e.tensor_reduce(out=rt[:, 2:3], in_=rt[:, 8:12], axis=AX.X, op=ALU.add), [rk], [rk])
            V(lambda e: e.reciprocal(out=rt[:, 3:4], in_=rt[:, 2:3]), [rk], [rk])
            V(lambda e: e.tensor_scalar(out=rt[:, 8:12], in0=rt[:, 4:8], scalar1=-1.0, scalar2=1.0e9, op0=ALU.add, op1=ALU.mult), [rk], [rk])
            V(lambda e: e.tensor_tensor(out=rt[:, 16:48].rearrange("p (g e) -> p g e", g=4), in0=lg[:, 4:36].rearrange("p (g e) -> p g e", g=4),
                                        in1=rt[:, 8:12].unsqueeze(2).to_broadcast([128, 4, 8]), op=ALU.add), [rk], [rk])
            V(lambda e: e.max(out=m8[:], in_=rt[:, 16:48]), [rk], [t["m8"]])
            V(lambda e: e.tensor_tensor(out=rt[:, 12:13], in0=m8[:, 0:1], in1=m8[:, 1:2], op=ALU.subtract), [t["m8"]], [rk])
            sy.op("scalar", lambda e: e.activation(out=rt[:, 12:13], in_=rt[:, 12:13], func=AF.Sigmoid), reads=[rk], writes=[rk])
            V(lambda e: e.tensor_scalar(out=rt[:, 13:14], in0=rt[:, 12:13], scalar1=-1.0, scalar2=1.0, op0=ALU.mult, op1=ALU.add), [rk], [rk])
            V(lambda e: e.tensor_scalar(out=rt[:, 12:14], in0=rt[:, 12:14], scalar1=rt[:, 3:4], scalar2=None, op0=ALU.mult), [rk], [rk])
            V(lambda e: e.tensor_scalar(out=rt[:, 48:64], in0=rt[:, 16:32], scalar1=0.0, scalar2=None, op0=ALU.mult), [rk], [rk])
            V(lambda e: e.tensor_scalar(out=Wt[:, i, :], in0=rt[:, 16:48], scalar1=m8[:, 1:2], scalar2=rt[:, 13:14], op0=ALU.is_ge, op1=ALU.mult),
              [rk, t["m8"]], [tH[("Wt", i)]])
            V(lambda e: e.tensor_scalar(out=lg[:, 4:36], in0=rt[:, 16:48], scalar1=m8[:, 0:1], scalar2=None, op0=ALU.is_ge), [rk, t["m8"]], [rk])
            V(lambda e: e.tensor_tensor(out=rt[:, 14:15], in0=rt[:, 12:13], in1=rt[:, 13:14], op=ALU.subtract), [rk], [rk])
            V(lambda e: e.scalar_tensor_tensor(out=Wt[:, i, :], in0=lg[:, 4:36], scalar=rt[:, 14:15], in1=Wt[:, i, :], op0=ALU.mult, op1=ALU.add),
              [rk], [tH[("Wt", i)]])
        sy.barrier()
        es.close()

    sy.new_epoch()
    if not dbg.get("skipC"):
        stage_C()
    es_y.close()
    if "h1" in dbg:
        o = P.dout("dbg_h1", [OWN, D], F32)
        sy.dma("sync", o[:, :], h1_d[:, :], reads=tH.all(), stream="o")
        o = P.dout("dbg_Wt", [128, NT * 32], F32)
        sy.dma("sync", o[:, :], Wt[:].rearrange("p a b -> p (a b)"), reads=tH.all(), stream="o")

    def stage_D():
        es = contextlib.ExitStack()
        t = TK()
        out_t = out.rearrange("(n p) d -> n p d", p=128)
        yacc = SB(es, "D_yacc", [128, 16, D], F32)
        lnrep = SB(es, "D_lnrep", [128, 2, D], F32)
        sy.dma("sync", lnrep[:].rearrange("p a d -> p (a d)"), cd["lnrep"][:, 2 * D:4 * D], writes=[t["ln"]], stream="c")
        h1T = SB(es, "D_h1T", [128, 16, 8, 128], BF16)
        wgu2 = [SB(es, f"D_wgu{i}", [128, 8, 2 * D_EXP], BF16) for i in range(2)]
        wdn2 = [SB(es, f"D_wdn{i}", [128, 4, D], BF16) for i in range(2)]
        ecount = [0]
        wst = [SB(es, f"D_wst{i}", [128, 2, 1024], F32) for i in range(2)]
        aT = SB(es, "D_aT", [128, 4, 512], BF16)
        sg = [SB(es, f"D_sg{i}", [128, 512], F32) for i in range(2)]
        hz = [SB(es, f"D_hz{i}", [128, D], F32) for i in range(2)]
        st6 = SB(es, "D_st6", [128, 12], F32)
        mv = SB(es, "D_mv", [128, 4], F32)
        ps_rot = [0]

        def next_ps():
            i = ps_rot[0]
            ps_rot[0] = i + 1
            return i % 8

        wcnt = [0]
        cast_eng = ("gpsimd", "vector", "gpsimd", "scalar")
        for hh in range(2):
            for j in range(16):
                sy.op("gpsimd", lambda e, j=j: e.memset(yacc[:, j, :], 0.0), writes=[t[("yacc", j)]])
                sy.dma("sync", h1T[:, j, :, :].rearrange("p k n -> p (k n)"), h1T_d[hh * 16 + j, :, :], reads=[tH[("h1T", hh * 16 + j)]],
                       writes=[t[("h1T", j)]], stream="h1r")
            for ex in range(N_EXP):
                wb = ecount[0] % 2
                ecount[0] += 1
                wgu, wdn = wgu2[wb], wdn2[wb]
                for r2 in range(6):
                    wi = wcnt[0] % 2
                    wcnt[0] += 1
                    if r2 < 4:
                        src = cd["w_gu"][ex, r2 * 256:(r2 + 1) * 256, :].rearrange("(k p) c -> p k c", p=128)
                        dst, dk = wgu[:, r2 * 2:r2 * 2 + 2, :], ("wgu", wb)
                    else:
                        src = cd["w_dn"][ex, (r2 - 4) * 256:(r2 - 3) * 256, :].rearrange("(k p) c -> p k c", p=128)
                        dst, dk = wdn[:, (r2 - 4) * 2:(r2 - 4) * 2 + 2, :], ("wdn", wb)
                    sy.dma("sync", wst[wi][:], src, writes=[t[("wst", wi)]], stream=f"e{wi}")
                    ce = "gpsimd"
                    if ce == "scalar":
                        sy.op("scalar", lambda e, dst=dst, wi=wi: e.copy(out=dst, in_=wst[wi][:]), reads=[t[("wst", wi)]], writes=[t[dk]])
                    else:
                        sy.op(ce, lambda e, dst=dst, wi=wi: e.tensor_copy(out=dst, in_=wst[wi][:]), reads=[t[("wst", wi)]], writes=[t[dk]])
                for c4 in range(4):
                    tok0 = hh * 2048 + c4 * 512
                    hr = [t[("h1T", c4 * 4 + jj)] for jj in range(4)]
                    for cc in range(4):
                        bg = next_ps()
                        bu = next_ps()
                        for (bb, ct) in ((bg, cc), (bu, 4 + cc)):
                            for kc in range(8):
                                sy.op("tensor", lambda e, bb=bb, ct=ct, kc=kc, tok0=tok0: e.matmul(
                                    psb[bb][:, :], lhsT=wgu[:, kc, ct * 128:(ct + 1) * 128], rhs=h1T[:, c4 * 4:c4 * 4 + 4, kc, :], start=(kc == 0), stop=(kc == 7)),
                                    reads=[t[("wgu", wb)]] + hr, writes=[pst[bb]])
                        si = cc % 2
                        sy.op("scalar", lambda e, bg=bg, si=si: e.activation(out=sg[si][:], in_=psb[bg][:, :], func=AF.Silu), reads=[pst[bg]], writes=[t[("sg", si)]])
                        sy.op("vector", lambda e, bu=bu, si=si, cc=cc: e.tensor_tensor(out=aT[:, cc, :], in0=psb[bu][:, :], in1=sg[si][:], op=ALU.mult),
                              reads=[pst[bu], t[("sg", si)]], writes=[t[("aT", cc)]])
                    for jj in range(4):
                        j = c4 * 4 + jj
                        for hf2 in range(2):
                            b = next_ps()
                            for k in range(4):
                                sy.op("tensor", lambda e, b=b, k=k, jj=jj, hf2=hf2: e.matmul(
                                    psb[b][:, :], lhsT=aT[:, k, jj * 128:(jj + 1) * 128], rhs=wdn[:, k, hf2 * 512:(hf2 + 1) * 512], start=(k == 0), stop=(k == 3)),
                                    reads=[t[("wdn", wb)], t[("aT", k)]], writes=[pst[b]])
                            sy.op("vector", lambda e, b=b, j=j, hf2=hf2, ex=ex: e.scalar_tensor_tensor(
                                out=yacc[:, j, hf2 * 512:(hf2 + 1) * 512], in0=psb[b][:, :], scalar=Wt[:, hh * 16 + j, ex:ex + 1],
                                in1=yacc[:, j, hf2 * 512:(hf2 + 1) * 512], op0=ALU.mult, op1=ALU.add),
                                reads=[pst[b], tH[("Wt", hh * 16 + j)]], writes=[t[("yacc", j)]])
            for j in range(16):
                i = hh * 16 + j
                hb_ = hz[j % 2]
                hk = t[("hz", j % 2)]
                sy.dma("sync", hb_[:], h1_t[i, :, :], reads=[tH[("h1d", i)]], writes=[hk], stream="h1r")
                sy.op("vector", lambda e, j=j, hb_=hb_: e.scalar_tensor_tensor(out=yacc[:, j, :], in0=hb_[:], scalar=ALPHA, in1=yacc[:, j, :], op0=ALU.mult, op1=ALU.add),
                      reads=[hk], writes=[t[("yacc", j)]])
                layer_norm(t, yacc[:, j, :], t[("yacc", j)], lnrep, hb_, hk, st6, mv)
                sy.dma("sync", out_t[i, :, :], hb_[:], reads=[hk], stream="o")
        sy.barrier()
        es.close()

    sy.new_epoch()
    if not dbg.get("skipD"):
        stage_D()
    sy.barrier()
    S = sy.dsem.get(("sync", "o"))
    if S is not None:
        nc.sync.wait_ge(S["sem"], 16 * S["cnt"])
    return P


def make_core_map(inputs, W, b, hf, names):
    x = np.asarray(inputs["x"][b], dtype=np.float32)
    if hf == 1:
        xin = x
    else:
        xin = np.concatenate([np.zeros((OWN, D), np.float32), x[:OWN]], axis=0)
    m = {"xin": np.ascontiguousarray(xin)}
    m.update(W)
    m.update(make_consts(hf))
    return {k: m[k] for k in names}


_PROG = None


def kernel(**inputs):
    global _PROG
    inputs = {k: np.asarray(v) for k, v in inputs.items()}
    if _PROG is None:
        _PROG = build_program()
    P = _PROG
    W = weight_layouts(inputs)
    names = list(P.ins)
    consts = [make_consts(0), make_consts(1)]
    maps = []
    for c in range(8):
        b, hf = c // 2, c % 2
        x = np.asarray(inputs["x"][b], dtype=np.float32)
        if hf == 1:
            xin = x
        else:
            xin = np.concatenate([np.zeros((OWN, D), np.float32), x[:OWN]], axis=0)
        m = {"xin": np.ascontiguousarray(xin)}
        m.update(W)
        m.update(consts[hf])
        maps.append({k: m[k] for k in names})
    res = run_bass_kernel_spmd(P.nc, maps, core_ids=list(range(8)))
    out = np.zeros((NB, SEQ, D), np.float32)
    for c in range(8):
        b, hf = c // 2, c % 2
        out[b, hf * OWN:(hf + 1) * OWN] = np.asarray(res.results[c]["out"], dtype=np.float32)
    return out
```

```python
import contextlib
import numpy as np
import ml_dtypes
import concourse.bass as bass
import concourse.mybir as mybir
from concourse.bass_utils import run_bass_kernel_spmd
from concourse.alu_op_type import AluOpType as ALU

F32 = mybir.dt.float32
BF16 = mybir.dt.bfloat16
AF = mybir.ActivationFunctionType
AX = mybir.AxisListType

D = 1024
SEQ = 8192
NB = 4
OWN = 4096
NT = OWN // 128
HD = 64
DIL = ((128, 1), (512, 4), (2048, 16))
IN_COLS = 5656
C_AQ, C_AK, C_AV = 0, 768, 1536
C_BQ = 2304
C_BKV = 2816
C_BG = 3584
C_MG = 3608
NEG = -30000.0
ALPHA = 2.0 ** 0.25
LN_EPS = 1e-5
N_EXP = 32
D_EXP = 512

DEBUG = {}


class Trk:
    __slots__ = ("w", "r", "x")

    def __init__(self, x=False):
        self.w = None
        self.r = {}
        self.x = x


class TK:
    def __init__(self):
        self.d = {}

    def __getitem__(self, k):
        t = self.d.get(k)
        if t is None:
            t = self.d[k] = Trk()
        return t

    def all(self):
        return list(self.d.values())


class Sy:
    def __init__(self, nc):
        self.nc = nc
        self.eng = {}
        for name in ("tensor", "vector", "scalar", "gpsimd", "sync"):
            self.eng[name] = dict(e=getattr(nc, name), sem=nc.alloc_semaphore(f"s_{name}"), cnt=0, known={})
        self.dsem = {}
        self.ninst = 0
        self.epoch = 0

    def new_epoch(self):
        self.barrier()
        self.epoch += 1
        for name, E in self.eng.items():
            E["sem"] = self.nc.alloc_semaphore(f"s_{name}_{self.epoch}")
            E["cnt"] = 0
            E["known"] = {}
        self.dsem = {}

    def _wait(self, E, deps):
        best = {}
        for sem, val in deps:
            k = id(sem)
            if k not in best or best[k][1] < val:
                best[k] = (sem, val)
        for k, (sem, val) in best.items():
            if E["known"].get(k, 0) >= val:
                continue
            E["e"].wait_ge(sem, val)
            E["known"][k] = val
            self.ninst += 1

    def _deps(self, E, reads, writes, skip_own):
        deps = []
        for t in reads:
            if t.w is not None:
                deps.append(t.w)
        for t in writes:
            if t.w is not None:
                deps.append(t.w)
            deps.extend(t.r.values())
        if skip_own:
            deps = [d for d in deps if d[0] is not E["sem"]]
        return deps

    def op(self, name, fn, reads=(), writes=()):
        E = self.eng[name]
        if any(t.x for t in reads):
            writes = list(writes) + [t for t in reads if t.x]
            reads = [t for t in reads if not t.x]
        self._wait(E, self._deps(E, reads, writes, name == "tensor"))
        ins = fn(E["e"])
        E["cnt"] += 1
        ins.then_inc(E["sem"], 1)
        self.ninst += 1
        tok = (E["sem"], E["cnt"])
        for t in writes:
            t.w = tok
            t.r = {}
        for t in reads:
            t.r[id(E["sem"])] = tok
        return tok

    def dma(self, qname, out, in_, reads=(), writes=(), stream="d"):
        E = self.eng[qname]
        skey = (qname, stream)
        S = self.dsem.get(skey)
        if S is None:
            S = self.dsem[skey] = dict(sem=self.nc.alloc_semaphore(f"d_{qname}_{stream}_{self.epoch}"), cnt=0)
        self._wait(E, self._deps(E, reads, writes, False))
        ins = E["e"].dma_start(out=out, in_=in_)
        S["cnt"] += 1
        ins.then_inc(S["sem"], 16)
        self.ninst += 1
        tok = (S["sem"], 16 * S["cnt"])
        for t in writes:
            t.w = tok
            t.r = {}
        for t in reads:
            t.r[id(S["sem"])] = tok
        return tok

    def barrier(self):
        toks = [(E["sem"], E["cnt"]) for E in self.eng.values() if E["cnt"] > 0]
        toks += [(S["sem"], 16 * S["cnt"]) for S in self.dsem.values()]
        for E in self.eng.values():
            self._wait(E, [t for t in toks if t[0] is not E["sem"]])


def SSL(base, d):
    return slice(base, base + 127 * d + 1, d)


def _bf(a):
    return np.asarray(a, dtype=np.float32).astype(ml_dtypes.bfloat16)


def alibi(n):
    return np.exp2(-8.0 * np.arange(1, n + 1, dtype=np.float32) / n).astype(np.float32)


def make_consts(hf):
    c = {}
    c["ident"] = np.eye(128, dtype=np.float32)
    c["identb"] = _bf(np.eye(128))
    k = np.arange(128)[:, None]
    q = np.arange(128)[None, :]
    sl = alibi(12).reshape(3, 4)
    bm = np.zeros((128, 3, 4, 2, 128), np.float32)
    for g, (win, d) in enumerate(DIL):
        for h in range(4):
            dprev = (q - k + 128).astype(np.float32)
            dcur = (q - k).astype(np.float32)
            bm[:, g, h, 0, :] = np.where(dprev <= 128, -8.0 * sl[g, h] * d * dprev, 8.0 * NEG)
            bm[:, g, h, 1, :] = np.where(dcur >= 0, -8.0 * sl[g, h] * d * dcur, 8.0 * NEG)
    c["bmA"] = bm.reshape(128, -1)
    c["vflag"] = np.tile(np.array([[float(hf), 1.0]], np.float32), (128, 1))
    slb = alibi(8)
    u = np.arange(2 * OWN)
    c["kaug"] = _bf(np.stack([(u % 128) - 64.0, u // 128, np.ones_like(u)]).astype(np.float32))
    qa = np.zeros((3, 8, OWN), np.float32)
    qt = 32 + np.arange(OWN) // 128
    for h in range(8):
        qa[0, h] = 8.0 * slb[h]
        qa[1, h] = 1024.0 * slb[h]
        qa[2, h] = -1024.0 * slb[h] * qt
    c["qaug"] = _bf(qa.reshape(3, 8 * OWN))
    cup = np.arange(128)[:, None]
    mcb = np.zeros((128, 17, 128), np.float32)
    for idx in range(17):
        off = idx * 8 - 2
        mcb[:, idx, :] = np.where(cup <= off + (q + 1) // 16, 0.0, NEG)
    c["mcb"] = _bf(mcb.reshape(128, -1))
    cu = np.arange(512)
    cval = ((cu <= 510) & ((cu >= 256) | (hf == 1))).astype(np.float32)
    c["cvalid"] = np.ascontiguousarray(cval.reshape(4, 128).T)
    c["validrep"] = _bf(np.repeat(cval.reshape(4, 128).T[:, :, None], 128, axis=2).reshape(128, -1))
    jb = np.arange(128)
    ovl = ((16 * cu[:, None] < 64 * jb[None, :] + 64) & (16 * cu[:, None] + 32 > 64 * jb[None, :])).astype(np.float32)
    ovl = ovl * cval[:, None]
    c["ovl"] = _bf(ovl.reshape(4, 128, 128).transpose(1, 0, 2).reshape(128, -1))
    wd = np.zeros((128, 190), np.float32)
    qq = np.arange(128)[:, None]
    jj = np.arange(190)[None, :] - 62
    cur = 64 + (qq >= 64)
    wd = np.where(jj > cur, -1.0e9, np.where((jj == cur) | (jj == cur - 1), 1.0e4, 0.0)).astype(np.float32)
    c["wd"] = wd
    fb = np.zeros((128,), np.float32)
    if hf == 1:
        fb[0] = 1.0e4
    else:
        fb[:64] = -1.0e9
        fb[64] = 1.0e4
    c["fbvec"] = np.tile(fb[None, :], (128, 1)).astype(np.float32)
    ind = np.zeros((128, 64, 128), np.float32)
    for kt in range(64):
        ind[2 * kt, kt, 0:64] = 1.0
        ind[2 * kt + 1, kt, 64:128] = 1.0
    c["indbig"] = _bf(ind.reshape(128, -1))
    c["tri_le"] = _bf(np.where(k <= q, 0.0, NEG))
    c["tri_gt"] = _bf(np.where(k > q, 0.0, NEG))
    return c


def weight_layouts(inp):
    w = {}
    w["w_in"] = np.ascontiguousarray(inp["w_in"][0])
    for kv in ("k", "v"):
        w1 = np.asarray(inp[f"cmp_w1_{kv}"][0])
        w[f"w1r_{kv}"] = np.ascontiguousarray(w1.reshape(32, 64, 256).transpose(1, 0, 2).reshape(64, 32 * 256))
        w[f"posT_{kv}"] = np.ascontiguousarray(np.asarray(inp[f"cmp_pos_{kv}"][0]).T)
        w[f"w2_{kv}"] = np.ascontiguousarray(inp[f"cmp_w2_{kv}"][0])
    w["w_ba"] = np.ascontiguousarray(inp["w_branch_a"][0])
    w["w_bb"] = np.ascontiguousarray(inp["w_branch_b"][0])
    w["w_out"] = np.ascontiguousarray(inp["w_out"][0])
    ln = np.concatenate([np.asarray(inp[k][0]).reshape(1, D) for k in ("ln1_g", "ln1_b", "ln2_g", "ln2_b")], axis=1)
    w["lnrep"] = np.ascontiguousarray(np.broadcast_to(ln, (128, 4 * D))).astype(np.float32)
    wf = np.asarray(inp["w_fine"][0]).transpose(1, 0, 2).reshape(D, 32)
    w["w_router"] = np.ascontiguousarray(np.concatenate([np.asarray(inp["w_coarse"][0]), wf], axis=1)).astype(np.float32)
    br = np.concatenate([np.asarray(inp["b_coarse"][0]).reshape(1, 4), np.asarray(inp["b_fine"][0]).reshape(1, 32)], axis=1)
    w["b_router"] = np.ascontiguousarray(np.broadcast_to(br, (128, 36))).astype(np.float32)
    if "w_gate_up" in inp:
        w["w_gu"] = np.ascontiguousarray(inp["w_gate_up"][0])
        w["w_dn"] = np.ascontiguousarray(inp["w_down"][0])
    return w


class Prog:
    def __init__(self, dbg=None):
        self.dbg = dbg or {}
        self.nc = bass.Bass("TRN2", target_bir_lowering=False)
        self.sy = Sy(self.nc)
        self.ins = {}
        self.outs = {}

    def din(self, name, shape, dt=F32):
        ap = self.nc.dram_tensor(name, list(shape), dt, kind="ExternalInput").ap()
        self.ins[name] = ap
        return ap

    def dout(self, name, shape, dt=F32):
        ap = self.nc.dram_tensor(name, list(shape), dt, kind="ExternalOutput").ap()
        self.outs[name] = ap
        return ap

    def dscratch(self, name, shape, dt=F32):
        return self.nc.dram_tensor(name, list(shape), dt, kind="Internal").ap()


def build_program(dbg=None):
    P = Prog(dbg)
    nc, sy = P.nc, P.sy
    dbg = P.dbg
    xin = P.din("xin", [2 * OWN, D])
    w_in = P.din("w_in", [D, IN_COLS])
    ident_d = P.din("ident", [128, 128])
    identb_d = P.din("identb", [128, 128], BF16)
    bmA_d = P.din("bmA", [128, 3 * 4 * 2 * 128])
    vflag_d = P.din("vflag", [128, 2])
    out = P.dout("out", [OWN, D])
    cd = {}
    for nm, shp, dt in (("kaug", [3, 2 * OWN], BF16), ("qaug", [3, 8 * OWN], BF16), ("mcb", [128, 17 * 128], BF16),
                        ("cvalid", [128, 4], F32), ("validrep", [128, 512], BF16), ("ovl", [128, 512], BF16),
                        ("wd", [128, 190], F32), ("fbvec", [128, 128], F32), ("indbig", [128, 64 * 128], BF16),
                        ("tri_le", [128, 128], BF16), ("tri_gt", [128, 128], BF16),
                        ("w1r_k", [64, 32 * 256], F32), ("posT_k", [64, 32], F32), ("w2_k", [256, 64], F32),
                        ("w1r_v", [64, 32 * 256], F32), ("posT_v", [64, 32], F32), ("w2_v", [256, 64], F32),
                        ("w_ba", [256, D], F32), ("w_bb", [512, D], F32), ("w_out", [D, D], F32),
                        ("lnrep", [128, 4 * D], F32), ("w_router", [D, 36], F32), ("b_router", [128, 36], F32),
                        ("w_gu", [N_EXP, D, 2 * D_EXP], F32), ("w_dn", [N_EXP, D_EXP, D], F32)):
        if nm in ("w_gu", "w_dn") and dbg.get("skipD"):
            continue
        cd[nm] = P.din(nm, shp, dt)
    h1_d = P.dscratch("h1_scratch", [OWN, D])

    es_glob = contextlib.ExitStack()
    SB = lambda es, name, shape, dt: es.enter_context(nc.sbuf_tensor(name, list(shape), dt))
    psb = [es_glob.enter_context(nc.psum_tensor(f"ps{i}", [128, 512], F32)) for i in range(8)]
    pst = [Trk(True) for _ in range(8)]

    ident = SB(es_glob, "ident_s", [128, 128], F32)
    identb = SB(es_glob, "identb_s", [128, 128], BF16)
    vflag = SB(es_glob, "vflag_s", [128, 2], F32)
    tC = TK()
    sy.dma("sync", ident[:], ident_d[:, :], writes=[tC["ident"]], stream="c")
    sy.dma("sync", identb[:], identb_d[:, :], writes=[tC["identb"]], stream="c")
    sy.dma("sync", vflag[:], vflag_d[:, :], writes=[tC["vflag"]], stream="c")

    Wt = SB(es_glob, "Wt", [128, NT, 32], F32)
    tH = TK()
    es_y = contextlib.ExitStack()
    yaT = SB(es_y, "yaT", [128, 2, OWN], BF16)
    tYa = TK()

    def stage_A():
        es = contextlib.ExitStack()
        xs = [SB(es, f"A_xs{i}", [128, D], F32) for i in range(2)]
        xT = SB(es, "A_xT", [128, 8, 2048], BF16)
        wst = [SB(es, "A_wst0", [128, 2, 768], F32)] * 2
        wA = SB(es, "A_w", [128, 8, 768], BF16)
        bmA = SB(es, "A_bm", [128, 4, 2, 128], F32)
        Kp = [SB(es, f"A_Kp{g}", [64, 4, 128 * d], BF16) for g, (_, d) in enumerate(DIL)]
        Vp = [SB(es, f"A_Vp{g}", [128, d, 4, 128], BF16) for g, (_, d) in enumerate(DIL)]
        Kc = SB(es, "A_Kc", [64, 4, 2048], BF16)
        Qc = SB(es, "A_Qc", [64, 4, 2048], BF16)
        Vc = SB(es, "A_Vc", [128, 16, 4, 128], BF16)
        acc = SB(es, "A_acc", [128, 4, 2048], F32)
        PT = [SB(es, f"A_PT{i}", [128, 512], BF16) for i in range(3)]
        t = TK()
        ps_rot = [0]

        def next_ps():
            i = ps_rot[0]
            ps_rot[0] = (i + 1) % 8
            return i

        xin_t = xin.rearrange("(n p) d -> n p d", p=128)
        nload = [0]

        def load_xT(tile0, ntiles):
            for j in range(ntiles):
                s = nload[0] % 2
                nload[0] += 1
                sy.dma("sync", xs[s][:], xin_t[tile0 + j, :, :], writes=[t[("xs", s)]], stream=f"x{s}")
                for half in range(2):
                    b = next_ps()
                    for kk in range(4):
                        kc = half * 4 + kk
                        sy.op("tensor", lambda e, b=b, kk=kk, kc=kc, s=s: e.transpose(
                            out=psb[b][:, kk * 128:(kk + 1) * 128], in_=xs[s][:, kc * 128:(kc + 1) * 128], identity=ident[:]),
                            reads=[t[("xs", s)], tC["ident"]], writes=[pst[b]])
                    eng = "vector" if half == 0 else "scalar"
                    if eng == "vector":
                        sy.op("vector", lambda e, b=b, half=half, j=j: e.tensor_copy(
                            out=xT[:, half * 4:half * 4 + 4, j * 128:(j + 1) * 128],
                            in_=psb[b][:, :].rearrange("p (k c) -> p k c", k=4)),
                            reads=[pst[b]], writes=[t[("xT", j, half)]])
                    else:
                        sy.op("scalar", lambda e, b=b, half=half, j=j: e.copy(
                            out=xT[:, half * 4:half * 4 + 4, j * 128:(j + 1) * 128],
                            in_=psb[b][:, :].rearrange("p (k c) -> p k c", k=4)),
                            reads=[pst[b]], writes=[t[("xT", j, half)]])

        def load_wA(g):
            sy.dma("gpsimd", bmA[:].rearrange("p b c d -> p (b c d)"), bmA_d[:, g * 1024:(g + 1) * 1024], writes=[t["bm"]], stream="c")
            for kc2 in range(4):
                s = 0
                for part, c0 in enumerate((C_AQ, C_AK, C_AV)):
                    col = c0 + g * 256
                    sy.dma("gpsimd", wst[s][:, :, part * 256:(part + 1) * 256],
                           w_in[kc2 * 256:(kc2 + 1) * 256, col:col + 256].rearrange("(k p) c -> p k c", p=128),
                           writes=[t[("wst", s)]], stream=f"w{s}")
                sy.op("gpsimd", lambda e, s=s, kc2=kc2: e.tensor_copy(out=wA[:, kc2 * 2:kc2 * 2 + 2, :], in_=wst[s][:]),
                      reads=[t[("wst", s)]], writes=[t["wA"]])

        xT_all = [t[("xT", j, hh)] for j in range(16) for hh in range(2)]
        aslopes = alibi(12).reshape(3, 4)

        for sc in (-1, 0, 1):
            own = sc >= 0
            tile0 = 32 + sc * 16
            load_xT(tile0, 16)
            for g, (win, d) in enumerate(DIL):
                nblk = 16 // d
                load_wA(g)
                for which, dst, cbase in (("q", Qc, 0), ("k", Kc, 256)):
                    if which == "q" and not own:
                        continue
                    for hp in range(2):
                        for nck in range(4):
                            b = next_ps()
                            for kc in range(8):
                                sy.op("tensor", lambda e, b=b, kc=kc, hp=hp, nck=nck, cbase=cbase: e.matmul(
                                    psb[b][:, :], lhsT=wA[:, kc, cbase + hp * 128:cbase + (hp + 1) * 128],
                                    rhs=xT[:, kc, nck * 512:(nck + 1) * 512], start=(kc == 0), stop=(kc == 7)),
                                    reads=[t["wA"]] + xT_all[nck * 8:nck * 8 + 8], writes=[pst[b]])
                            for lo in range(2):
                                h = 2 * hp + lo
                                if lo == 0:
                                    sy.op("vector", lambda e, b=b, dst=dst, h=h, nck=nck: e.tensor_copy(
                                        out=dst[:, h, nck * 512:(nck + 1) * 512], in_=psb[b][0:64, :]),
                                        reads=[pst[b]], writes=[t[(which, h)]])
                                else:
                                    sy.op("scalar", lambda e, b=b, dst=dst, h=h, nck=nck: e.copy(
                                        out=dst[:, h, nck * 512:(nck + 1) * 512], in_=psb[b][64:128, :]),
                                        reads=[pst[b]], writes=[t[(which, h)]])
                for n in range(nblk):
                    for r in range(d):
                        ti = n * d + r
                        b = next_ps()
                        base = n * 128 * d + r
                        for kc in range(8):
                            sy.op("tensor", lambda e, b=b, kc=kc, base=base, d=d: e.matmul(
                                psb[b][:, 0:256], lhsT=xT[:, kc, SSL(base, d)] if d > 1 else xT[:, kc, base:base + 128],
                                rhs=wA[:, kc, 512:768], start=(kc == 0), stop=(kc == 7)),
                                reads=[t["wA"]] + xT_all, writes=[pst[b]])
                        sy.op("vector", lambda e, b=b, ti=ti: e.tensor_copy(
                            out=Vc[:, ti, :, 0:64], in_=psb[b][:, 0:256].rearrange("p (h e) -> p h e", h=4)),
                            reads=[pst[b]], writes=[t[("V", ti)]])
                        fcol = 1 if own else 0
                        sy.op("gpsimd", lambda e, ti=ti, fcol=fcol: e.tensor_copy(
                            out=Vc[:, ti, :, 64:128], in_=vflag[:, None, fcol:fcol + 1].to_broadcast([128, 4, 64])),
                            reads=[tC["vflag"]], writes=[t[("Vf", ti)]])
                if own:
                    pairs = [(r, n) for n in range(nblk) for r in range(d)]
                    units = [(h, p0, half) for h in range(4) for p0 in range(0, 16, 4) for half in range(2)]
                    pvbank = {}
                    ptidx = {}

                    def ksl_of(h, r, n, pc):
                        if pc == 1:
                            return (Kc[:, h, SSL(n * 128 * d + r, d)] if d > 1 else Kc[:, h, n * 128:(n + 1) * 128]), [t[("k", h)]]
                        if n > 0:
                            return (Kc[:, h, SSL((n - 1) * 128 * d + r, d)] if d > 1 else Kc[:, h, (n - 1) * 128:n * 128]), [t[("k", h)]]
                        return (Kp[g][:, h, SSL(r, d)] if d > 1 else Kp[g][:, h, 0:128]), [t[("Kp", g)]]

                    def vsl_of(h, r, n, pc):
                        if pc == 1:
                            return Vc[:, n * d + r, h, :], [t[("V", n * d + r)], t[("Vf", n * d + r)]]
                        if n > 0:
                            return Vc[:, (n - 1) * d + r, h, :], [t[("V", (n - 1) * d + r)], t[("Vf", (n - 1) * d + r)]]
                        return Vp[g][:, r, h, :], [t[("Vp", g)]]

                    def emit_SA(u):
                        h, p0, half = u
                        sb = next_ps()
                        sub = pairs[p0:p0 + 4][half * 2:half * 2 + 2]
                        first = True
                        for si, (r, n) in enumerate(sub):
                            qsl = Qc[:, h, SSL(n * 128 * d + r, d)] if d > 1 else Qc[:, h, n * 128:(n + 1) * 128]
                            for pc in range(2):
                                col = (si * 2 + pc) * 128
                                ksl, kr = ksl_of(h, r, n, pc)
                                sy.op("tensor", lambda e, sb=sb, col=col, ksl=ksl, qsl=qsl, first=first: e.matmul(
                                    psb[sb][:, col:col + 128], lhsT=ksl, rhs=qsl, start=first, stop=False, skip_group_check=True),
                                    reads=kr + [t[("q", h)]], writes=[pst[sb]])
                                first = False
                                sy.op("tensor", lambda e, sb=sb, col=col, h=h, pc=pc: e.matmul(
                                    psb[sb][:, col:col + 128], lhsT=ident[:], rhs=bmA[:, h, pc, :], start=False, stop=True, skip_group_check=True),
                                    reads=[tC["ident"], t["bm"]], writes=[pst[sb]])
                        return sb

                    pcount = [0]

                    def emit_restA(u, sb):
                        h, p0, half = u
                        grp = pairs[p0:p0 + 4]
                        sub = grp[half * 2:half * 2 + 2]
                        pi = pcount[0] % 3
                        pcount[0] += 1
                        pt, ptk = PT[pi], t[("PT", pi)]
                        if half == 0:
                            pvbank[(h, p0)] = next_ps()
                        pvb = pvbank[(h, p0)]
                        sy.op("scalar", lambda e, sb=sb, pt=pt: e.activation(out=pt[:], in_=psb[sb][:, :], func=AF.Exp, scale=0.125),
                              reads=[pst[sb]], writes=[ptk])
                        for si, (r, n) in enumerate(sub):
                            reg = (half * 2 + si) * 128
                            for pc in range(2):
                                col = (si * 2 + pc) * 128
                                vsl, vr = vsl_of(h, r, n, pc)
                                sy.op("tensor", lambda e, pvb=pvb, reg=reg, vsl=vsl, pt=pt, col=col, st=(half == 0 and si == 0 and pc == 0): e.matmul(
                                    psb[pvb][:, reg:reg + 128], lhsT=vsl, rhs=pt[:, col:col + 128], start=st, stop=True, skip_group_check=True),
                                    reads=vr + [ptk], writes=[pst[pvb]])
                        if half == 1:
                            r0, n0 = grp[0]
                            if d == 1:
                                dst = acc[:, h, n0 * 128:(n0 + 4) * 128]
                                src = psb[pvb][:, :]
                            else:
                                dst = acc[:, h, n0 * 128 * d:(n0 + 1) * 128 * d].rearrange("p (l r) -> p l r", r=d)[:, :, r0:r0 + 4]
                                src = psb[pvb][:, :].rearrange("p (r l) -> p l r", r=4)
                            if g == 0:
                                sy.op("vector", lambda e, dst=dst, src=src: e.tensor_copy(out=dst, in_=src),
                                      reads=[pst[pvb]], writes=[t[("acc", h)]])
                            else:
                                sy.op("vector", lambda e, dst=dst, src=src: e.tensor_tensor(out=dst, in0=dst, in1=src, op=ALU.add),
                                      reads=[pst[pvb]], writes=[t[("acc", h)]])

                    LOOKA = 2
                    pend = [emit_SA(u) for u in units[:LOOKA]]
                    for ui, u in enumerate(units):
                        if ui + LOOKA < len(units):
                            pend.append(emit_SA(units[ui + LOOKA]))
                        emit_restA(u, pend.pop(0))
                sy.op("gpsimd", lambda e, g=g, d=d: e.tensor_copy(out=Kp[g][:], in_=Kc[:, :, 2048 - 128 * d:2048]),
                      reads=[t[("k", h)] for h in range(4)], writes=[t[("Kp", g)]])
                sy.op("gpsimd", lambda e, g=g, d=d: e.tensor_copy(out=Vp[g][:], in_=Vc[:, 16 - d:16, :, :]),
                      reads=[t[("V", i)] for i in range(16)] + [t[("Vf", i)] for i in range(16)], writes=[t[("Vp", g)]])
            if own:
                for h in range(4):
                    hp, lo = h // 2, (h % 2) * 64
                    for s2 in range(2):
                        sy.op("vector", lambda e, h=h, s2=s2: e.reciprocal(out=xs[s2][0:64, :], in_=acc[64:128, h, s2 * 1024:(s2 + 1) * 1024]),
                              reads=[t[("acc", h)]], writes=[t[("xs", s2)]])
                        sy.op("vector", lambda e, h=h, hp=hp, lo=lo, s2=s2: e.tensor_tensor(
                            out=yaT[lo:lo + 64, hp, sc * 2048 + s2 * 1024:sc * 2048 + (s2 + 1) * 1024],
                            in0=acc[0:64, h, s2 * 1024:(s2 + 1) * 1024], in1=xs[s2][0:64, :], op=ALU.mult),
                            reads=[t[("acc", h)], t[("xs", s2)]], writes=[tYa[(hp, sc, lo, s2)]])
        sy.barrier()
        es.close()

    if not dbg.get("skipA"):
        stage_A()
    else:
        sy.op("gpsimd", lambda e: e.memset(yaT[:], 0.0), writes=[tYa["z"]])

    if "yaT" in dbg:
        o = P.dout("dbg_yaT", [128, 2 * OWN], BF16)
        sy.dma("sync", o[:, :], yaT[:].rearrange("p a b -> p (a b)"), reads=tYa.all(), stream="o")


    ybT = SB(es_y, "ybT", [128, 4, OWN], BF16)
    tYb = TK()

    def stage_B():
        es = contextlib.ExitStack()
        t = TK()
        xin_t = xin.rearrange("(n p) d -> n p d", p=128)
        ps_rot = [0]

        def next_ps(lo=0, hi=8):
            i = ps_rot[0]
            ps_rot[0] = i + 1
            return lo + i % (hi - lo)

        xs = [SB(es, "B_xs0", [128, D], F32)] * 2
        nload = [0]

        ps_hi = [8]

        def emit_xT(tile_u, dst, j, key):
            sI = 0
            nload[0] += 1
            sy.dma("sync", xs[sI][:], xin_t[tile_u, :, :], writes=[t[("xs", sI)]], stream=f"x{sI}")
            for half in range(2):
                b = next_ps(0, ps_hi[0])
                for kk in range(4):
                    kc = half * 4 + kk
                    sy.op("tensor", lambda e, b=b, kk=kk, kc=kc, sI=sI: e.transpose(
                        out=psb[b][:, kk * 128:(kk + 1) * 128], in_=xs[sI][:, kc * 128:(kc + 1) * 128], identity=ident[:]),
                        reads=[t[("xs", sI)], tC["ident"]], writes=[pst[b]])
                src = psb[b][:, :].rearrange("p (k c) -> p k c", k=4)
                o = dst[:, half * 4:half * 4 + 4, j * 128:(j + 1) * 128]
                if half == 0:
                    sy.op("vector", lambda e, o=o, src=src: e.tensor_copy(out=o, in_=src), reads=[pst[b]], writes=[t[(key, j, half)]])
                else:
                    sy.op("scalar", lambda e, o=o, src=src: e.copy(out=o, in_=src), reads=[pst[b]], writes=[t[(key, j, half)]])

        wst = SB(es, "B_wst", [128, 2, 768], F32)

        def load_w(dst, c0, ncol, key):
            for kc2 in range(4):
                sy.dma("gpsimd", wst[:, :, 0:ncol], w_in[kc2 * 256:(kc2 + 1) * 256, c0:c0 + ncol].rearrange("(k p) c -> p k c", p=128),
                       writes=[t["wst"]], stream="w0")
                sy.op("gpsimd", lambda e, kc2=kc2: e.tensor_copy(out=dst[:, kc2 * 2:kc2 * 2 + 2, :], in_=wst[:, :, 0:ncol]),
                      reads=[t["wst"]], writes=[t[key]])

        slcK = SB(es, "B_slcK", [67, 2, 2 * OWN], BF16)
        winK = SB(es, "B_winK", [67, 2, 36 * 128], BF16)
        slcV = SB(es, "B_slcV", [128, 64, 2, 66], BF16)
        winV = SB(es, "B_winV", [128, 36, 2, 66], BF16)
        KcT = SB(es, "B_KcT", [64, 2, 512], BF16)
        Vcm = SB(es, "B_Vcm", [128, 4, 2, 64], BF16)
        for g in range(2):
            sy.dma("gpsimd", slcK[64:67, g, :], cd["kaug"][:, :], writes=[t[("slcKaug", g)]], stream="c")
            sy.dma("gpsimd", winK[64:67, g, :], cd["kaug"][:, 28 * 128:], writes=[t[("winKaug", g)]], stream="c")
        sy.op("gpsimd", lambda e: e.tensor_copy(out=slcV[:, 0:32, :, 64:66], in_=vflag[:, None, None, 0:1].to_broadcast([128, 32, 2, 2])),
              reads=[tC["vflag"]], writes=[t["slcVf"]])
        sy.op("gpsimd", lambda e: e.tensor_copy(out=slcV[:, 32:64, :, 64:66], in_=vflag[:, None, None, 1:2].to_broadcast([128, 32, 2, 2])),
              reads=[tC["vflag"]], writes=[t["slcVf"]])
        sy.op("gpsimd", lambda e: e.tensor_copy(out=winV[:, 0:4, :, 64:66], in_=vflag[:, None, None, 0:1].to_broadcast([128, 4, 2, 2])),
              reads=[tC["vflag"]], writes=[t["winVf"]])
        sy.op("gpsimd", lambda e: e.tensor_copy(out=winV[:, 4:36, :, 64:66], in_=vflag[:, None, None, 1:2].to_broadcast([128, 32, 2, 2])),
              reads=[tC["vflag"]], writes=[t["winVf"]])

        es1 = contextlib.ExitStack()
        raw = SB(es1, "B_raw", [128, 2, 2 * OWN], BF16)
        es1b = contextlib.ExitStack()
        wB = SB(es1b, "B_wB", [128, 8, 768], BF16)
        xTc = [SB(es1b, f"B_xTc{i}", [128, 8, 512], BF16) for i in range(2)]
        load_w(wB, C_BKV, 768, "wB")
        for ch in range(16):
            xb_ = xTc[ch % 2]
            xk = ("xTc", ch % 2)
            for j in range(4):
                emit_xT(ch * 4 + j, xb_, j, xk)
            xr = [t[(xk, j, hh)] for j in range(4) for hh in range(2)]
            for (ii, dst, off, key) in () if dbg.get("noFM") else ((0, raw, 0, "rawK"), (1, raw, 0, "rawV"), (2, slcK, 0, "slcK"), (4, winK, -28 * 128, "winK")):
                if ii == 4 and ch < 7:
                    continue
                b = next_ps()
                c0 = ii * 128
                for kc in range(8):
                    sy.op("tensor", lambda e, b=b, kc=kc, c0=c0, xb_=xb_: e.matmul(
                        psb[b][:, :], lhsT=wB[:, kc, c0:c0 + 128], rhs=xb_[:, kc, :], start=(kc == 0), stop=(kc == 7)),
                        reads=[t["wB"]] + xr, writes=[pst[b]])
                pb = 64 if ii == 1 else 0
                for g in range(2):
                    o = dst[pb:pb + 64, g, ch * 512 + off:ch * 512 + off + 512]
                    if g == 0:
                        sy.op("vector", lambda e, o=o, b=b: e.tensor_copy(out=o, in_=psb[b][0:64, :]), reads=[pst[b]], writes=[t[(key, g, ch)]])
                    else:
                        sy.op("scalar", lambda e, o=o, b=b: e.copy(out=o, in_=psb[b][64:128, :]), reads=[pst[b]], writes=[t[(key, g, ch)]])
            for j in range(0 if dbg.get("noTM") else 4):
                tu = ch * 4 + j
                b = next_ps()
                for kc in range(8):
                    sy.op("tensor", lambda e, b=b, kc=kc, j=j, xb_=xb_: e.matmul(
                        psb[b][:, 0:384], lhsT=xb_[:, kc, j * 128:(j + 1) * 128], rhs=wB[:, kc, 384:768], start=(kc == 0), stop=(kc == 7)),
                        reads=[t["wB"]] + xr, writes=[pst[b]])
                sy.op("vector", lambda e, b=b, tu=tu: e.tensor_copy(
                    out=slcV[:, tu, :, 0:64], in_=psb[b][:, 0:128].rearrange("p (g e) -> p g e", g=2)),
                    reads=[pst[b]], writes=[t[("slcV", tu)]])
                if tu >= 28:
                    sy.op("scalar", lambda e, b=b, tu=tu: e.copy(
                        out=winV[:, tu - 28, :, 0:64], in_=psb[b][:, 256:384].rearrange("p (g e) -> p g e", g=2)),
                        reads=[pst[b]], writes=[t[("winV", tu - 28)]])
        sy.barrier()
        es1b.close()
        if dbg.get("stopB1"):
            if "B1" in dbg and not dbg.get("noOut"):
                o3 = P.dout("dbg_slcK", [67, 2 * 2 * OWN], BF16)
                sy.dma("sync", o3[:, :], slcK[:].rearrange("p a b -> p (a b)"), stream="o")
                o4 = P.dout("dbg_winV", [128, 36 * 2 * 66], BF16)
                sy.dma("sync", o4[:, :], winV[:].rearrange("p a b c -> p (a b c)"), stream="o")
                o5 = P.dout("dbg_raw", [128, 2 * 2 * OWN], BF16)
                sy.dma("sync", o5[:, :], raw[:].rearrange("p a b -> p (a b)"), stream="o")
            sy.barrier()
            es1.close()
            es.close()
            return
        es2 = contextlib.ExitStack()
        w1r = SB(es2, "B_w1r", [128, 32, 256], BF16)
        w1st = SB(es2, "B_w1st", [128, 4, 256], F32)
        posT = SB(es2, "B_posT", [128, 32], F32)
        posTb = SB(es2, "B_posTb", [128, 32], BF16)
        w2s = SB(es2, "B_w2s", [128, 2, 64], F32)
        w2b = SB(es2, "B_w2b", [128, 2, 64], BF16)
        hb = SB(es2, "B_hb", [128, 2], F32)
        h1 = SB(es2, "B_h1", [128, 512], F32)
        h1x = SB(es2, "B_h1x", [128, 512], F32)
        h1T = SB(es2, "B_h1T", [128, 2, 512], BF16)
        cvalid = SB(es2, "B_cvalid", [128, 4], F32)
        sy.dma("sync", cvalid[:], cd["cvalid"][:, :], writes=[t["cvalid"]], stream="c")
        sy.op("gpsimd", lambda e: e.memset(h1T[:], 0.0), writes=[t["h1T"]])
        for kv, PB in (("k", 0), ("v", 64)):
            for pp in range(8):
                sy.dma("sync", w1st[PB:PB + 64].rearrange("e p h -> e (p h)"), cd[f"w1r_{kv}"][:, pp * 1024:(pp + 1) * 1024], writes=[t["w1st"]], stream="c2")
                sy.op("gpsimd", lambda e, pp=pp, PB=PB: e.tensor_copy(out=w1r[PB:PB + 64, pp * 4:pp * 4 + 4, :], in_=w1st[PB:PB + 64]), reads=[t["w1st"]], writes=[t["w1r"]])
            sy.dma("sync", posT[PB:PB + 64, :], cd[f"posT_{kv}"][:, :], writes=[t["posT"]], stream="c2")
            sy.op("gpsimd", lambda e, PB=PB: e.tensor_copy(out=posTb[PB:PB + 64, :], in_=posT[PB:PB + 64, :]), reads=[t["posT"]], writes=[t["posTb"]])
            sy.dma("sync", w2s[:], cd[f"w2_{kv}"].rearrange("(c p) e -> p c e", p=128), writes=[t["w2s"]], stream="c2")
            sy.op("gpsimd", lambda e: e.tensor_copy(out=w2b[:], in_=w2s[:]), reads=[t["w2s"]], writes=[t["w2b"]])
            b = next_ps()
            for hc in range(2):
                for p in range(32):
                    sy.op("tensor", lambda e, b=b, hc=hc, p=p, PB=PB: e.matmul(
                        psb[b][:, hc:hc + 1], lhsT=w1r[PB:PB + 64, p, hc * 128:(hc + 1) * 128], rhs=posTb[PB:PB + 64, p:p + 1],
                        start=(p == 0 and hc == 0), stop=(p == 31), skip_group_check=True),
                        reads=[t["w1r"], t["posTb"]], writes=[pst[b]])
            sy.op("vector", lambda e, b=b: e.tensor_copy(out=hb[:], in_=psb[b][:, 0:2]), reads=[pst[b]], writes=[t["hb"]])
            rawr = [t[("rawK" if kv == "k" else "rawV", g, ch)] for g in range(2) for ch in range(16)]
            for g in range(2):
                for hc in range(2):
                    b = next_ps()
                    for p in range(32):
                        sy.op("tensor", lambda e, b=b, hc=hc, p=p, g=g, PB=PB: e.matmul(
                            psb[b][:, 0:511], lhsT=w1r[PB:PB + 64, p, hc * 128:(hc + 1) * 128], rhs=raw[PB:PB + 64, g, slice(p, p + 16 * 510 + 1, 16)],
                            start=(p == 0), stop=(p == 31)),
                            reads=[t["w1r"]] + rawr, writes=[pst[b]])
                    sy.op("vector", lambda e, b=b, hc=hc: e.tensor_scalar(out=h1[:, 0:511], in0=psb[b][:, 0:511], scalar1=hb[:, hc:hc + 1], scalar2=None, op0=ALU.add),
                          reads=[pst[b], t["hb"]], writes=[t["h1"]])
                    sy.op("vector", lambda e: e.tensor_tensor(out=h1x[:, 0:511], in0=h1[:, 0:511], in1=h1[:, 0:511], op=ALU.mult),
                          reads=[t["h1"]], writes=[t["h1x"]])
                    sy.op("vector", lambda e: e.tensor_scalar(out=h1x[:, 0:511], in0=h1x[:, 0:511], scalar1=0.044715, scalar2=1.0, op0=ALU.mult, op1=ALU.add),
                          reads=[t["h1x"]], writes=[t["h1x"]])
                    sy.op("vector", lambda e: e.tensor_tensor(out=h1x[:, 0:511], in0=h1x[:, 0:511], in1=h1[:, 0:511], op=ALU.mult),
                          reads=[t["h1"], t["h1x"]], writes=[t["h1x"]])
                    sy.op("scalar", lambda e: e.activation(out=h1x[:, 0:511], in_=h1x[:, 0:511], func=AF.Sigmoid, scale=1.5957691216057308),
                          reads=[t["h1x"]], writes=[t["h1x"]])
                    sy.op("vector", lambda e, hc=hc: e.tensor_tensor(out=h1T[:, hc, 0:511], in0=h1x[:, 0:511], in1=h1[:, 0:511], op=ALU.mult),
                          reads=[t["h1"], t["h1x"]], writes=[t["h1T"]])
                if kv == "k":
                    b = next_ps()
                    for hc in range(2):
                        sy.op("tensor", lambda e, b=b, hc=hc: e.matmul(psb[b][0:64, :], lhsT=w2b[:, hc, :], rhs=h1T[:, hc, :], start=(hc == 0), stop=(hc == 1)),
                              reads=[t["w2b"], t["h1T"]], writes=[pst[b]])
                    sy.op("vector", lambda e, b=b, g=g: e.tensor_copy(out=KcT[:, g, :], in_=psb[b][0:64, :]), reads=[pst[b]], writes=[t[("KcT", g)]])
                else:
                    b = next_ps()
                    for ct in range(4):
                        for hc in range(2):
                            sy.op("tensor", lambda e, b=b, hc=hc, ct=ct: e.matmul(
                                psb[b][:, ct * 64:(ct + 1) * 64], lhsT=h1T[:, hc, ct * 128:(ct + 1) * 128], rhs=w2b[:, hc, :],
                                start=(hc == 0 and ct == 0), stop=(hc == 1), skip_group_check=True),
                                reads=[t["w2b"], t["h1T"]], writes=[pst[b]])
                    for ct in range(4):
                        sy.op("vector", lambda e, b=b, g=g, ct=ct: e.tensor_scalar(
                            out=Vcm[:, ct, g, :], in0=psb[b][:, ct * 64:(ct + 1) * 64], scalar1=cvalid[:, ct:ct + 1], scalar2=None, op0=ALU.mult),
                            reads=[pst[b], t["cvalid"]], writes=[t[("Vcm", g)]])
        sy.barrier()
        es2.close()
        es1.close()
        if "B2" in dbg:
            o1 = P.dout("dbg_KcT", [64, 1024], BF16)
            sy.dma("sync", o1[:, :], KcT[:].rearrange("p a b -> p (a b)"), reads=t.all(), stream="o")
            o2 = P.dout("dbg_Vcm", [128, 512], BF16)
            sy.dma("sync", o2[:, :], Vcm[:].rearrange("p a b c -> p (a b c)"), reads=t.all(), stream="o")
            o3 = P.dout("dbg_slcK", [67, 2 * 2 * OWN], BF16)
            sy.dma("sync", o3[:, :], slcK[:].rearrange("p a b -> p (a b)"), reads=t.all(), stream="o")
            o4 = P.dout("dbg_winV", [128, 36 * 2 * 66], BF16)
            sy.dma("sync", o4[:, :], winV[:].rearrange("p a b c -> p (a b c)"), reads=t.all(), stream="o")
        if dbg.get("stopB2"):
            sy.barrier()
            es.close()
            return

        ps_hi[0] = 6
        wq = SB(es, "B_wq", [128, 8, 512], BF16)
        wg = SB(es, "B_wg", [128, 8, 24], BF16)
        load_w(wq, C_BQ, 512, "wq")
        load_w(wg, C_BG, 24, "wg")
        cs = {}
        for nm, shp, dt in (("mcb", [128, 17, 128], BF16), ("validrep", [128, 4, 128], BF16), ("ovl", [128, 4, 128], BF16),
                            ("wd", [128, 190], F32), ("fbvec", [128, 128], F32), ("indbig", [128, 64, 128], BF16),
                            ("tri_le", [128, 128], BF16), ("tri_gt", [128, 128], BF16)):
            cs[nm] = SB(es, "Bc_" + nm, shp, dt)
            dst = cs[nm][:]
            if len(shp) == 3:
                dst = dst.rearrange("p a b -> p (a b)")
            sy.dma("sync", dst, cd[nm][:, :], writes=[t["c_" + nm]], stream="c")
        xT1 = [SB(es, f"B_xT1{i}", [128, 8, 128], BF16) for i in range(2)]
        qTa = [SB(es, f"B_qTa{i}", [67, 8, 128], BF16) for i in range(2)]
        gates = [SB(es, f"B_gates{i}", [128, 24], F32) for i in range(2)]
        PcT = SB(es, "B_PcT", [128, 4, 512], BF16)
        Pn = SB(es, "B_Pn", [128, 4, 512], BF16)
        rden = SB(es, "B_rden", [128, 512], F32)
        score = SB(es, "B_score", [128, 128], F32)
        work = SB(es, "B_work", [128, 128], F32)
        pen = SB(es, "B_pen", [128, 128], F32)
        pen2 = SB(es, "B_pen2", [128, 128], F32)
        m8a = SB(es, "B_m8a", [128, 8], F32)
        m8b = SB(es, "B_m8b", [128, 8], F32)
        penT = [SB(es, f"B_penT{g}", [128, 4, 128], BF16) for g in range(2)]
        PT = [SB(es, f"B_PT{i}", [128, 512], BF16) for i in range(3)]
        oc_sb = SB(es, "B_oc", [128, 2, 4, 64], F32)
        os_sb = SB(es, "B_os", [128, 2, 4, 66], F32)
        ow_sb = SB(es, "B_ow", [128, 2, 4, 66], F32)
        rs = SB(es, "B_rs", [128, 2, 4, 2], F32)
        yb_tm = SB(es, "B_ybtm", [128, 512], F32)
        ytmp = SB(es, "B_ytmp", [128, 64], F32)
        OSB, OWB = 6, 7
        qaug3 = cd["qaug"].rearrange("r (h n) -> r h n", h=8)
        ptc = [0]

        def attn_tiles(i, g, kts, Ksrc, koff, Vsrc, accb, with_pen, qa, qk):
            kts = list(kts)
            LOOK = 2

            def emit_S(kt):
                sb = next_ps(0, 6)
                kl = kt - koff
                sy.op("tensor", lambda e, sb=sb, kl=kl: e.matmul(
                    psb[sb][:, :], lhsT=Ksrc[0:67, g, kl * 128:(kl + 1) * 128], rhs=qa[0:67, 4 * g:4 * g + 4, :], start=True, stop=False),
                    reads=[qk[0], qk[1]], writes=[pst[sb]])
                adds = []
                if with_pen:
                    adds.append((cs["indbig"][:, kt, :], penT[g][:], [t["c_indbig"], t[("penT", g)]]))
                if kt == 32 + i:
                    adds.append((identb[:], cs["tri_le"][:, None, :].to_broadcast([128, 4, 128]), [tC["identb"], t["c_tri_le"]]))
                if (not with_pen) and kt == 28 + i:
                    adds.append((identb[:], cs["tri_gt"][:, None, :].to_broadcast([128, 4, 128]), [tC["identb"], t["c_tri_gt"]]))
                for (l_, r_, rd) in adds:
                    sy.op("tensor", lambda e, sb=sb, l_=l_, r_=r_: e.matmul(psb[sb][:, :], lhsT=l_, rhs=r_, start=False, stop=True),
                          reads=rd, writes=[pst[sb]])
                return sb

            def emit_rest(kt, sb, first):
                kl = kt - koff
                pi = ptc[0] % 3
                ptc[0] += 1
                sy.op("scalar", lambda e, sb=sb, pi=pi: e.activation(out=PT[pi][:], in_=psb[sb][:, :], func=AF.Exp, scale=0.125),
                      reads=[pst[sb]], writes=[t[("PT", pi)]])
                for r in range(4):
                    sy.op("tensor", lambda e, r=r, pi=pi, kl=kl, st=(first and r == 0): e.matmul(
                        psb[accb][:, r * 66:(r + 1) * 66], lhsT=PT[pi][:, r * 128:(r + 1) * 128], rhs=Vsrc[:, kl, g, :],
                        start=st, stop=True, skip_group_check=True),
                        reads=[t[("PT", pi)]], writes=[pst[accb]])

            pend = [emit_S(kt) for kt in kts[:LOOK]]
            for n, kt in enumerate(kts):
                if n + LOOK < len(kts):
                    pend.append(emit_S(kts[n + LOOK]))
                emit_rest(kt, pend.pop(0), n == 0)

        for i in range(NT):
            xb_ = xT1[i % 2]
            xk = ("xT1", i % 2)
            emit_xT(32 + i, xb_, 0, xk)
            xr = [t[(xk, 0, 0)], t[(xk, 0, 1)]]
            qa = qTa[i % 2]
            qk = (t[("qTa", i % 2)], t[("qTaug", i % 2)])
            sy.dma("gpsimd", qa[64:67, :, :], qaug3[:, :, i * 128:(i + 1) * 128], writes=[qk[1]], stream="qa")
            for g in range(2):
                b = next_ps(0, 6)
                for r in range(4):
                    hh = 4 * g + r
                    for kc in range(8):
                        sy.op("tensor", lambda e, b=b, r=r, hh=hh, kc=kc, xb_=xb_: e.matmul(
                            psb[b][0:64, r * 128:(r + 1) * 128], lhsT=wq[:, kc, hh * 64:(hh + 1) * 64], rhs=xb_[:, kc, :],
                            start=(kc == 0 and r == 0), stop=(kc == 7), skip_group_check=True),
                            reads=[t["wq"]] + xr, writes=[pst[b]])
                sy.op("vector", lambda e, b=b, g=g, qa=qa: e.tensor_copy(
                    out=qa[0:64, 4 * g:4 * g + 4, :], in_=psb[b][0:64, :].rearrange("p (r n) -> p r n", r=4)),
                    reads=[pst[b]], writes=[qk[0]])
            b = next_ps(0, 6)
            for kc in range(8):
                sy.op("tensor", lambda e, b=b, kc=kc, xb_=xb_: e.matmul(psb[b][:, 0:24], lhsT=xb_[:, kc, :], rhs=wg[:, kc, :], start=(kc == 0), stop=(kc == 7)),
                      reads=[t["wg"]] + xr, writes=[pst[b]])
            gt = gates[i % 2]
            gk = t[("gates", i % 2)]
            sy.op("scalar", lambda e, b=b, gt=gt: e.activation(out=gt[:], in_=psb[b][:, 0:24], func=AF.Sigmoid), reads=[pst[b]], writes=[gk])
            for g in range(2):
                ctmax = (262 + 8 * i) // 128
                ncts = ctmax + 1
                for ct in range(ncts):
                    sb = next_ps(0, 6)
                    off = 254 + 8 * i - 128 * ct
                    need_mask = off < 127
                    sy.op("tensor", lambda e, sb=sb, ct=ct, nm=need_mask: e.matmul(
                        psb[sb][:, :], lhsT=KcT[:, g, ct * 128:(ct + 1) * 128], rhs=qa[0:64, 4 * g:4 * g + 4, :], start=True, stop=(not nm)),
                        reads=[t[("KcT", g)], qk[0]], writes=[pst[sb]])
                    if need_mask:
                        idx = (off + 2) // 8
                        assert 0 <= idx < 17, (i, ct, off)
                        sy.op("tensor", lambda e, sb=sb, idx=idx: e.matmul(
                            psb[sb][:, :], lhsT=identb[:], rhs=cs["mcb"][:, idx:idx + 1, :].to_broadcast([128, 4, 128]), start=False, stop=True),
                            reads=[tC["identb"], t["c_mcb"]], writes=[pst[sb]])
                    sy.op("scalar", lambda e, sb=sb, ct=ct: e.activation(out=PcT[:, ct, :], in_=psb[sb][:, :], func=AF.Exp, scale=0.125),
                          reads=[pst[sb]], writes=[t[("PcT", ct)]])
                db = next_ps(0, 6)
                for ct in range(ncts):
                    sy.op("tensor", lambda e, db=db, ct=ct: e.matmul(psb[db][:, :], lhsT=cs["validrep"][:, ct, :], rhs=PcT[:, ct, :], start=(ct == 0), stop=(ct == ncts - 1)),
                          reads=[t["c_validrep"], t[("PcT", ct)]], writes=[pst[db]])
                sy.op("vector", lambda e, db=db: e.tensor_scalar(out=rden[:], in0=psb[db][:, :], scalar1=1e-30, scalar2=None, op0=ALU.max),
                      reads=[pst[db]], writes=[t["rden"]])
                sy.op("vector", lambda e: e.reciprocal(out=rden[:], in_=rden[:]), reads=[t["rden"]], writes=[t["rden"]])
                for ct in range(ncts):
                    sy.op("vector" if ct % 2 == 0 else "gpsimd", lambda e, ct=ct: e.tensor_tensor(out=Pn[:, ct, :], in0=PcT[:, ct, :], in1=rden[:], op=ALU.mult),
                          reads=[t[("PcT", ct)], t["rden"]], writes=[t[("Pn", ct)]])
                ob = next_ps(0, 6)
                firstm = True
                for r in range(4):
                    for ct in range(ncts):
                        sy.op("tensor", lambda e, ob=ob, r=r, ct=ct, st=firstm: e.matmul(
                            psb[ob][:, r * 64:(r + 1) * 64], lhsT=Pn[:, ct, r * 128:(r + 1) * 128], rhs=Vcm[:, ct, g, :],
                            start=st, stop=True, skip_group_check=True),
                            reads=[t[("Pn", ct)], t[("Vcm", g)]], writes=[pst[ob]])
                        firstm = False
                for r in range(4):
                    for ct in range(ncts):
                        sy.op("tensor", lambda e, ob=ob, r=r, ct=ct: e.matmul(
                            psb[ob][:, 256:384], lhsT=Pn[:, ct, r * 128:(r + 1) * 128], rhs=cs["ovl"][:, ct, :],
                            start=False, stop=True, skip_group_check=True),
                            reads=[t[("Pn", ct)], t["c_ovl"]], writes=[pst[ob]])
                sy.op("scalar", lambda e, ob=ob, g=g: e.copy(out=oc_sb[:, g, :, :], in_=psb[ob][:, 0:256].rearrange("p (r e) -> p r e", r=4)),
                      reads=[pst[ob]], writes=[t[("oc", g)]])
                sy.op("vector", lambda e, ob=ob: e.tensor_tensor(out=score[:], in0=psb[ob][:, 256:384], in1=cs["wd"][:, 62 - 2 * i:190 - 2 * i], op=ALU.add),
                      reads=[pst[ob], t["c_wd"]], writes=[t["score"]])
                sy.op("vector", lambda e: e.tensor_tensor(out=score[:], in0=score[:], in1=cs["fbvec"][:], op=ALU.add),
                      reads=[t["c_fbvec"]], writes=[t["score"]])
                sy.op("vector", lambda e: e.max(out=m8a[:], in_=score[:]), reads=[t["score"]], writes=[t["m8a"]])
                sy.op("vector", lambda e: e.match_replace(out=work[:], in_to_replace=m8a[:], in_values=score[:], imm_value=-3.0e38),
                      reads=[t["score"], t["m8a"]], writes=[t["work"]])
                sy.op("vector", lambda e: e.max(out=m8b[:], in_=work[:]), reads=[t["work"]], writes=[t["m8b"]])
                sy.op("vector", lambda e: e.tensor_scalar(out=pen[:], in0=score[:], scalar1=m8b[:, 7:8], scalar2=NEG, op0=ALU.is_lt, op1=ALU.mult),
                      reads=[t["score"], t["m8b"]], writes=[t["pen"]])
                sy.op("vector", lambda e: e.tensor_scalar(out=pen2[:], in0=score[:], scalar1=-5.0e8, scalar2=NEG, op0=ALU.is_lt, op1=ALU.mult),
                      reads=[t["score"]], writes=[t["pen2"]])
                sy.op("vector", lambda e: e.tensor_tensor(out=pen[:], in0=pen[:], in1=pen2[:], op=ALU.min),
                      reads=[t["pen2"]], writes=[t["pen"]])
                tb = next_ps(0, 6)
                sy.op("tensor", lambda e, tb=tb: e.transpose(out=psb[tb][:, 0:128], in_=pen[:], identity=ident[:]),
                      reads=[t["pen"], tC["ident"]], writes=[pst[tb]])
                sy.op("vector", lambda e, tb=tb, g=g: e.tensor_copy(out=penT[g][:], in_=psb[tb][:, None, 0:128].to_broadcast([128, 4, 128])),
                      reads=[pst[tb]], writes=[t[("penT", g)]])
                attn_tiles(i, g, range(28 + i, 33 + i), winK, 28, winV, OWB, False, qa, qk)
                sy.op("vector", lambda e, g=g: e.tensor_copy(out=ow_sb[:, g, :, :], in_=psb[OWB][:, 0:264].rearrange("p (r e) -> p r e", r=4)),
                      reads=[pst[OWB]], writes=[t[("ow", g)]])
                attn_tiles(i, g, range(0, 33 + i), slcK, 0, slcV, OSB, True, qa, qk)
                sy.op("vector", lambda e, g=g: e.tensor_copy(out=os_sb[:, g, :, :], in_=psb[OSB][:, 0:264].rearrange("p (r e) -> p r e", r=4)),
                      reads=[pst[OSB]], writes=[t[("os", g)]])
            gt3 = gt[:].rearrange("p (g r b) -> p g r b", g=2, r=4)
            sy.op("vector", lambda e: e.reciprocal(out=rs[:, :, :, 0:1], in_=os_sb[:, :, :, 64:65]), reads=[t[("os", 0)], t[("os", 1)]], writes=[t["rs"]])
            sy.op("vector", lambda e: e.reciprocal(out=rs[:, :, :, 1:2], in_=ow_sb[:, :, :, 64:65]), reads=[t[("ow", 0)], t[("ow", 1)]], writes=[t["rs"]])
            sy.op("vector", lambda e, gt3=gt3: e.tensor_tensor(out=rs[:], in0=rs[:], in1=gt3[:, :, :, 1:3], op=ALU.mult), reads=[gk], writes=[t["rs"]])
            for g in range(2):
                for r in range(4):
                    col = (g * 4 + r) * 64
                    gc = gt[:, g * 12 + r * 3:g * 12 + r * 3 + 1]
                    sy.op("vector", lambda e, g=g, r=r, gc=gc: e.tensor_scalar(out=ytmp[:], in0=oc_sb[:, g, r, :], scalar1=gc, scalar2=None, op0=ALU.mult),
                          reads=[t[("oc", g)], gk], writes=[t["ytmp"]])
                    sy.op("vector", lambda e, g=g, r=r: e.scalar_tensor_tensor(out=ytmp[:], in0=os_sb[:, g, r, 0:64], scalar=rs[:, g, r, 0:1], in1=ytmp[:], op0=ALU.mult, op1=ALU.add),
                          reads=[t[("os", g)], t["rs"]], writes=[t["ytmp"]])
                    sy.op("vector", lambda e, g=g, r=r, col=col: e.scalar_tensor_tensor(out=yb_tm[:, col:col + 64], in0=ow_sb[:, g, r, 0:64], scalar=rs[:, g, r, 1:2], in1=ytmp[:], op0=ALU.mult, op1=ALU.add),
                          reads=[t[("ow", g)], t["rs"], t["ytmp"]], writes=[t["ybtm"]])
            tb = next_ps(0, 6)
            for c4 in range(4):
                sy.op("tensor", lambda e, tb=tb, c4=c4: e.transpose(out=psb[tb][:, c4 * 128:(c4 + 1) * 128], in_=yb_tm[:, c4 * 128:(c4 + 1) * 128], identity=ident[:]),
                      reads=[t["ybtm"], tC["ident"]], writes=[pst[tb]])
            sy.op("scalar", lambda e, tb=tb, i=i: e.copy(out=ybT[:, :, i * 128:(i + 1) * 128], in_=psb[tb][:, :].rearrange("p (c n) -> p c n", c=4)),
                  reads=[pst[tb]], writes=[tYb[i]])
        sy.barrier()
        es.close()

    sy.new_epoch()
    if not dbg.get("skipB"):
        stage_B()
    else:
        for i in range(NT):
            sy.op("gpsimd", lambda e, i=i: e.memset(ybT[:, :, i * 128:(i + 1) * 128], 0.0), writes=[tYb[i]])

    if "ybT" in dbg:
        o = P.dout("dbg_ybT", [128, 4 * OWN], BF16)
        sy.dma("sync", o[:, :], ybT[:].rearrange("p a b -> p (a b)"), reads=tYb.all(), stream="o")

    h1T_d = P.dscratch("h1T_scratch", [NT, 128, 8 * 128], BF16)
    h1_t = h1_d.rearrange("(n p) d -> n p d", p=128)

    def layer_norm(t, z, zk, lnrep, dst, dstk, tmp_stats, tmp_mv):
        for hh in range(2):
            sy.op("vector", lambda e, hh=hh: e.bn_stats(out=tmp_stats[:, hh * 6:(hh + 1) * 6], in_=z[:, hh * 512:(hh + 1) * 512]),
                  reads=[zk], writes=[t["lnst"]])
        sy.op("vector", lambda e: e.bn_aggr(out=tmp_mv[:, 0:2], in_=tmp_stats[:, 0:12]), reads=[t["lnst"]], writes=[t["lnmv"]])
        sy.op("vector", lambda e: e.tensor_scalar(out=tmp_mv[:, 2:3], in0=tmp_mv[:, 1:2], scalar1=LN_EPS, scalar2=None, op0=ALU.add),
              reads=[t["lnmv"]], writes=[t["lnmv"]])
        sy.op("scalar", lambda e: e.activation(out=tmp_mv[:, 2:3], in_=tmp_mv[:, 2:3], func=AF.Sqrt), reads=[t["lnmv"]], writes=[t["lnmv"]])
        sy.op("vector", lambda e: e.reciprocal(out=tmp_mv[:, 3:4], in_=tmp_mv[:, 2:3]), reads=[t["lnmv"]], writes=[t["lnmv"]])
        sy.op("vector", lambda e: e.tensor_scalar(out=dst[:], in0=z[:], scalar1=tmp_mv[:, 0:1], scalar2=tmp_mv[:, 3:4], op0=ALU.subtract, op1=ALU.mult),
              reads=[zk, t["lnmv"]], writes=[dstk])
        sy.op("gpsimd", lambda e: e.tensor_tensor(out=dst[:], in0=dst[:], in1=lnrep[:, 0, :], op=ALU.mult), reads=[t["ln"]], writes=[dstk])
        sy.op("gpsimd", lambda e: e.tensor_tensor(out=dst[:], in0=dst[:], in1=lnrep[:, 1, :], op=ALU.add), reads=[t["ln"]], writes=[dstk])

    def stage_C():
        es = contextlib.ExitStack()
        t = TK()
        xin_t = xin.rearrange("(n p) d -> n p d", p=128)
        ps_rot = [0]

        def next_ps():
            i = ps_rot[0]
            ps_rot[0] = i + 1
            return i % 8

        lnrep = SB(es, "C_lnrep", [128, 2, D], F32)
        sy.dma("sync", lnrep[:].rearrange("p a d -> p (a d)"), cd["lnrep"][:, 0:2 * D], writes=[t["ln"]], stream="c")
        wst = SB(es, "C_wst", [128, 2, 1024], F32)
        wM = SB(es, "C_wM", [128, 8, 2048], BF16)
        wAB = SB(es, "C_wAB", [128, 6, D], BF16)
        wO = SB(es, "C_wO", [128, 8, D], BF16)
        wR = SB(es, "C_wR", [128, 8, 36], F32)
        brep = SB(es, "C_brep", [128, 36], F32)
        xs = [SB(es, f"C_xs{i}", [128, D], F32) for i in range(2)]
        xT1 = SB(es, "C_xT1", [128, 8, 128], BF16)
        gT = SB(es, "C_gT", [128, 16, 128], F32)
        mT = SB(es, "C_mT", [128, 8, 128], BF16)
        tmp1 = SB(es, "C_tmp1", [128, 128], F32)
        tmp2 = SB(es, "C_tmp2", [128, 128], F32)
        z = SB(es, "C_z", [128, D], F32)
        h1 = [SB(es, f"C_h1{i}", [128, D], F32) for i in range(2)]
        h1T32 = SB(es, "C_h1T32", [128, 8, 128], F32)
        h1Tb = [SB(es, f"C_h1Tb{i}", [128, 8, 128], BF16) for i in range(2)]
        st6 = SB(es, "C_st6", [128, 12], F32)
        mv = SB(es, "C_mv", [128, 4], F32)
        lg = SB(es, "C_lg", [128, 36], F32)
        rt = SB(es, "C_rt", [128, 64], F32)
        m8 = SB(es, "C_m8", [128, 8], F32)

        def load_wgen(dst, src2d, rows, ncol, key):
            for r2 in range(rows // 256):
                sy.dma("gpsimd", wst[:, :, 0:ncol], src2d[r2 * 256:(r2 + 1) * 256, :].rearrange("(k p) c -> p k c", p=128),
                       writes=[t["wst"]], stream="w0")
                sy.op("gpsimd", lambda e, r2=r2: e.tensor_copy(out=dst[:, r2 * 2:r2 * 2 + 2, :], in_=wst[:, :, 0:ncol]),
                      reads=[t["wst"]], writes=[t[key]])

        load_wgen(wM[:, :, 0:1024], w_in[:, C_MG:C_MG + 1024], D, 1024, "wM")
        load_wgen(wM[:, :, 1024:2048], w_in[:, C_MG + 1024:C_MG + 2048], D, 1024, "wM")
        load_wgen(wAB[:, 0:2, :], cd["w_ba"], 256, D, "wAB")
        load_wgen(wAB[:, 2:6, :], cd["w_bb"], 512, D, "wAB")
        load_wgen(wO, cd["w_out"], D, D, "wO")
        sy.dma("sync", wR[:], cd["w_router"].rearrange("(k p) c -> p k c", p=128), writes=[t["wR"]], stream="c")
        sy.dma("sync", brep[:], cd["b_router"][:, :], writes=[t["brep"]], stream="c")

        for i in range(NT):
            sI = i % 2
            sy.dma("sync", xs[sI][:], xin_t[32 + i, :, :], writes=[t[("xs", sI)]], stream=f"x{sI}")
            for half in range(2):
                b = next_ps()
                for kk in range(4):
                    kc = half * 4 + kk
                    sy.op("tensor", lambda e, b=b, kk=kk, kc=kc, sI=sI: e.transpose(
                        out=psb[b][:, kk * 128:(kk + 1) * 128], in_=xs[sI][:, kc * 128:(kc + 1) * 128], identity=ident[:]),
                        reads=[t[("xs", sI)], tC["ident"]], writes=[pst[b]])
                sy.op("vector" if half == 0 else "scalar",
                      (lambda e, b=b, half=half: e.tensor_copy(out=xT1[:, half * 4:half * 4 + 4, :], in_=psb[b][:, :].rearrange("p (k c) -> p k c", k=4))) if half == 0 else
                      (lambda e, b=b, half=half: e.copy(out=xT1[:, half * 4:half * 4 + 4, :], in_=psb[b][:, :].rearrange("p (k c) -> p k c", k=4))),
                      reads=[pst[b]], writes=[t[("xT1", half)]])
            xr = [t[("xT1", 0)], t[("xT1", 1)]]
            tok = slice(i * 128, (i + 1) * 128)
            for c4 in range(4):
                b = next_ps()
                for cc in range(4):
                    ct = c4 * 4 + cc
                    for kc in range(8):
                        sy.op("tensor", lambda e, b=b, cc=cc, ct=ct, kc=kc: e.matmul(
                            psb[b][:, cc * 128:(cc + 1) * 128], lhsT=wM[:, kc, ct * 128:(ct + 1) * 128], rhs=xT1[:, kc, :],
                            start=(kc == 0 and cc == 0), stop=(kc == 7), skip_group_check=True),
                            reads=[t["wM"]] + xr, writes=[pst[b]])
                sy.op("scalar", lambda e, b=b, c4=c4: e.activation(out=gT[:, c4 * 4:c4 * 4 + 4, :], in_=psb[b][:, :].rearrange("p (c n) -> p c n", c=4), func=AF.Sigmoid),
                      reads=[pst[b]], writes=[t[("gT", c4)]])
            for c in range(8):
                b = next_ps()
                for k2 in range(2):
                    sy.op("tensor", lambda e, b=b, c=c, k2=k2: e.matmul(
                        psb[b][:, 0:128], lhsT=wAB[:, k2, c * 128:(c + 1) * 128], rhs=yaT[:, k2, tok], start=(k2 == 0), stop=(k2 == 1), skip_group_check=True),
                        reads=[t["wAB"]] + tYa.all(), writes=[pst[b]])
                for k4 in range(4):
                    sy.op("tensor", lambda e, b=b, c=c, k4=k4: e.matmul(
                        psb[b][:, 128:256], lhsT=wAB[:, 2 + k4, c * 128:(c + 1) * 128], rhs=ybT[:, k4, tok], start=False, stop=(k4 == 3), skip_group_check=True),
                        reads=[t["wAB"], tYb[i]], writes=[pst[b]])
                sy.op("vector", lambda e, b=b, c=c: e.tensor_tensor(out=tmp1[:], in0=psb[b][:, 0:128], in1=gT[:, c, :], op=ALU.mult),
                      reads=[pst[b], t[("gT", c // 4)]], writes=[t["tmp1"]])
                sy.op("vector", lambda e, b=b, c=c: e.tensor_tensor(out=tmp2[:], in0=psb[b][:, 128:256], in1=gT[:, 8 + c, :], op=ALU.mult),
                      reads=[pst[b], t[("gT", 2 + c // 4)]], writes=[t["tmp2"]])
                sy.op("gpsimd", lambda e, c=c: e.tensor_tensor(out=mT[:, c, :], in0=tmp1[:], in1=tmp2[:], op=ALU.add),
                      reads=[t["tmp1"], t["tmp2"]], writes=[t[("mT", c)]])
            for hf2 in range(2):
                b = next_ps()
                for c in range(8):
                    sy.op("tensor", lambda e, b=b, c=c, hf2=hf2: e.matmul(
                        psb[b][:, :], lhsT=mT[:, c, :], rhs=wO[:, c, hf2 * 512:(hf2 + 1) * 512], start=(c == 0), stop=(c == 7)),
                        reads=[t["wO"], t[("mT", c)]], writes=[pst[b]])
                sy.op("vector", lambda e, b=b, hf2=hf2, sI=sI: e.scalar_tensor_tensor(
                    out=z[:, hf2 * 512:(hf2 + 1) * 512], in0=xs[sI][:, hf2 * 512:(hf2 + 1) * 512], scalar=ALPHA, in1=psb[b][:, :], op0=ALU.mult, op1=ALU.add),
                    reads=[pst[b], t[("xs", sI)]], writes=[t["z"]])
            hb_ = h1[i % 2]
            hk = t[("h1", i % 2)]
            layer_norm(t, z, t["z"], lnrep, hb_, hk, st6, mv)
            sy.dma("sync", h1_t[i, :, :], hb_[:], reads=[hk], writes=[tH[("h1d", i)]], stream="h1w")
            for half in range(2):
                b = next_ps()
                for kk in range(4):
                    kc = half * 4 + kk
                    sy.op("tensor", lambda e, b=b, kk=kk, kc=kc, hb_=hb_: e.transpose(
                        out=psb[b][:, kk * 128:(kk + 1) * 128], in_=hb_[:, kc * 128:(kc + 1) * 128], identity=ident[:]),
                        reads=[hk, tC["ident"]], writes=[pst[b]])
                src = psb[b][:, :].rearrange("p (k c) -> p k c", k=4)
                sy.op("vector", lambda e, src=src, half=half: e.tensor_copy(out=h1T32[:, half * 4:half * 4 + 4, :], in_=src),
                      reads=[pst[b]], writes=[t[("h1T32", half)]])
                sy.op("scalar", lambda e, src=src, half=half: e.copy(out=h1Tb[i % 2][:, half * 4:half * 4 + 4, :], in_=src),
                      reads=[pst[b]], writes=[t[("h1Tb", i % 2, half)]])
            sy.dma("sync", h1T_d[i, :, :], h1Tb[i % 2][:].rearrange("p k n -> p (k n)"), reads=[t[("h1Tb", i % 2, 0)], t[("h1Tb", i % 2, 1)]],
                   writes=[tH[("h1T", i)]], stream="h1w")
            b = next_ps()
            for kc in range(8):
                sy.op("tensor", lambda e, b=b, kc=kc: e.matmul(psb[b][:, 0:36], lhsT=h1T32[:, kc, :], rhs=wR[:, kc, :], start=(kc == 0), stop=(kc == 7)),
                      reads=[t[("h1T32", 0)], t[("h1T32", 1)], t["wR"]], writes=[pst[b]])
            V = lambda fn, rd, wr: sy.op("vector", fn, reads=rd, writes=wr)
            rk = t["rt"]
            V(lambda e, b=b: e.tensor_tensor(out=lg[:], in0=psb[b][:, 0:36], in1=brep[:], op=ALU.add), [pst[b], t["brep"]], [rk])
            V(lambda e: e.tensor_reduce(out=rt[:, 0:1], in_=lg[:, 0:4], axis=AX.X, op=ALU.max), [rk], [rk])
            V(lambda e: e.tensor_scalar(out=rt[:, 4:8], in0=lg[:, 0:4], scalar1=rt[:, 0:1], scalar2=None, op0=ALU.is_ge), [rk], [rk])
            V(lambda e: e.tensor_scalar(out=rt[:, 1:2], in0=rt[:, 0:1], scalar1=-1.0, scalar2=None, op0=ALU.mult), [rk], [rk])
            sy.op("scalar", lambda e: e.activation(out=rt[:, 8:12], in_=lg[:, 0:4], func=AF.Exp, bias=rt[:, 1:2], scale=1.0), reads=[rk], writes=[rk])
            V(lambda e: e.tensor_reduce(out=rt[:, 2:3], in_=rt[:, 8:12], axis=AX.X, op=ALU.add), [rk], [rk])
            V(lambda e: e.reciprocal(out=rt[:, 3:4], in_=rt[:, 2:3]), [rk], [rk])
            V(lambda e: e.tensor_scalar(out=rt[:, 8:12], in0=rt[:, 4:8], scalar1=-1.0, scalar2=1.0e9, op0=ALU.add, op1=ALU.mult), [rk], [rk])
            V(lambda e: e.tensor_tensor(out=rt[:, 16:48].rearrange("p (g e) -> p g e", g=4), in0=lg[:, 4:36].rearrange("p (g e) -> p g e", g=4),
                                        in1=rt[:, 8:12].unsqueeze(2).to_broadcast([128, 4, 8]), op=ALU.add), [rk], [rk])
            V(lambda e: e.max(out=m8[:], in_=rt[:, 16:48]), [rk], [t["m8"]])
            V(lambda e: e.tensor_tensor(out=rt[:, 12:13], in0=m8[:, 0:1], in1=m8[:, 1:2], op=ALU.subtract), [t["m8"]], [rk])
            sy.op("scalar", lambda e: e.activation(out=rt[:, 12:13], in_=rt[:, 12:13], func=AF.Sigmoid), reads=[rk], writes=[rk])
            V(lambda e: e.tensor_scalar(out=rt[:, 13:14], in0=rt[:, 12:13], scalar1=-1.0, scalar2=1.0, op0=ALU.mult, op1=ALU.add), [rk], [rk])
            V(lambda e: e.tensor_scalar(out=rt[:, 12:14], in0=rt[:, 12:14], scalar1=rt[:, 3:4], scalar2=None, op0=ALU.mult), [rk], [rk])
            V(lambda e: e.tensor_scalar(out=rt[:, 48:64], in0=rt[:, 16:32], scalar1=0.0, scalar2=None, op0=ALU.mult), [rk], [rk])
            V(lambda e: e.tensor_scalar(out=Wt[:, i, :], in0=rt[:, 16:48], scalar1=m8[:, 1:2], scalar2=rt[:, 13:14], op0=ALU.is_ge, op1=ALU.mult),
              [rk, t["m8"]], [tH[("Wt", i)]])
            V(lambda e: e.tensor_scalar(out=lg[:, 4:36], in0=rt[:, 16:48], scalar1=m8[:, 0:1], scalar2=None, op0=ALU.is_ge), [rk, t["m8"]], [rk])
            V(lambda e: e.tensor_tensor(out=rt[:, 14:15], in0=rt[:, 12:13], in1=rt[:, 13:14], op=ALU.subtract), [rk], [rk])
            V(lambda e: e.scalar_tensor_tensor(out=Wt[:, i, :], in0=lg[:, 4:36], scalar=rt[:, 14:15], in1=Wt[:, i, :], op0=ALU.mult, op1=ALU.add),
              [rk], [tH[("Wt", i)]])
        sy.barrier()
        es.close()

    sy.new_epoch()
    if not dbg.get("skipC"):
        stage_C()
    es_y.close()
    if "h1" in dbg:
        o = P.dout("dbg_h1", [OWN, D], F32)
        sy.dma("sync", o[:, :], h1_d[:, :], reads=tH.all(), stream="o")
        o = P.dout("dbg_Wt", [128, NT * 32], F32)
        sy.dma("sync", o[:, :], Wt[:].rearrange("p a b -> p (a b)"), reads=tH.all(), stream="o")

    def stage_D():
        es = contextlib.ExitStack()
        t = TK()
        out_t = out.rearrange("(n p) d -> n p d", p=128)
        yacc = SB(es, "D_yacc", [128, 16, D], F32)
        lnrep = SB(es, "D_lnrep", [128, 2, D], F32)
        sy.dma("sync", lnrep[:].rearrange("p a d -> p (a d)"), cd["lnrep"][:, 2 * D:4 * D], writes=[t["ln"]], stream="c")
        h1T = SB(es, "D_h1T", [128, 16, 8, 128], BF16)
        wgu2 = [SB(es, f"D_wgu{i}", [128, 8, 2 * D_EXP], BF16) for i in range(2)]
        wdn2 = [SB(es, f"D_wdn{i}", [128, 4, D], BF16) for i in range(2)]
        ecount = [0]
        wst = [SB(es, f"D_wst{i}", [128, 2, 1024], F32) for i in range(2)]
        aT = SB(es, "D_aT", [128, 4, 512], BF16)
        sg = [SB(es, f"D_sg{i}", [128, 512], F32) for i in range(2)]
        hz = [SB(es, f"D_hz{i}", [128, D], F32) for i in range(2)]
        st6 = SB(es, "D_st6", [128, 12], F32)
        mv = SB(es, "D_mv", [128, 4], F32)
        ps_rot = [0]

        def next_ps():
            i = ps_rot[0]
            ps_rot[0] = i + 1
            return i % 8

        wcnt = [0]
        cast_eng = ("gpsimd", "vector", "gpsimd", "scalar")
        for hh in range(2):
            for j in range(16):
                sy.op("gpsimd", lambda e, j=j: e.memset(yacc[:, j, :], 0.0), writes=[t[("yacc", j)]])
                sy.dma("sync", h1T[:, j, :, :].rearrange("p k n -> p (k n)"), h1T_d[hh * 16 + j, :, :], reads=[tH[("h1T", hh * 16 + j)]],
                       writes=[t[("h1T", j)]], stream="h1r")
            for ex in range(N_EXP):
                wb = ecount[0] % 2
                ecount[0] += 1
                wgu, wdn = wgu2[wb], wdn2[wb]
                for r2 in range(6):
                    wi = wcnt[0] % 2
                    wcnt[0] += 1
                    if r2 < 4:
                        src = cd["w_gu"][ex, r2 * 256:(r2 + 1) * 256, :].rearrange("(k p) c -> p k c", p=128)
                        dst, dk = wgu[:, r2 * 2:r2 * 2 + 2, :], ("wgu", wb)
                    else:
                        src = cd["w_dn"][ex, (r2 - 4) * 256:(r2 - 3) * 256, :].rearrange("(k p) c -> p k c", p=128)
                        dst, dk = wdn[:, (r2 - 4) * 2:(r2 - 4) * 2 + 2, :], ("wdn", wb)
                    sy.dma("sync", wst[wi][:], src, writes=[t[("wst", wi)]], stream=f"e{wi}")
                    ce = "gpsimd"
                    if ce == "scalar":
                        sy.op("scalar", lambda e, dst=dst, wi=wi: e.copy(out=dst, in_=wst[wi][:]), reads=[t[("wst", wi)]], writes=[t[dk]])
                    else:
                        sy.op(ce, lambda e, dst=dst, wi=wi: e.tensor_copy(out=dst, in_=wst[wi][:]), reads=[t[("wst", wi)]], writes=[t[dk]])
                for c4 in range(4):
                    tok0 = hh * 2048 + c4 * 512
                    hr = [t[("h1T", c4 * 4 + jj)] for jj in range(4)]
                    for cc in range(4):
                        bg = next_ps()
                        bu = next_ps()
                        for (bb, ct) in ((bg, cc), (bu, 4 + cc)):
                            for kc in range(8):
                                sy.op("tensor", lambda e, bb=bb, ct=ct, kc=kc, tok0=tok0: e.matmul(
                                    psb[bb][:, :], lhsT=wgu[:, kc, ct * 128:(ct + 1) * 128], rhs=h1T[:, c4 * 4:c4 * 4 + 4, kc, :], start=(kc == 0), stop=(kc == 7)),
                                    reads=[t[("wgu", wb)]] + hr, writes=[pst[bb]])
                        si = cc % 2
                        sy.op("scalar", lambda e, bg=bg, si=si: e.activation(out=sg[si][:], in_=psb[bg][:, :], func=AF.Silu), reads=[pst[bg]], writes=[t[("sg", si)]])
                        sy.op("vector", lambda e, bu=bu, si=si, cc=cc: e.tensor_tensor(out=aT[:, cc, :], in0=psb[bu][:, :], in1=sg[si][:], op=ALU.mult),
                              reads=[pst[bu], t[("sg", si)]], writes=[t[("aT", cc)]])
                    for jj in range(4):
                        j = c4 * 4 + jj
                        for hf2 in range(2):
                            b = next_ps()
                            for k in range(4):
                                sy.op("tensor", lambda e, b=b, k=k, jj=jj, hf2=hf2: e.matmul(
                                    psb[b][:, :], lhsT=aT[:, k, jj * 128:(jj + 1) * 128], rhs=wdn[:, k, hf2 * 512:(hf2 + 1) * 512], start=(k == 0), stop=(k == 3)),
                                    reads=[t[("wdn", wb)], t[("aT", k)]], writes=[pst[b]])
                            sy.op("vector", lambda e, b=b, j=j, hf2=hf2, ex=ex: e.scalar_tensor_tensor(
                                out=yacc[:, j, hf2 * 512:(hf2 + 1) * 512], in0=psb[b][:, :], scalar=Wt[:, hh * 16 + j, ex:ex + 1],
                                in1=yacc[:, j, hf2 * 512:(hf2 + 1) * 512], op0=ALU.mult, op1=ALU.add),
                                reads=[pst[b], tH[("Wt", hh * 16 + j)]], writes=[t[("yacc", j)]])
            for j in range(16):
                i = hh * 16 + j
                hb_ = hz[j % 2]
                hk = t[("hz", j % 2)]
                sy.dma("sync", hb_[:], h1_t[i, :, :], reads=[tH[("h1d", i)]], writes=[hk], stream="h1r")
                sy.op("vector", lambda e, j=j, hb_=hb_: e.scalar_tensor_tensor(out=yacc[:, j, :], in0=hb_[:], scalar=ALPHA, in1=yacc[:, j, :], op0=ALU.mult, op1=ALU.add),
                      reads=[hk], writes=[t[("yacc", j)]])
                layer_norm(t, yacc[:, j, :], t[("yacc", j)], lnrep, hb_, hk, st6, mv)
                sy.dma("sync", out_t[i, :, :], hb_[:], reads=[hk], stream="o")
        sy.barrier()
        es.close()

    sy.new_epoch()
    if not dbg.get("skipD"):
        stage_D()
    sy.barrier()
    S = sy.dsem.get(("sync", "o"))
    if S is not None:
        nc.sync.wait_ge(S["sem"], 16 * S["cnt"])
    return P


def make_core_map(inputs, W, b, hf, names):
    x = np.asarray(inputs["x"][b], dtype=np.float32)
    if hf == 1:
        xin = x
    else:
        xin = np.concatenate([np.zeros((OWN, D), np.float32), x[:OWN]], axis=0)
    m = {"xin": np.ascontiguousarray(xin)}
    m.update(W)
    m.update(make_consts(hf))
    return {k: m[k] for k in names}


_PROG = None


def kernel(**inputs):
    global _PROG
    inputs = {k: np.asarray(v) for k, v in inputs.items()}
    if _PROG is None:
        _PROG = build_program()
    P = _PROG
    W = weight_layouts(inputs)
    names = list(P.ins)
    consts = [make_consts(0), make_consts(1)]
    maps = []
    for c in range(8):
        b, hf = c // 2, c % 2
        x = np.asarray(inputs["x"][b], dtype=np.float32)
        if hf == 1:
            xin = x
        else:
            xin = np.concatenate([np.zeros((OWN, D), np.float32), x[:OWN]], axis=0)
        m = {"xin": np.ascontiguousarray(xin)}
        m.update(W)
        m.update(consts[hf])
        maps.append({k: m[k] for k in names})
    res = run_bass_kernel_spmd(P.nc, maps, core_ids=list(range(8)))
    out = np.zeros((NB, SEQ, D), np.float32)
    for c in range(8):
        b, hf = c // 2, c % 2
        out[b, hf * OWN:(hf + 1) * OWN] = np.asarray(res.results[c]["out"], dtype=np.float32)
    return out
```
